# Optimizing a Trainium2 kernel written in Bass

```python
import jax
import jax.numpy as jnp
from jax import lax
import numpy as np

D_MODEL = 1024
BATCH = 4
SEQ = 8192
DEPTH = 2

CHUNK = 64
Q_BLOCK = 128
HEAD_DIM = 64
ROPE_THETA = 10000.0

DSA_HEADS = 8
IDX_HEADS = 4
IDX_DIM = 64
DSA_TOPK_MAX = 256

FOX_HEADS = 8

HGRN_HEADS = 8
HGRN_DK = 64
HGRN_DV = 64

N_BRANCH = 3

N_EXPERTS = 32
TOP_K = 4
D_EXPERT = D_MODEL
SWIGLU_LIMIT = 7.0
SWIGLU_ALPHA = 1.702
MOE_BLOCK = 256

LN_EPS = 1e-5
RMS_EPS = 1e-6
DEEPNORM_ALPHA = (2 * DEPTH) ** 0.25
DEEPNORM_BETA = (8 * DEPTH) ** -0.25

IN_SPLITS = (
    DSA_HEADS * HEAD_DIM, DSA_HEADS * HEAD_DIM, DSA_HEADS * HEAD_DIM,
    IDX_HEADS * IDX_DIM, IDX_DIM, IDX_HEADS,
    FOX_HEADS * HEAD_DIM, FOX_HEADS * HEAD_DIM, FOX_HEADS * HEAD_DIM,
    FOX_HEADS,
    HGRN_HEADS * HGRN_DK, HGRN_HEADS * HGRN_DK,
    HGRN_HEADS * HGRN_DV, HGRN_HEADS * HGRN_DV,
    N_BRANCH * D_MODEL,
)
D_IN = sum(IN_SPLITS)
SPLIT_POINTS = tuple(int(v) for v in np.cumsum(IN_SPLITS)[:-1])

kernel_name = 'hybrid_dsa_fox_hgrn2_moe_deepnorm'


def layer_norm(x, g, b):
    x32 = x.astype(jnp.float32)
    mu = jnp.mean(x32, axis=-1, keepdims=True)
    var = jnp.mean(jnp.square(x32 - mu), axis=-1, keepdims=True)
    return ((x32 - mu) * lax.rsqrt(var + LN_EPS) * g + b).astype(x.dtype)


def rope(t, pos):
    half = t.shape[-1] // 2
    inv = ROPE_THETA ** (-jnp.arange(half, dtype=jnp.float32) / half)
    ang = pos.astype(jnp.float32)[:, None] * inv[None, :]
    cos = jnp.cos(ang)[None, :, None, :]
    sin = jnp.sin(ang)[None, :, None, :]
    t32 = t.astype(jnp.float32)
    t1, t2 = t32[..., :half], t32[..., half:]
    return jnp.concatenate([t1 * cos - t2 * sin, t2 * cos + t1 * sin], axis=-1).astype(t.dtype)


def dsa_attention(q, k, v, iq, ik, iw):
    B, S, H, Dh = q.shape
    k_sel = min(DSA_TOPK_MAX, S // 4)
    n_blk = S // Q_BLOCK
    key_pos = jnp.arange(S)
    scale = Dh ** -0.5
    iscale = IDX_DIM ** -0.5
    ik32 = ik.astype(jnp.float32)
    gather = jax.vmap(lambda t, idx: t[idx])

    def block(i):
        t0 = i * Q_BLOCK
        qpos = t0 + jnp.arange(Q_BLOCK)
        iq_b = lax.dynamic_slice_in_dim(iq, t0, Q_BLOCK, axis=1).astype(jnp.float32)
        iw_b = lax.dynamic_slice_in_dim(iw, t0, Q_BLOCK, axis=1)
        rel = jax.nn.relu(jnp.einsum('bqhd,bsd->bqhs', iq_b, ik32) * iscale)
        score = jnp.einsum('bqhs,bqh->bqs', rel, iw_b)
        admissible = key_pos[None, :] < ((qpos // CHUNK + 1) * CHUNK)[:, None]
        score = jnp.where(admissible[None], score, -jnp.inf)
        sel_score, sel_idx = lax.top_k(score, k_sel)
        valid = jnp.isfinite(sel_score)
        kg = gather(k, sel_idx)
        vg = gather(v, sel_idx)
        q_b = lax.dynamic_slice_in_dim(q, t0, Q_BLOCK, axis=1)
        logits = jnp.einsum('bqhd,bqkhd->bhqk', q_b, kg,
                            preferred_element_type=jnp.float32) * scale
        logits = jnp.where(valid[:, None], logits, -jnp.inf)
        p = jax.nn.softmax(logits, axis=-1)
        return jnp.einsum('bhqk,bqkhd->bqhd', p.astype(v.dtype), vg)

    out = lax.map(block, jnp.arange(n_blk))
    return out.transpose(1, 0, 2, 3, 4).reshape(B, S, H * Dh)


def fox_attention(q, k, v, log_f):
    B, S, H, Dh = q.shape
    n_blk = S // Q_BLOCK
    key_pos = jnp.arange(S)
    scale = Dh ** -0.5
    c = jnp.cumsum(log_f, axis=1).transpose(0, 2, 1)

    def block(i):
        t0 = i * Q_BLOCK
        qpos = t0 + jnp.arange(Q_BLOCK)
        q_b = lax.dynamic_slice_in_dim(q, t0, Q_BLOCK, axis=1)
        c_b = lax.dynamic_slice_in_dim(c, t0, Q_BLOCK, axis=2)
        logits = (jnp.einsum('bqhd,bshd->bhqs', q_b, k,
                             preferred_element_type=jnp.float32) * scale
                  + c_b[..., None] - c[:, :, None, :])
        causal = key_pos[None, :] <= qpos[:, None]
        logits = jnp.where(causal, logits, -jnp.inf)
        p = jax.nn.softmax(logits, axis=-1)
        return jnp.einsum('bhqs,bshd->bqhd', p.astype(v.dtype), v)

    out = lax.map(block, jnp.arange(n_blk))
    return out.transpose(1, 0, 2, 3, 4).reshape(B, S, H * Dh)


def hgrn2_recurrence(q, k, v, log_f):
    B, S, H, DK = q.shape
    DV = v.shape[-1]
    n_c = S // CHUNK

    def to_chunks(t):
        return t.reshape(B, n_c, CHUNK, H, t.shape[-1]).transpose(1, 0, 3, 2, 4)

    tri = jnp.tril(jnp.ones((CHUNK, CHUNK), dtype=bool))

    def step(state, inp):
        qc, kc, vc, lfc = inp
        b = jnp.cumsum(lfc, axis=2)
        inter = jnp.einsum('bhtd,bhde->bhte', qc * jnp.exp(b), state)
        decay = jnp.exp(jnp.where(tri[:, :, None],
                                  b[:, :, :, None, :] - b[:, :, None, :, :], -jnp.inf))
        scores = jnp.einsum('bhtsd,bhsd->bhts', qc[:, :, :, None, :] * decay, kc)
        intra = jnp.einsum('bhts,bhse->bhte', scores, vc)
        b_last = b[:, :, -1, :]
        new_state = (jnp.exp(b_last)[..., None] * state
                     + jnp.einsum('bhsd,bhse->bhde', kc * jnp.exp(b_last[:, :, None, :] - b), vc))
        return new_state, inter + intra

    state0 = jnp.zeros((B, H, DK, DV), jnp.float32)
    _, out = lax.scan(step, state0, (to_chunks(q), to_chunks(k), to_chunks(v), to_chunks(log_f)))
    return out.transpose(1, 0, 3, 2, 4).reshape(B, S, H, DV)


def token_mixers(x, w_in, b_fox_f, lb, hgrn_norm_g, w_pa, w_pb, w_pc, w_out):
    B, S, _ = x.shape
    pos = jnp.arange(S)
    z = x @ w_in
    (aq, ak, av, iq, ik, iw, fq, fk, fv, ff, cq, cf, ci, cg, gt) = jnp.split(z, SPLIT_POINTS, axis=-1)

    def heads(t, h):
        return t.reshape(B, S, h, -1)

    aq = rope(heads(aq, DSA_HEADS), pos)
    ak = rope(heads(ak, DSA_HEADS), pos)
    av = heads(av, DSA_HEADS)
    iq = rope(heads(iq, IDX_HEADS), pos)
    ik = rope(ik[:, :, None, :], pos)[:, :, 0, :]
    iw = iw.astype(jnp.float32) * IDX_HEADS ** -0.5
    o_a = dsa_attention(aq, ak, av, iq, ik, iw)

    log_f_fox = jax.nn.log_sigmoid(ff.astype(jnp.float32) + b_fox_f.astype(jnp.float32))
    o_b = fox_attention(heads(fq, FOX_HEADS), heads(fk, FOX_HEADS), heads(fv, FOX_HEADS), log_f_fox)

    zf = cf.astype(jnp.float32)
    log_f_c = jnp.logaddexp(jnp.log(lb), jnp.log1p(-lb) + jax.nn.log_sigmoid(zf))
    k_c = (1.0 - lb) * jax.nn.sigmoid(-zf)
    o_c = hgrn2_recurrence(heads(cq.astype(jnp.float32), HGRN_HEADS), heads(k_c, HGRN_HEADS),
                           heads(ci.astype(jnp.float32), HGRN_HEADS), heads(log_f_c, HGRN_HEADS))
    o_c = (o_c * lax.rsqrt(jnp.mean(jnp.square(o_c), axis=-1, keepdims=True) + RMS_EPS)
           * hgrn_norm_g.astype(jnp.float32).reshape(HGRN_HEADS, HGRN_DV)
           * jax.nn.sigmoid(heads(cg.astype(jnp.float32), HGRN_HEADS)))
    o_c = o_c.reshape(B, S, HGRN_HEADS * HGRN_DV).astype(x.dtype)

    g = jax.nn.sigmoid(gt.reshape(B, S, N_BRANCH, D_MODEL))
    merged = g[:, :, 0] * (o_a @ w_pa) + g[:, :, 1] * (o_b @ w_pb) + g[:, :, 2] * (o_c @ w_pc)
    return merged @ w_out


def moe_ffn(h, w_router, b_router, w_gu, b_gu, w_down, b_down):
    B, S, D = h.shape
    xt = h.reshape(-1, D)
    n_tok = xt.shape[0]
    n_assign = n_tok * TOP_K
    logits = xt.astype(jnp.float32) @ w_router.astype(jnp.float32) + b_router.astype(jnp.float32)
    top_val, top_idx = lax.top_k(logits, TOP_K)
    gates = jax.nn.softmax(top_val, axis=-1)

    flat_e = top_idx.reshape(-1).astype(jnp.int32)
    order = jnp.argsort(flat_e).astype(jnp.int32)
    sorted_e = flat_e[order]
    counts = jnp.bincount(flat_e, length=N_EXPERTS).astype(jnp.int32)
    padded = (counts + MOE_BLOCK - 1) // MOE_BLOCK * MOE_BLOCK
    pad_end = jnp.cumsum(padded)
    pad_start = pad_end - padded
    start = jnp.cumsum(counts) - counts
    rank = jnp.arange(n_assign, dtype=jnp.int32) - start[sorted_e]
    dest = pad_start[sorted_e] + rank
    n_blocks = -(-(n_assign + N_EXPERTS * (MOE_BLOCK - 1)) // MOE_BLOCK)
    tok_sorted = order // TOP_K
    slot_tok = jnp.zeros((n_blocks * MOE_BLOCK,), jnp.int32).at[dest].set(tok_sorted)
    blk_e = jnp.searchsorted(pad_end, jnp.arange(n_blocks, dtype=jnp.int32) * MOE_BLOCK, side='right')
    blk_e = jnp.minimum(blk_e, N_EXPERTS - 1)

    def expert_block(args):
        tok, e = args
        xb = xt[tok]
        gu = xb @ w_gu[e] + b_gu[e]
        gate = jnp.minimum(gu[:, :D_EXPERT], SWIGLU_LIMIT)
        up = jnp.clip(gu[:, D_EXPERT:], -SWIGLU_LIMIT, SWIGLU_LIMIT)
        act = gate * jax.nn.sigmoid(SWIGLU_ALPHA * gate) * (up + 1.0)
        return act @ w_down[e] + b_down[e]

    y_slots = lax.map(expert_block, (slot_tok.reshape(n_blocks, MOE_BLOCK), blk_e))
    y_slots = y_slots.reshape(n_blocks * MOE_BLOCK, D)
    gate_sorted = gates.reshape(-1)[order].astype(y_slots.dtype)
    y_assign = y_slots[dest] * gate_sorted[:, None]
    out = jax.ops.segment_sum(y_assign, tok_sorted, num_segments=n_tok)
    return out.reshape(B, S, D).astype(h.dtype)


def setup_inputs(seed: int = 0) -> dict:
    key = jax.random.key(seed)
    ks = jax.random.split(key, 20)
    f32 = jnp.float32
    wa = DSA_HEADS * HEAD_DIM
    wb = FOX_HEADS * HEAD_DIM
    wc = HGRN_HEADS * HGRN_DV
    nrm = lambda k, shape: jax.random.normal(k, shape, f32)
    return {
        'x': nrm(ks[0], (BATCH, SEQ, D_MODEL)),
        'w_in': nrm(ks[1], (DEPTH, D_MODEL, D_IN)) * D_MODEL ** -0.5,
        'b_fox_f': 3.0 + 0.1 * nrm(ks[2], (DEPTH, FOX_HEADS)),
        'hgrn_lb_logits': 0.5 * nrm(ks[3], (DEPTH, HGRN_HEADS * HGRN_DK)),
        'hgrn_norm_g': 1.0 + 0.02 * nrm(ks[4], (DEPTH, HGRN_HEADS * HGRN_DV)),
        'w_branch_a': nrm(ks[5], (DEPTH, wa, D_MODEL)) * wa ** -0.5 * DEEPNORM_BETA,
        'w_branch_b': nrm(ks[6], (DEPTH, wb, D_MODEL)) * wb ** -0.5 * DEEPNORM_BETA,
        'w_branch_c': nrm(ks[7], (DEPTH, wc, D_MODEL)) * wc ** -0.5 * DEEPNORM_BETA,
        'w_out': nrm(ks[8], (DEPTH, D_MODEL, D_MODEL)) * D_MODEL ** -0.5 * DEEPNORM_BETA,
        'ln1_g': 1.0 + 0.02 * nrm(ks[9], (DEPTH, D_MODEL)),
        'ln1_b': 0.02 * nrm(ks[10], (DEPTH, D_MODEL)),
        'w_router': nrm(ks[11], (DEPTH, D_MODEL, N_EXPERTS)) * D_MODEL ** -0.5,
        'b_router': 0.01 * nrm(ks[12], (DEPTH, N_EXPERTS)),
        'w_gu': nrm(ks[13], (DEPTH, N_EXPERTS, D_MODEL, 2 * D_EXPERT)) * D_MODEL ** -0.5,
        'b_gu': 0.01 * nrm(ks[14], (DEPTH, N_EXPERTS, 2 * D_EXPERT)),
        'w_down': nrm(ks[15], (DEPTH, N_EXPERTS, D_EXPERT, D_MODEL)) * D_EXPERT ** -0.5 * DEEPNORM_BETA,
        'b_down': 0.01 * nrm(ks[16], (DEPTH, N_EXPERTS, D_MODEL)),
        'ln2_g': 1.0 + 0.02 * nrm(ks[17], (DEPTH, D_MODEL)),
        'ln2_b': 0.02 * nrm(ks[18], (DEPTH, D_MODEL)),
    }


def reference(x, w_in, b_fox_f, hgrn_lb_logits, hgrn_norm_g, w_branch_a, w_branch_b, w_branch_c,
              w_out, ln1_g, ln1_b, w_router, b_router, w_gu, b_gu, w_down, b_down, ln2_g, ln2_b):
    p = jax.nn.softmax(hgrn_lb_logits.astype(jnp.float32), axis=0)
    lbs = jnp.cumsum(p, axis=0)
    lbs = lbs - lbs[0]
    for l in range(DEPTH):
        mix = token_mixers(x, w_in[l], b_fox_f[l], lbs[l], hgrn_norm_g[l],
                           w_branch_a[l], w_branch_b[l], w_branch_c[l], w_out[l])
        x = layer_norm(DEEPNORM_ALPHA * x + mix, ln1_g[l], ln1_b[l])
        ffn = moe_ffn(x, w_router[l], b_router[l], w_gu[l], b_gu[l], w_down[l], b_down[l])
        x = layer_norm(DEEPNORM_ALPHA * x + ffn, ln2_g[l], ln2_b[l])
    return x
```

```python
import numpy as np
import ml_dtypes
import concourse.bass as bass
import concourse.mybir as mybir
from concourse.bass_utils import run_bass_kernel_spmd

F32 = mybir.dt.float32
BF16 = mybir.dt.bfloat16
I32 = mybir.dt.int32
ALU = mybir.AluOpType
AF = mybir.ActivationFunctionType
AX = mybir.AxisListType
NPBF = ml_dtypes.bfloat16

D_MODEL = 1024
SEQ = 8192
BATCH = 4
DEPTH = 2
NCORES = 8
TOK = 4096
ALPHA = (2 * DEPTH) ** 0.25
N_EXP = 32

class Prog:
    def __init__(self, nc):
        self.nc = nc
        self.ops = []
        self.last_w = {}
        self.readers = {}

    def op(self, eng, fn, reads=(), writes=(), dma=None):
        idx = len(self.ops)
        deps = set()
        for b in reads:
            w = self.last_w.get(b)
            if w is not None:
                deps.add(w)
        for b in writes:
            w = self.last_w.get(b)
            if w is not None:
                deps.add(w)
            for r in self.readers.get(b, ()):
                deps.add(r)
        for b in writes:
            self.last_w[b] = idx
            self.readers[b] = []
        for b in reads:
            if b not in writes:
                self.readers.setdefault(b, []).append(idx)
        deps.discard(idx)
        self.ops.append(dict(eng=eng, fn=fn, deps=deps, dma=dma))
        return idx

    def dma(self, q, out, in_, reads=(), writes=(), key=None, **kw):
        assert key is not None
        return self.op(q, lambda e: e.dma_start(out=out, in_=in_, **kw), reads, writes, dma=key)

    def mm(self, out, lhsT, rhs, start, stop, reads=(), writes=()):
        return self.op('pe', lambda e: e.matmul(out, lhsT, rhs, start=start, stop=stop), reads, writes)

    def tr(self, out, in_, ident, reads=(), writes=()):
        return self.op('pe', lambda e: e.transpose(out, in_, ident), reads, writes)

    def barrier(self):
        last = {}
        for i, o in enumerate(self.ops):
            if o['fn'] is None:
                continue
            last[('dma', o['dma']) if o['dma'] is not None else ('eng', o['eng'])] = i
        deps = set(last.values())
        for eng in ('pe', 'act', 'dve', 'pool', 'sp'):
            self.ops.append(dict(eng=eng, fn=None, deps=set(deps), dma=None))
        self.last_w = {}
        self.readers = {}

    def emit(self, final_keys=()):
        nc = self.nc
        ops = self.ops
        n = len(ops)
        need = [False] * n
        for o in ops:
            best = {}
            ed = set()
            for d in o['deps']:
                po = ops[d]
                if po['eng'] == 'pe' and o['eng'] == 'pe' and po['dma'] is None and o['dma'] is None:
                    continue
                if po['dma'] is not None:
                    ed.add(d)
                else:
                    best[po['eng']] = max(best.get(po['eng'], -1), d)
            ed.update(best.values())
            o['edeps'] = ed
            for d in ed:
                need[d] = True
        for i, o in enumerate(ops):
            if o['dma'] is not None:
                need[i] = True
        eng_cnt = {}
        key_cnt = {}
        sig = [None] * n
        key_before = [None] * n
        key_events = {}
        for i, o in enumerate(ops):
            if o['dma'] is not None:
                k = o['dma']
                key_cnt[k] = key_cnt.get(k, 0) + 1
                key_events.setdefault(k, []).append(i)
                sig[i] = ('dma:' + str(k), 16 * key_cnt[k])
            elif need[i]:
                e = o['eng']
                eng_cnt[e] = eng_cnt.get(e, 0) + 1
                sig[i] = ('eng:' + e, eng_cnt[e])
        semnames = sorted(set(s[0] for s in sig if s is not None))
        import bisect
        from contextlib import ExitStack
        with ExitStack() as st:
            sems = {nm: st.enter_context(nc.semaphore('s%d' % j)) for j, nm in enumerate(semnames)}
            block = st.enter_context(nc.Block())
            by_eng = {}
            for i, o in enumerate(ops):
                by_eng.setdefault(o['eng'], []).append(i)

            def run(engname, e):
                waited = {}
                for i in by_eng.get(engname, []):
                    o = ops[i]
                    wl = {}
                    for d in o['edeps']:
                        po = ops[d]
                        nm, val = sig[d]
                        if po['dma'] is not None:
                            ev = key_events[po['dma']]
                            cnt = bisect.bisect_left(ev, i)
                            val = 16 * cnt
                        if val > wl.get(nm, 0):
                            wl[nm] = val
                    for nm, val in wl.items():
                        if val > waited.get(nm, 0):
                            e.wait_ge(sems[nm], val)
                            waited[nm] = val
                    if o['fn'] is None:
                        continue
                    ins = o['fn'](e)
                    if sig[i] is not None:
                        nm, val = sig[i]
                        ins.then_inc(sems[nm], 16 if o['dma'] is not None else 1)
                for k in final_keys:
                    ev = key_events.get(k, [])
                    if ev and ops[ev[-1]]['eng'] == engname:
                        e.wait_ge(sems['dma:' + str(k)], 16 * len(ev))

            @block.tensor
            def _(e):
                run('pe', e)

            @block.scalar
            def _(e):
                run('act', e)

            @block.vector
            def _(e):
                run('dve', e)

            @block.gpsimd
            def _(e):
                run('pool', e)

            @block.sync
            def _(e):
                run('sp', e)
        self.stats = dict(n_ops=n, eng_cnt=eng_cnt, n_sems=len(semnames))


SPL = dict(aq=(0, 512), ak=(512, 1024), av=(1024, 1536), iq=(1536, 1792), ik=(1792, 1856), iw=(1856, 1860),
           fq=(1860, 2372), fk=(2372, 2884), fv=(2884, 3396), ff=(3396, 3404), cq=(3404, 3916), cf=(3916, 4428),
           ci=(4428, 4940), cg=(4940, 5452), gt=(5452, 8524))


def _rot_cols(lo, hi):
    c = np.arange(lo, hi)
    base = lo + ((c - lo) // 64) * 64
    return base + ((c - base + 32) % 64)


def k1_weight_columns():
    cols = []
    off = {}
    pos = 0

    def add(name, idx):
        nonlocal pos
        off[name] = (pos, len(idx))
        cols.append(np.asarray(idx))
        pos += len(idx)

    for nm in ('aq', 'ak', 'iq', 'ik'):
        lo, hi = SPL[nm]
        add(nm, np.arange(lo, hi))
        add(nm + '_rot', _rot_cols(lo, hi))
    for nm in ('fq', 'fk', 'cq', 'cf', 'cg', 'gt', 'av', 'fv', 'ci'):
        lo, hi = SPL[nm]
        add(nm, np.arange(lo, hi))
    add('small', np.concatenate([np.arange(*SPL['iw']), np.arange(*SPL['ff'])]))
    return np.concatenate(cols), off


K1_COLS, K1_OFF = k1_weight_columns()
K1_NCOLS = len(K1_COLS)


def rope_tables(positions):
    half = 32
    inv = (10000.0 ** (-np.arange(half, dtype=np.float32) / half)).astype(np.float32)
    ang = positions.astype(np.float32)[None, :] * inv[:, None]
    cos = np.cos(ang).astype(np.float32)
    sin = np.sin(ang).astype(np.float32)
    cosT = np.concatenate([cos, cos, cos, cos], axis=0)
    sinT = np.concatenate([-sin, sin, -sin, sin], axis=0)
    return np.ascontiguousarray(cosT), np.ascontiguousarray(sinT)


def build_k1(layer=0):
    nc = bass.Bass("TRN2", target_bir_lowering=False)
    P = Prog(nc)
    phase_k1(nc, P, '', layer)
    P.emit(final_keys=['out'])
    return nc, P


def phase_k1(nc, P, px, layer=0, ext=None):
    ext = ext or {}
    T = TOK
    NT = T // 128
    NTC = T // 512
    x_d = ext['x'] if 'x' in ext else nc.dram_tensor(px + "x", [T, D_MODEL], F32, kind="ExternalInput").ap()
    w_d = nc.dram_tensor(px + "w", [D_MODEL, K1_NCOLS], F32, kind="ExternalInput").ap()
    cos_d = nc.dram_tensor(px + "cosT", [128, T], F32, kind="ExternalInput").ap()
    sin_d = nc.dram_tensor(px + "sinT", [128, T], F32, kind="ExternalInput").ap()
    bfox_d = nc.dram_tensor(px + "bfox", [1, 8], F32, kind="ExternalInput").ap()
    lbl_d = nc.dram_tensor(px + "lbl", [128, 2, 4], F32, kind="ExternalInput").ap()
    ident_d = nc.dram_tensor(px + "ident", [128, 128], F32, kind="ExternalInput").ap()
    rmask_d = nc.dram_tensor(px + "rmask", [128, 512], F32, kind="ExternalInput").ap()

    def out_t(name, shape, dt):
        return nc.dram_tensor(px + name, shape, dt, kind="ExternalOutput").ap()

    o_aqT = out_t("aqT", [512, T], BF16)
    o_akT = out_t("akT", [512, T], BF16)
    o_iqT = out_t("iqT", [256, T], BF16)
    o_ikT = out_t("ikT", [64, T], BF16)
    o_fqT = out_t("fqT", [512, T], BF16)
    o_fkT = out_t("fkT", [512, T], BF16)
    o_av = out_t("av", [T, 512], BF16)
    o_fv = out_t("fv", [T, 512], BF16)
    o_cv = out_t("cv", [T, 512], BF16)
    o_small = out_t("small", [T, 12], F32)
    o_qpT = out_t("qpT", [512, T], BF16)
    o_kpT = out_t("kpT", [512, T], BF16)
    o_kp = out_t("kp", [T, 512], BF16)
    o_em = out_t("em", [512, T // 64], F32)
    o_g = out_t("g", [512, T // 64], F32)
    o_cgT = out_t("cgT", [512, T], BF16)
    o_gT = out_t("gT", [3072, T], BF16)

    from contextlib import ExitStack
    with ExitStack() as st:
        def sb(name, shape, dt):
            return st.enter_context(nc.sbuf_tensor(px + name, shape, dt))
        xT = sb("xT", [128, 8, T], BF16)
        cosT = sb("cosT_s", [128, T], F32)
        sinT = sb("sinT_s", [128, T], F32)
        ident = sb("ident_s", [128, 128], F32)
        identb = sb("identb", [128, 128], BF16)
        rmask = sb("rmask_s", [128, 512], F32)
        lb = sb("lb_s", [128, 4], F32)
        oml = sb("oml", [128, 4], F32)
        noml = sb("noml", [128, 4], F32)
        bfox = sb("bfox_s", [128, 8], F32)
        xin = [sb("xin%d" % i, [128, D_MODEL], F32) for i in range(2)]
        xbf = [sb("xbf%d" % i, [128, D_MODEL], BF16) for i in range(2)]
        wst = [sb("wst%d" % i, [128, 8, 512], F32) for i in range(2)]
        wbf = [sb("wbf%d" % i, [128, 8, 512], BF16) for i in range(2)]
        t1 = [sb("t1_%d" % i, [128, 512], F32) for i in range(2)]
        t2 = [sb("t2_%d" % i, [128, 512], F32) for i in range(2)]
        t3 = [sb("t3_%d" % i, [128, 512], F32) for i in range(2)]
        t4 = [sb("t4_%d" % i, [128, 512], F32) for i in range(2)]
        ob = [sb("ob%d" % i, [128, 512], BF16) for i in range(4)]
        ob2 = [sb("ob2_%d" % i, [128, 512], BF16) for i in range(2)]
        osm = [sb("osm%d" % i, [128, 12], F32) for i in range(2)]
        emg = sb("emg", [128, 2, 4, T // 64], F32)
        ps = st.enter_context(nc.psum_tensor(px + "ps", [128, 8, 512], F32))
        psb = ps
        cnt = dict(ps=0, ob=0, ob2=0, t=0, w=0, x=0, osm=0)

        def nxt(k, n):
            v = cnt[k] % n
            cnt[k] += 1
            return v

        P.dma('sp', cosT[:], cos_d, writes=['cosT'], key='c0')
        P.dma('sp', sinT[:], sin_d, writes=['sinT'], key='c1')
        P.dma('sp', ident[:], ident_d, writes=['ident'], key='c2')
        P.dma('sp', rmask[:], rmask_d, writes=['rmask'], key='c3')
        lbl = sb("lbl_s", [128, 2, 4], F32)
        P.dma('sp', lbl[:], lbl_d, writes=['lbl'], key='c4')
        if layer == 0:
            P.op('dve', lambda e: e.memset(lb[:], 0.0), writes=['lb'])
        else:
            P.op('dve', lambda e: e.tensor_tensor(lb[:], lbl[:, 1, :], lbl[:, 0, :], ALU.subtract), reads=['lbl'], writes=['lb'])
            P.op('act', lambda e: e.activation(lb[:], lb[:], AF.Sigmoid), reads=['lb'], writes=['lb'])
        P.dma('sp', bfox[:], bfox_d.partition_broadcast(128), writes=['bfox'], key='c5')
        P.op('dve', lambda e: e.tensor_copy(identb[:], ident[:]), reads=['ident'], writes=['identb'])
        P.op('dve', lambda e: e.tensor_scalar(oml[:], lb[:], -1.0, 1.0, ALU.mult, ALU.add), reads=['lb'], writes=['oml'])
        P.op('dve', lambda e: e.tensor_scalar(noml[:], lb[:], 1.0, -1.0, ALU.mult, ALU.add), reads=['lb'], writes=['noml'])

        for ti in range(NT):
            b = nxt('x', 2)
            P.dma('sp', xin[b][:], x_d[ti * 128:(ti + 1) * 128, :], writes=[('xin', b)], key=('xin', b))
            P.op('act', lambda e, b=b: e.copy(xbf[b][:], xin[b][:]), reads=[('xin', b)], writes=[('xbf', b)])
            for half in range(2):
                pb = nxt('ps', 8)
                pst = ps[:, pb, :].bitcast(BF16)
                for j in range(4):
                    k = half * 4 + j
                    P.tr(pst[:, j * 128:(j + 1) * 128], xbf[b][:, k * 128:(k + 1) * 128], identb[:],
                         reads=[('xbf', b), 'identb'], writes=[('ps', pb)])
                P.op('dve', lambda e, pst=pst, half=half, ti=ti: e.tensor_copy(
                    xT[:, half * 4:(half + 1) * 4, ti * 128:(ti + 1) * 128],
                    pst[:, 0:512].rearrange("p (j t) -> p j t", j=4)),
                    reads=[('ps', pb)], writes=[('xT', ti)])

        w_v = w_d.rearrange("(c p) n -> p c n", p=128)

        def load_w(col0, ncols):
            b = nxt('w', 2)
            P.dma('sp', wst[b][:, :, 0:ncols], w_v[:, :, col0:col0 + ncols], writes=[('wst', b)], key=('wst', b))
            P.op('pool', lambda e: e.tensor_copy(wbf[b][:, :, 0:ncols], wst[b][:, :, 0:ncols]),
                 reads=[('wst', b)], writes=[('wbf', b)])
            return b

        def fm_matmul(wb, c0, m, tc):
            pb = nxt('ps', 8)
            for k in range(8):
                P.mm(ps[0:m, pb, :], wbf[wb][:, k, c0:c0 + m], xT[:, k, tc * 512:(tc + 1) * 512], k == 0, k == 7,
                     reads=[('wbf', wb)] + [('xT', tc * 4 + i) for i in range(4)], writes=[('ps', pb)])
            return pb

        all_xT = [('xT', i) for i in range(NT)]

        jobs = []
        def rope_group(nm, o_d):
            s0, n = K1_OFF[nm]
            r0, _ = K1_OFF[nm + '_rot']
            nblk = max(1, n // 128)
            m = min(n, 128)
            for bi in range(nblk):
              def load(bi=bi):
                b = nxt('w', 2)
                P.dma('sp', wst[b][:, :, 0:m], w_v[:, :, s0 + bi * 128:s0 + bi * 128 + m], writes=[('wst', b)], key=('wst', b))
                P.dma('sp', wst[b][:, :, 128:128 + m], w_v[:, :, r0 + bi * 128:r0 + bi * 128 + m], writes=[('wst', b)], key=('wst', b))
                P.op('pool', lambda e, b=b: e.tensor_copy(wbf[b][:, :, 0:256], wst[b][:, :, 0:256]),
                     reads=[('wst', b)], writes=[('wbf', b)])
                return b
              def comp(b, bi=bi):
                for tc in range(NTC):
                    p0 = fm_matmul(b, 0, m, tc)
                    p1 = fm_matmul(b, 128, m, tc)
                    tb = nxt('t', 2)
                    sl = slice(tc * 512, (tc + 1) * 512)
                    P.op('dve', lambda e, p0=p0, tb=tb, sl=sl: e.tensor_tensor(t1[tb][0:m, :], ps[0:m, p0, :], cosT[0:m, sl], ALU.mult),
                         reads=[('ps', p0), 'cosT'], writes=[('t1', tb)])
                    P.op('dve', lambda e, p1=p1, tb=tb, sl=sl: e.tensor_tensor(t2[tb][0:m, :], ps[0:m, p1, :], sinT[0:m, sl], ALU.mult),
                         reads=[('ps', p1), 'sinT'], writes=[('t2', tb)])
                    o = nxt('ob', 4)
                    P.op('pool', lambda e, tb=tb, o=o: e.tensor_tensor(ob[o][0:m, :], t1[tb][0:m, :], t2[tb][0:m, :], ALU.add),
                         reads=[('t1', tb), ('t2', tb)], writes=[('ob', o)])
                    P.dma('sp', o_d[bi * 128:bi * 128 + m, sl], ob[o][0:m, :], reads=[('ob', o)], key='out')
              jobs.append((load, comp))

        rope_group('aq', o_aqT)
        rope_group('ak', o_akT)
        rope_group('iq', o_iqT)
        rope_group('ik', o_ikT)

        def plain_group(nm, o_d, func):
            s0, n = K1_OFF[nm]
            for g0 in range(0, n, 512):
                gn = min(512, n - g0)
                def load(g0=g0, gn=gn):
                    return load_w(s0 + g0, gn)
                def comp(b, g0=g0, gn=gn):
                  for bi in range(gn // 128):
                    for tc in range(NTC):
                        p0 = fm_matmul(b, bi * 128, 128, tc)
                        o = nxt('ob', 4)
                        sl = slice(tc * 512, (tc + 1) * 512)
                        P.op('act', lambda e, p0=p0, o=o: e.activation(ob[o][:], ps[:, p0, :], func),
                             reads=[('ps', p0)], writes=[('ob', o)])
                        r0 = g0 + bi * 128
                        P.dma('sp', o_d[r0:r0 + 128, sl], ob[o][:], reads=[('ob', o)], key='out')
                jobs.append((load, comp))

        plain_group('fq', o_fqT, AF.Copy)
        plain_group('fk', o_fkT, AF.Copy)
        plain_group('cg', o_cgT, AF.Sigmoid)
        plain_group('gt', o_gT, AF.Sigmoid)

        sq0, _ = K1_OFF['cq']
        sf0, _ = K1_OFF['cf']
        for pr in range(4):
          def load(pr=pr):
            b = nxt('w', 2)
            P.dma('sp', wst[b][:, :, 0:128], w_v[:, :, sq0 + pr * 128:sq0 + (pr + 1) * 128], writes=[('wst', b)], key=('wst', b))
            P.dma('sp', wst[b][:, :, 128:256], w_v[:, :, sf0 + pr * 128:sf0 + (pr + 1) * 128], writes=[('wst', b)], key=('wst', b))
            P.op('pool', lambda e, b=b: e.tensor_copy(wbf[b][:, :, 0:256], wst[b][:, :, 0:256]),
                 reads=[('wst', b)], writes=[('wbf', b)])
            return b
          def comp(b, pr=pr):
            for tc in range(NTC):
                pq = fm_matmul(b, 0, 128, tc)
                pf = fm_matmul(b, 128, 128, tc)
                tb = nxt('t', 2)
                sl = slice(tc * 512, (tc + 1) * 512)
                P.op('act', lambda e, pf=pf, tb=tb: e.activation(t1[tb][:], ps[:, pf, :], AF.Sigmoid),
                     reads=[('ps', pf)], writes=[('t1', tb)])
                P.op('act', lambda e, tb=tb, pr=pr: e.activation(t2[tb][:], t1[tb][:], AF.Ln, bias=lb[:, pr:pr + 1], scale=oml[:, pr:pr + 1]),
                     reads=[('t1', tb), 'lb', 'oml'], writes=[('t2', tb)])
                P.op('pool', lambda e, tb=tb, pr=pr: e.tensor_scalar(t3[tb][:], t1[tb][:], noml[:, pr:pr + 1], oml[:, pr:pr + 1], ALU.mult, ALU.add),
                     reads=[('t1', tb), 'oml', 'noml'], writes=[('t3', tb)])
                P.op('dve', lambda e, tb=tb: e.tensor_tensor_scan(t4[tb][:], rmask[:], t2[tb][:], 0.0, ALU.mult, ALU.add),
                     reads=[('t2', tb), 'rmask'], writes=[('t4', tb)])
                b3 = t4[tb][:].rearrange("p (c t) -> p c t", t=64)
                P.op('act', lambda e, b3=b3, pr=pr, tc=tc: e.activation(emg[:, 0, pr, tc * 8:(tc + 1) * 8], b3[:, :, 31], AF.Exp),
                     reads=[('t4', tb)], writes=[('emg', pr)])
                P.op('dve', lambda e, tb=tb, b3=b3: e.tensor_tensor(t2[tb][:].rearrange("p (c t) -> p c t", t=64), b3,
                                                                   b3[:, :, 31:32].to_broadcast([128, 8, 64]), ALU.subtract),
                     reads=[('t4', tb)], writes=[('t2', tb)])
                P.op('act', lambda e, tb=tb: e.activation(t4[tb][:], t2[tb][:], AF.Exp),
                     reads=[('t2', tb)], writes=[('t4', tb)])
                P.op('act', lambda e, tb=tb: e.activation(t1[tb][:], t2[tb][:], AF.Exp, scale=-1.0),
                     reads=[('t2', tb)], writes=[('t1', tb)])
                P.op('pool', lambda e, tb=tb, pr=pr, tc=tc: e.tensor_copy(emg[:, 1, pr, tc * 8:(tc + 1) * 8],
                                                                       t4[tb][:].rearrange("p (c t) -> p c t", t=64)[:, :, 63]),
                     reads=[('t4', tb)], writes=[('emg', pr)])
                o = nxt('ob', 4)
                P.op('dve', lambda e, pq=pq, tb=tb, o=o: e.tensor_tensor(ob[o][:], ps[:, pq, :], t4[tb][:], ALU.mult),
                     reads=[('ps', pq), ('t4', tb)], writes=[('ob', o)])
                P.dma('sp', o_qpT[pr * 128:(pr + 1) * 128, sl], ob[o][:], reads=[('ob', o)], key='out')
                o2 = nxt('ob', 4)
                P.op('pool', lambda e, tb=tb, o2=o2: e.tensor_tensor(ob[o2][:], t3[tb][:], t1[tb][:], ALU.mult),
                     reads=[('t3', tb), ('t1', tb)], writes=[('ob', o2)])
                P.dma('sp', o_kpT[pr * 128:(pr + 1) * 128, sl], ob[o2][:], reads=[('ob', o2)], key='out')
                pb = nxt('ps', 8)
                pst = ps[:, pb, :].bitcast(BF16)
                for j in range(4):
                    P.tr(pst[:, j * 128:(j + 1) * 128], ob[o2][:, j * 128:(j + 1) * 128], identb[:],
                         reads=[('ob', o2), 'identb'], writes=[('ps', pb)])
                o3 = nxt('ob2', 2)
                P.op('act', lambda e, pst=pst, o3=o3: e.copy(ob2[o3][:], pst[:, 0:512]),
                     reads=[('ps', pb)], writes=[('ob2', o3)])
                P.dma('sp', o_kp[tc * 512:(tc + 1) * 512, pr * 128:(pr + 1) * 128].rearrange("(j t) c -> t j c", t=128),
                      ob2[o3][:].rearrange("t (j c) -> t j c", j=4), reads=[('ob2', o3)], key='out')
            P.dma('sp', o_em[pr * 128:(pr + 1) * 128, :], emg[:, 0, pr, :], reads=[('emg', pr)], key='out')
            P.dma('sp', o_g[pr * 128:(pr + 1) * 128, :], emg[:, 1, pr, :], reads=[('emg', pr)], key='out')
          jobs.append((load, comp))

        for nm, o_d in (('av', o_av), ('fv', o_fv), ('ci', o_cv)):
            s0, n = K1_OFF[nm]
            def load(s0=s0):
                return load_w(s0, 512)
            def comp(b, o_d=o_d):
              for ti in range(NT):
                pb = nxt('ps', 8)
                for k in range(8):
                    P.mm(ps[:, pb, :], xT[:, k, ti * 128:(ti + 1) * 128], wbf[b][:, k, :], k == 0, k == 7,
                         reads=[('wbf', b), ('xT', ti)], writes=[('ps', pb)])
                o = nxt('ob', 4)
                P.op('act', lambda e, pb=pb, o=o: e.copy(ob[o][:], ps[:, pb, :]), reads=[('ps', pb)], writes=[('ob', o)])
                P.dma('sp', o_d[ti * 128:(ti + 1) * 128, :], ob[o][:], reads=[('ob', o)], key='out')
            jobs.append((load, comp))

        s0, n = K1_OFF['small']
        def load(s0=s0):
            return load_w(s0, 12)
        def comp(b):
          for ti in range(NT):
            pb = nxt('ps', 8)
            for k in range(8):
                P.mm(ps[:, pb, 0:12], xT[:, k, ti * 128:(ti + 1) * 128], wbf[b][:, k, 0:12], k == 0, k == 7,
                     reads=[('wbf', b), ('xT', ti)], writes=[('ps', pb)])
            o = nxt('osm', 2)
            P.op('dve', lambda e, pb=pb, o=o: e.tensor_scalar(osm[o][:, 0:4], ps[:, pb, 0:4], 1.0 / 16.0, None, ALU.mult),
                 reads=[('ps', pb)], writes=[('osm', o)])
            P.op('dve', lambda e, pb=pb, o=o: e.tensor_tensor(osm[o][:, 4:12], ps[:, pb, 4:12], bfox[:], ALU.add),
                 reads=[('ps', pb), 'bfox'], writes=[('osm', o)])
            P.op('act', lambda e, o=o: e.activation(osm[o][:, 4:12], osm[o][:, 4:12], AF.Sigmoid),
                 reads=[('osm', o)], writes=[('osm', o)])
            P.op('act', lambda e, o=o: e.activation(osm[o][:, 4:12], osm[o][:, 4:12], AF.Ln),
                 reads=[('osm', o)], writes=[('osm', o)])
            P.dma('sp', o_small[ti * 128:(ti + 1) * 128, :], osm[o][:], reads=[('osm', o)], key='out')
        jobs.append((load, comp))

        nb = jobs[0][0]()
        for ji in range(len(jobs)):
            cur = nb
            if ji + 1 < len(jobs):
                nb = jobs[ji + 1][0]()
            jobs[ji][1](cur)

    return dict(aqT=o_aqT)


def k1_inputs(core, x_slice, w_perm, b_fox, lb_logits):
    pos = (np.arange(TOK) + (core % 2) * TOK).astype(np.float32)
    cosT, sinT = rope_tables(pos)
    rmask = np.ones((128, 512), np.float32)
    rmask[:, ::64] = 0.0
    lbl = np.ascontiguousarray(lb_logits.reshape(2, 4, 128).transpose(2, 0, 1)).astype(np.float32)
    return dict(x=np.ascontiguousarray(x_slice), w=w_perm, cosT=cosT, sinT=sinT,
                bfox=np.ascontiguousarray(b_fox.reshape(1, 8)).astype(np.float32), lbl=lbl,
                ident=np.eye(128, dtype=np.float32), rmask=rmask)


NSLOT = 8
NBIS = 18
TOPK = 256.0
NEG = -1.0e30


def build_k2a():
    nc = bass.Bass("TRN2", target_bir_lowering=False)
    P = Prog(nc)
    phase_k2a(nc, P, '')
    P.emit(final_keys=['out'])
    return nc, P


def phase_k2a(nc, P, px, ext=None):
    ext = ext or {}
    S = SEQ
    QT = NSLOT * 512
    din = lambda name, shape, dt: ext[name] if name in ext else nc.dram_tensor(px + name, shape, dt, kind="ExternalInput").ap()
    aq_d = din("aq", [4, 128, QT], BF16)
    ak_d = din("ak", [4, 128, S], BF16)
    av_d = din("av", [4, 128, 64, 130], BF16)
    fq_d = din("fq", [4, 128, QT], BF16)
    fk_d = din("fk", [4, 128, S], BF16)
    fv_d = din("fv", [4, 128, 64, 130], BF16)
    iq_d = din("iq", [128, 2, QT], BF16)
    ik_d = din("ik2", [128, S], BF16)
    iw_d = din("iw", [128, NSLOT * 4, 4], F32)
    lf_d = din("lf", [128, 64, 8], F32)
    adm_d = din("adm", [4, 128, 1024], F32)
    cm_d = din("cm", [128, 8, 512], BF16)
    jf_d = din("jflag", [128, 1], F32)
    tri_d = din("tri", [128, 128], F32)
    ident_d = din("identb", [128, 128], BF16)
    pw_d = din("pw2", [128, NBIS], F32)
    oa_d = nc.dram_tensor(px + "oaT", [64, 8, QT], BF16, kind="ExternalOutput").ap()
    ob_d = nc.dram_tensor(px + "obT", [64, 8, QT], BF16, kind="ExternalOutput").ap()

    from contextlib import ExitStack
    with ExitStack() as st:
        def sb(name, shape, dt):
            return st.enter_context(nc.sbuf_tensor(px + name, shape, dt))
        Sc = sb("Sc", [128, S], F32)
        Mk = sb("Mk", [128, S], BF16)
        MT = sb("MT", [128, 64, 512], BF16)
        ik2 = sb("ik2s", [128, S], BF16)
        KT = [sb("KT%d" % i, [128, 2048], BF16) for i in range(2)]
        VA = [sb("VA%d" % i, [128, 16, 130], BF16) for i in range(2)]
        qT = [sb("qT%d" % i, [128, 512], BF16) for i in range(2)]
        iqc = sb("iqc", [128, 2, 512], BF16)
        iw = sb("iws", [128, NSLOT * 4, 4], F32)
        lf = sb("lfs", [128, 64, 8], F32)
        cw = sb("cw", [128, 64, 8], F32)
        offs = sb("offs", [128, 65, 8], F32)
        tot = sb("tot", [128, 64, 8], F32)
        adm = [sb("adm%d" % i, [128, 1024], F32) for i in range(2)]
        cm = sb("cms", [128, 8, 512], BF16)
        jf = sb("jfs", [128, 1], F32)
        tri = sb("tris", [128, 128], F32)
        ones = sb("ones", [128, 128], F32)
        identb = sb("identbs", [128, 128], BF16)
        pw = sb("pws", [128, NBIS], F32)
        rl = [sb("rl%d" % i, [128, 512], F32) for i in range(3)]
        pT = [sb("pT%d" % i, [128, 512], BF16) for i in range(4)]
        U = [sb("U%d" % i, [65, 512], F32) for i in range(2)]
        R = [sb("R%d" % i, [65, 512], F32) for i in range(2)]
        oT = [sb("oT%d" % i, [64, 512], BF16) for i in range(2)]
        bfx = [sb("bfx%d" % i, [128, 64], F32) for i in range(2)]
        cref = sb("cref", [128, 8], F32)
        sm = sb("sm", [128, 16], F32)
        dk = sb("dk", [128, NBIS], F32)
        ps = st.enter_context(nc.psum_tensor(px + "ps", [128, 8, 512], F32))
        cnt = {}

        def nxt(k, n):
            v = cnt.get(k, 0) % n
            cnt[k] = cnt.get(k, 0) + 1
            return v

        for i, (t, d, kn) in enumerate([(ik2, ik_d, 'ik2s'), (iw, iw_d, 'iws'), (lf, lf_d, 'lfs'), (cm, cm_d, 'cms'), (jf, jf_d, 'jfs'),
                                        (tri, tri_d, 'tris'), (identb, ident_d, 'identbs'), (pw, pw_d, 'pws')]):
            P.dma('sp', t[:], d, writes=[kn], key='c%d' % i)
        P.op('pool', lambda e: e.memset(ones[:], 1.0), writes=['ones'])

        lf2 = lf[:].rearrange("p b h -> p (b h)")
        P.mm(ps[:, 0, :], tri[:], lf2, True, True, reads=['tris', 'lfs'], writes=[('ps', 0)])
        P.mm(ps[:, 1, :], ones[:], lf2, True, True, reads=['ones', 'lfs'], writes=[('ps', 1)])
        P.op('act', lambda e: e.copy(tot[:].rearrange("p b h -> p (b h)"), ps[:, 1, :]), reads=[('ps', 1)], writes=['tot'])
        P.op('dve', lambda e: e.memset(offs[:, 0, :], 0.0), writes=['offs'])
        for bl in range(64):
            P.op('dve', lambda e, bl=bl: e.tensor_tensor(offs[:, bl + 1, :], offs[:, bl, :], tot[:, bl, :], ALU.add),
                 reads=['offs', 'tot'], writes=['offs'])
        P.op('dve', lambda e: e.tensor_tensor(cw[:], ps[:, 0, :].rearrange("p (b h) -> p b h", h=8), offs[:, 0:64, :], ALU.add),
             reads=[('ps', 0), 'offs'], writes=['cw'])
        P.op('dve', lambda e: e.tensor_scalar(cw[:], cw[:], -1.0, None, ALU.mult), reads=['cw'], writes=['cw'])

        def gbank():
            return nxt('g', 4)

        def attention(slot, branch):
            nblk = (2 * slot + 2) * 4
            q_d, k_d, v_d, o_d = (aq_d, ak_d, av_d, oa_d) if branch == 'a' else (fq_d, fk_d, fv_d, ob_d)
            jobs = []
            for pr in range(4):
                segs = [(s0, min(16, nblk - s0)) for s0 in range(0, nblk, 16)]
                for si, (s0, sn) in enumerate(segs):
                    def load(pr=pr, s0=s0, sn=sn, si=si):
                        b = nxt('kv', 2)
                        P.dma('sp', KT[b][:, 0:sn * 128], k_d[pr, :, s0 * 128:(s0 + sn) * 128], writes=[('KT', b)], key=('KT', b))
                        P.dma('sp', VA[b][:, 0:sn, :], v_d[pr, :, s0:s0 + sn, :], writes=[('VA', b)], key=('VA', b))
                        qb_ = None
                        if si == 0:
                            qb_ = nxt('q', 2)
                            P.dma('sp', qT[qb_][:], q_d[pr, :, slot * 512:(slot + 1) * 512], writes=[('qT', qb_)], key=('qT', qb_))
                        return (b, qb_)

                    def comp(ld, st_, pr=pr, s0=s0, sn=sn, si=si, last=(si == len(segs) - 1)):
                        b, qb_ = ld
                        if si == 0:
                            st_['q'] = qb_
                            st_['acc'] = [4, 5]
                            if branch == 'b':
                                for hh in range(2):
                                    h = pr * 2 + hh
                                    P.op('dve', lambda e, hh=hh, h=h: e.tensor_scalar(bfx[hh][:, 0:nblk], cw[:, 0:nblk, h], cref[:, h:h + 1], 60.0, ALU.subtract, ALU.min),
                                         reads=['cw', 'cref'], writes=[('bfx', hh)])
                        qb = st_['q']
                        for hh in range(2):
                            acc = st_['acc'][hh]
                            for kl in range(sn):
                                kb = s0 + kl
                                g = gbank()
                                P.mm(ps[:, g, :], KT[b][hh * 64:(hh + 1) * 64, kl * 128:(kl + 1) * 128], qT[qb][hh * 64:(hh + 1) * 64, :], True, True,
                                     reads=[('KT', b), ('qT', qb)], writes=[('ps', g)])
                                pi = nxt('pT', 4)
                                if branch == 'a':
                                    P.op('act', lambda e, g=g, pi=pi: e.activation(pT[pi][:], ps[:, g, :], AF.Exp, scale=0.125),
                                         reads=[('ps', g)], writes=[('pT', pi)])
                                    P.op('pool', lambda e, pi=pi, kb=kb: e.tensor_tensor(pT[pi][:], pT[pi][:], MT[:, kb, :], ALU.mult),
                                         reads=[('pT', pi), 'MT'], writes=[('pT', pi)])
                                else:
                                    P.op('act', lambda e, g=g, pi=pi, hh=hh, kb=kb: e.activation(pT[pi][:], ps[:, g, :], AF.Exp, bias=bfx[hh][:, kb:kb + 1], scale=0.125),
                                         reads=[('ps', g), ('bfx', hh)], writes=[('pT', pi)])
                                    if kb >= nblk - 8:
                                        P.op('pool', lambda e, pi=pi, kb=kb: e.tensor_tensor(pT[pi][:], pT[pi][:], cm[:, kb - (nblk - 8), :], ALU.mult),
                                             reads=[('pT', pi), 'cms'], writes=[('pT', pi)])
                                P.mm(ps[0:65, acc, :], VA[b][:, kl, hh * 65:(hh + 1) * 65], pT[pi][:], kb == 0, kb == nblk - 1,
                                     reads=[('VA', b), ('pT', pi)], writes=[('ps', acc)])
                            if last:
                                h = pr * 2 + hh
                                ui = nxt('U', 2)
                                P.op('act', lambda e, ui=ui, acc=acc: e.copy(U[ui][:], ps[0:65, acc, :]), reads=[('ps', acc)], writes=[('U', ui)])
                                P.op('dve', lambda e, ui=ui: e.reciprocal(R[ui][64:65, :], U[ui][64:65, :]), reads=[('U', ui)], writes=[('R', ui)])
                                P.mm(ps[0:64, 7, :], ones[64:65, 0:64], R[ui][64:65, :], True, True, reads=['ones', ('R', ui)], writes=[('ps', 7)])
                                oi = nxt('oT', 2)
                                P.op('dve', lambda e, ui=ui, oi=oi: e.tensor_tensor(oT[oi][:], U[ui][0:64, :], ps[0:64, 7, :], ALU.mult),
                                     reads=[('U', ui), ('ps', 7)], writes=[('oT', oi)])
                                P.dma('sp', o_d[:, h, slot * 512:(slot + 1) * 512], oT[oi][:], reads=[('oT', oi)], key='out')
                    jobs.append((load, comp))
            return jobs

        def run_jobs(jobs):
            st_ = {}
            nb = jobs[0][0]()
            for ji in range(len(jobs)):
                cur = nb
                if ji + 1 < len(jobs):
                    nb = jobs[ji + 1][0]()
                jobs[ji][1](cur, st_)

        for slot in range(NSLOT):
            nblk = (2 * slot + 2) * 4
            n = nblk * 128
            P.dma('sp', iqc[:], iq_d[:, :, slot * 512:(slot + 1) * 512], writes=['iqc'], key='iqc')
            P.op('dve', lambda e, slot=slot: e.tensor_tensor(cref[:], offs[:, 8 * slot + 4, :], offs[:, 8 * slot, :], ALU.subtract),
                 reads=['offs'], writes=['cref'])
            P.op('dve', lambda e, slot=slot: e.scalar_tensor_tensor(cref[:], cref[:], jf[:, 0:1], offs[:, 8 * slot, :], ALU.mult, ALU.add),
                 reads=['cref', 'offs', 'jfs'], writes=['cref'])
            P.op('dve', lambda e: e.tensor_scalar(cref[:], cref[:], -1.0, None, ALU.mult), reads=['cref'], writes=['cref'])
            for qb in range(4):
                qi = slot * 4 + qb
                ai = nxt('adm', 2)
                P.dma('sp', adm[ai][:], adm_d[qb], writes=[('adm', ai)], key=('adm', ai))
                for sc in range(n // 512):
                    cs = slice(sc * 512, (sc + 1) * 512)
                    for h in range(4):
                        g = gbank()
                        hb = (h % 2) * 64
                        P.mm(ps[:, g, :], iqc[hb:hb + 64, h // 2, qb * 128:(qb + 1) * 128], ik2[hb:hb + 64, cs], True, True,
                             reads=['iqc', 'ik2s'], writes=[('ps', g)])
                        ri = nxt('rl', 3)
                        P.op('act', lambda e, g=g, ri=ri: e.activation(rl[ri][:], ps[:, g, :], AF.Relu), reads=[('ps', g)], writes=[('rl', ri)])
                        if h == 0:
                            P.op('dve', lambda e, ri=ri, cs=cs, qi=qi: e.tensor_scalar(Sc[:, cs], rl[ri][:], iw[:, qi, 0:1], None, ALU.mult),
                                 reads=[('rl', ri), 'iws'], writes=[('Sc', sc)])
                        else:
                            P.op('dve', lambda e, ri=ri, cs=cs, qi=qi, h=h: e.scalar_tensor_tensor(Sc[:, cs], rl[ri][:], iw[:, qi, h:h + 1], Sc[:, cs], ALU.mult, ALU.add),
                                 reads=[('rl', ri), 'iws', ('Sc', sc)], writes=[('Sc', sc)])
                scall = [('Sc', sc) for sc in range(n // 512)]
                P.op('dve', lambda e, n=n: e.tensor_reduce(sm[:, 0:1], Sc[:, 0:n], AX.X, ALU.max, apply_absolute_value=True),
                     reads=scall, writes=['sm0'])
                P.op('dve', lambda e: e.tensor_scalar(sm[:, 1:2], sm[:, 0:1], -1.0, -1.0, ALU.mult, ALU.add), reads=['sm0'], writes=['lo'])
                P.op('dve', lambda e: e.tensor_scalar(sm[:, 2:3], sm[:, 0:1], 2.0, 2.0, ALU.mult, ALU.add), reads=['sm0'], writes=['w0'])
                P.op('dve', lambda e: e.tensor_scalar(dk[:], pw[:], sm[:, 2:3], None, ALU.mult), reads=['w0', 'pws'], writes=['dk'])
                P.op('dve', lambda e, n=n, ai=ai: e.tensor_tensor(Sc[:, n - 1024:n], Sc[:, n - 1024:n], adm[ai][:], ALU.add),
                     reads=[('adm', ai)] + scall[-2:], writes=scall[-2:])
                for k in range(NBIS):
                    P.op('dve', lambda e, k=k: e.tensor_tensor(sm[:, 3:4], sm[:, 1:2], dk[:, k:k + 1], ALU.add), reads=['lo', 'dk'], writes=['mid'])
                    P.op('dve', lambda e, n=n: e.tensor_scalar(Mk[:, 0:n], Sc[:, 0:n], sm[:, 3:4], None, ALU.is_ge, ALU.add, accum_out=sm[:, 4:5]),
                         reads=scall + ['mid'], writes=['Mk', 'cnt'])
                    P.op('dve', lambda e, k=k: e.scalar_tensor_tensor(sm[:, 5:6], sm[:, 4:5], TOPK, dk[:, k:k + 1], ALU.is_ge, ALU.mult),
                         reads=['cnt', 'dk'], writes=['stp'])
                    P.op('dve', lambda e: e.tensor_tensor(sm[:, 1:2], sm[:, 1:2], sm[:, 5:6], ALU.add), reads=['lo', 'stp'], writes=['lo'])
                P.op('dve', lambda e, n=n: e.tensor_scalar(Mk[:, 0:n], Sc[:, 0:n], sm[:, 1:2], None, ALU.is_ge),
                     reads=scall + ['lo'], writes=['Mk'])
                pst = ps[:, 6, :].bitcast(BF16)
                for k0 in range(0, nblk, 4):
                    for j in range(4):
                        P.tr(pst[:, j * 128:(j + 1) * 128], Mk[:, (k0 + j) * 128:(k0 + j + 1) * 128], identb[:],
                             reads=['Mk', 'identbs'], writes=[('ps', 6)])
                    P.op('act', lambda e, k0=k0, qb=qb: e.copy(MT[:, k0:k0 + 4, qb * 128:(qb + 1) * 128],
                                                              pst[:, 0:512].rearrange("p (j t) -> p j t", j=4)),
                         reads=[('ps', 6)], writes=['MT'])
            run_jobs(attention(slot, 'a') + attention(slot, 'b'))

    return dict(oaT=oa_d, obT=ob_d)


def _slot_cols(j):
    return np.concatenate([np.arange((2 * i + j) * 512, (2 * i + j + 1) * 512) for i in range(NSLOT)])


def k2a_consts(j):
    p = np.arange(128)
    col = np.arange(1024)
    adm = np.zeros((4, 128, 1024), np.float32)
    for qb in range(4):
        lim = j * 512 + qb * 128 + (p // 64) * 64 + 64
        adm[qb] = np.where(col[None, :] < lim[:, None], 0.0, NEG)
    kbl = np.arange(8)
    q = np.arange(512)
    cm = ((kbl[None, :, None] * 128 + p[:, None, None]) <= (j * 512 + q[None, None, :])).astype(np.float32).astype(NPBF)
    tri = (p[:, None] <= p[None, :]).astype(np.float32)
    pw = np.tile((2.0 ** -(np.arange(NBIS, dtype=np.float32) + 1))[None, :], (128, 1)).astype(np.float32)
    return dict(adm=adm, cm=np.ascontiguousarray(cm), jflag=np.full((128, 1), float(j), np.float32), tri=tri,
                identb=np.eye(128, dtype=np.float32).astype(NPBF), pw2=pw)


def _vaug(v_full):
    S = v_full.shape[0]
    va = np.ones((S, 8, 65), dtype=v_full.dtype)
    va[:, :, :64] = v_full.reshape(S, 8, 64)
    return np.ascontiguousarray(va.reshape(S // 128, 128, 4, 130).transpose(2, 1, 0, 3))


def k2a_inputs(j, aqT, akT, av, fqT, fkT, fv, iqT, ikT, small):
    cols = _slot_cols(j)
    S = SEQ
    d = dict(
        aq=np.ascontiguousarray(aqT.reshape(4, 128, S)[:, :, cols]),
        ak=np.ascontiguousarray(akT.reshape(4, 128, S)),
        av=_vaug(av),
        fq=np.ascontiguousarray(fqT.reshape(4, 128, S)[:, :, cols]),
        fk=np.ascontiguousarray(fkT.reshape(4, 128, S)),
        fv=_vaug(fv),
        iq=np.ascontiguousarray(iqT.reshape(2, 128, S)[:, :, cols].transpose(1, 0, 2)),
        ik2=np.ascontiguousarray(np.concatenate([ikT, ikT], axis=0)),
        iw=np.ascontiguousarray(small[cols, 0:4].reshape(NSLOT * 4, 128, 4).transpose(1, 0, 2)),
        lf=np.ascontiguousarray(small[:, 4:12].reshape(64, 128, 8).transpose(1, 0, 2)),
    )
    d.update(k2a_consts(j))
    return d


def build_k2b():
    nc = bass.Bass("TRN2", target_bir_lowering=False)
    P = Prog(nc)
    phase_k2b(nc, P, '')
    P.emit(final_keys=['out'])
    return nc, P


def phase_k2b(nc, P, px, ext=None):
    ext = ext or {}
    S = SEQ
    NCH = S // 64
    GRP = 16
    din = lambda name, shape, dt: ext[name] if name in ext else nc.dram_tensor(px + name, shape, dt, kind="ExternalInput").ap()
    qp_d = din("qpT", [64, 4, S], BF16)
    kpT_d = din("kpT", [64, 4, S], BF16)
    kp_d = din("kp", [64, NCH, 256], BF16)
    v_d = din("v", [64, NCH, 256], BF16)
    em_d = din("em", [64, 4, NCH], F32)
    g_d = din("g", [64, 4, NCH], F32)
    triu_d = din("triu", [64, 64], F32)
    oc_d = nc.dram_tensor(px + "ocT", [64, 4, S], BF16, kind="ExternalOutput").ap()

    from contextlib import ExitStack
    with ExitStack() as st:
        def sb(name, shape, dt):
            return st.enter_context(nc.sbuf_tensor(px + name, shape, dt))
        qp = [sb("qp%d" % i, [64, 4, GRP * 64], BF16) for i in range(2)]
        kpT = [sb("kpT%d" % i, [64, 4, GRP * 64], BF16) for i in range(2)]
        kp = [sb("kp%d" % i, [64, GRP, 256], BF16) for i in range(2)]
        vv = [sb("vv%d" % i, [64, GRP, 256], BF16) for i in range(2)]
        ocb = [sb("ocb%d" % i, [64, 4, GRP * 64], BF16) for i in range(2)]
        em = sb("ems", [64, 4, NCH], F32)
        gg = sb("ggs", [64, 4, NCH], F32)
        triu = sb("trius", [64, 64], F32)
        Sm = sb("Sm", [64, 4, 64], F32)
        Smb = sb("Smb", [64, 4, 64], BF16)
        tmp = sb("tmp", [64, 4, 64], F32)
        scm = [sb("scm%d" % i, [64, 4, 64], BF16) for i in range(2)]
        ps = st.enter_context(nc.psum_tensor(px + "ps", [128, 8, 512], F32))
        cnt = {}

        def nxt(k, n):
            v = cnt.get(k, 0) % n
            cnt[k] = cnt.get(k, 0) + 1
            return v

        P.dma('sp', em[:], em_d, writes=['em'], key='c0')
        P.dma('sp', gg[:], g_d, writes=['gg'], key='c1')
        P.dma('sp', triu[:], triu_d, writes=['triu'], key='c2')
        P.op('dve', lambda e: e.tensor_tensor(gg[:, :, 0:NCH - 1], gg[:, :, 0:NCH - 1], em[:, :, 1:NCH], ALU.mult), reads=['em', 'gg'], writes=['gg'])
        P.op('dve', lambda e: e.memset(Sm[:], 0.0), writes=['Sm'])
        P.op('dve', lambda e: e.memset(Smb[:], 0.0), writes=['Smb'])

        ngrp = NCH // GRP

        def load(gi):
            b = gi % 2
            ts_ = slice(gi * GRP * 64, (gi + 1) * GRP * 64)
            cs_ = slice(gi * GRP, (gi + 1) * GRP)
            P.dma('sp', qp[b][:], qp_d[:, :, ts_], writes=[('qp', b)], key=('qp', b))
            P.dma('sp', kpT[b][:], kpT_d[:, :, ts_], writes=[('kpT', b)], key=('kpT', b))
            P.dma('sp', kp[b][:], kp_d[:, cs_, :], writes=[('kp', b)], key=('kp', b))
            P.dma('sp', vv[b][:], v_d[:, cs_, :], writes=[('vv', b)], key=('vv', b))

        load(0)
        for gi in range(ngrp):
            if gi + 1 < ngrp:
                load(gi + 1)
            b = gi % 2
            for cl in range(GRP):
                c = gi * GRP + cl
                tsl = slice(cl * 64, (cl + 1) * 64)
                gs = nxt('sc', 2)
                for hl in range(4):
                    P.mm(ps[0:64, gs, hl * 64:(hl + 1) * 64], kpT[b][:, hl, tsl], qp[b][:, hl, tsl], True, True,
                         reads=[('kpT', b), ('qp', b)], writes=[('ps', gs)])
                si = nxt('scm', 2)
                P.op('dve', lambda e, gs=gs, si=si: e.tensor_tensor(scm[si][:], ps[0:64, gs, 0:256].rearrange("p (h t) -> p h t", h=4),
                                                                  triu[:].unsqueeze(1).to_broadcast([64, 4, 64]), ALU.mult),
                     reads=[('ps', gs), 'triu'], writes=[('scm', si)])
                go = 2 + nxt('o', 2)
                for hl in range(4):
                    P.mm(ps[0:64, go, hl * 64:(hl + 1) * 64], vv[b][:, cl, hl * 64:(hl + 1) * 64], scm[si][:, hl, :], True, False,
                         reads=[('vv', b), ('scm', si)], writes=[('ps', go)])
                    P.mm(ps[0:64, go, hl * 64:(hl + 1) * 64], Smb[:, hl, :], qp[b][:, hl, tsl], False, True,
                         reads=['Smb', ('qp', b)], writes=[('ps', go)])
                P.op('act', lambda e, go=go, b=b, tsl=tsl: e.copy(ocb[b][:, :, tsl], ps[0:64, go, 0:256].rearrange("p (h t) -> p h t", h=4)),
                     reads=[('ps', go)], writes=[('ocb', b)])
                gk = 4 + nxt('kv', 2)
                for hl in range(4):
                    P.mm(ps[0:64, gk, hl * 64:(hl + 1) * 64], kp[b][:, cl, hl * 64:(hl + 1) * 64], vv[b][:, cl, hl * 64:(hl + 1) * 64], True, True,
                         reads=[('kp', b), ('vv', b)], writes=[('ps', gk)])
                if c < NCH - 1:
                    P.op('dve', lambda e, gk=gk: e.tensor_tensor(tmp[:], ps[0:64, gk, 0:256].rearrange("p (h t) -> p h t", h=4), Sm[:], ALU.add),
                         reads=[('ps', gk), 'Sm'], writes=['tmp'])
                    P.op('dve', lambda e, c=c: e.tensor_tensor(Sm[:], tmp[:], gg[:, :, c:c + 1].to_broadcast([64, 4, 64]), ALU.mult),
                         reads=['tmp', 'gg'], writes=['Sm'])
                    P.op('dve', lambda e, c=c: e.tensor_tensor(Smb[:], tmp[:], gg[:, :, c:c + 1].to_broadcast([64, 4, 64]), ALU.mult),
                         reads=['tmp', 'gg'], writes=['Smb'])
            P.dma('sp', oc_d[:, :, gi * GRP * 64:(gi + 1) * GRP * 64], ocb[b][:], reads=[('ocb', b)], key='out')
    return dict(ocT=oc_d)


def k2b_inputs(j, qpT, kpT, kp, cv, em, g):
    S = SEQ
    hs = slice(4 * j, 4 * j + 4)
    cs = slice(256 * j, 256 * j + 256)
    return dict(
        qpT=np.ascontiguousarray(qpT.reshape(8, 64, S)[hs].transpose(1, 0, 2)),
        kpT=np.ascontiguousarray(kpT.reshape(8, 64, S)[hs].transpose(1, 0, 2)),
        kp=np.ascontiguousarray(kp[:, cs].reshape(S // 64, 64, 256).transpose(1, 0, 2)),
        v=np.ascontiguousarray(cv[:, cs].reshape(S // 64, 64, 256).transpose(1, 0, 2)),
        em=np.ascontiguousarray(em.reshape(8, 64, S // 64)[hs].transpose(1, 0, 2)).astype(np.float32),
        g=np.ascontiguousarray(g.reshape(8, 64, S // 64)[hs].transpose(1, 0, 2)).astype(np.float32),
        triu=np.triu(np.ones((64, 64), np.float32)),
    )


def emit_layernorm(P, y, out, lng, lnb, stats, mv, rstd, ykey, okey, gkeys, tag):
    for c in range(2):
        P.op('dve', lambda e, c=c: e.bn_stats(stats[:, c, :], y[:, c * 512:(c + 1) * 512]), reads=[ykey], writes=[(tag, 'st', c)])
    P.op('dve', lambda e: e.bn_aggr(mv[:], stats[:].rearrange("p c s -> p (c s)")), reads=[(tag, 'st', 0), (tag, 'st', 1)], writes=[(tag, 'mv')])
    P.op('act', lambda e: e.activation(rstd[:], mv[:, 1:2], AF.Sqrt, bias=1e-5), reads=[(tag, 'mv')], writes=[(tag, 'rs')])
    P.op('dve', lambda e: e.reciprocal(rstd[:], rstd[:]), reads=[(tag, 'rs')], writes=[(tag, 'rs')])
    P.op('dve', lambda e: e.tensor_scalar(out, y, mv[:, 0:1], rstd[:, 0:1], ALU.subtract, ALU.mult), reads=[ykey, (tag, 'mv'), (tag, 'rs')], writes=[okey])
    P.op('pool', lambda e: e.tensor_tensor(out, out, lng, ALU.mult), reads=[okey, gkeys[0]], writes=[okey])
    P.op('pool', lambda e: e.tensor_tensor(out, out, lnb, ALU.add), reads=[okey, gkeys[1]], writes=[okey])


def build_k3a():
    nc = bass.Bass("TRN2", target_bir_lowering=False)
    P = Prog(nc)
    phase_k3a(nc, P, '')
    P.emit(final_keys=['out'])
    return nc, P


def phase_k3a(nc, P, px, ext=None):
    ext = ext or {}
    T = TOK
    din = lambda name, shape, dt: ext[name] if name in ext else nc.dram_tensor(px + name, shape, dt, kind="ExternalInput").ap()
    oa_d = din("oaT", [64, 8, T], BF16)
    ob_d = din("obT", [64, 8, T], BF16)
    oc_d = din("ocT", [64, 8, T], BF16)
    cg_d = din("cgT", [64, 8, T], BF16)
    gt_d = din("gT", [128, 24, T], BF16)
    x_d = din("x", [T, D_MODEL], F32)
    wp_d = [din("wp%d" % i, [64, 8, D_MODEL], F32) for i in range(3)]
    wo_d = din("wo", [128, 8, D_MODEL], F32)
    gn_d = din("gn", [64, 8], F32)
    lng_d = din("lng", [1, D_MODEL], F32)
    lnb_d = din("lnb", [1, D_MODEL], F32)
    h_d = nc.dram_tensor(px + "h", [T, D_MODEL], F32, kind="ExternalOutput").ap()

    from contextlib import ExitStack
    with ExitStack() as st:
        def sb(name, shape, dt):
            return st.enter_context(nc.sbuf_tensor(px + name, shape, dt))
        wst = sb("wst", [128, 8, 512], F32)
        wp = [sb("wpb%d" % i, [64, 8, D_MODEL], BF16) for i in range(3)]
        wo = sb("wob", [128, 8, D_MODEL], BF16)
        gn = sb("gns", [64, 8], F32)
        lng = sb("lngs", [128, D_MODEL], F32)
        lnb = sb("lnbs", [128, D_MODEL], F32)
        on64 = sb("on64", [64, 64], BF16)
        oT = [[sb("oT%d_%d" % (br, i), [64, 8, 512], BF16) for i in range(1)] for br in range(3)]
        cg = [sb("cg%d" % i, [64, 8, 512], BF16) for i in range(1)]
        gt = [sb("gt%d" % i, [128, 3, 512], BF16) for i in range(2)]
        mT = sb("mT", [128, 8, 512], BF16)
        sq = [sb("sq%d" % i, [64, 512], BF16) for i in range(2)]
        rs = [sb("rs%d" % i, [64, 512], F32) for i in range(2)]
        tA = [sb("tA%d" % i, [128, 512], F32) for i in range(2)]
        tB = [sb("tB%d" % i, [128, 512], F32) for i in range(2)]
        tC = [sb("tC%d" % i, [128, 512], F32) for i in range(2)]
        xin = [sb("xin%d" % i, [128, D_MODEL], F32) for i in range(2)]
        yb = [sb("yb%d" % i, [128, D_MODEL], F32) for i in range(2)]
        hb = [sb("hb%d" % i, [128, D_MODEL], F32) for i in range(2)]
        stats = sb("stats", [128, 2, 6], F32)
        mv = sb("mv", [128, 2], F32)
        rstd = sb("rstd", [128, 1], F32)
        ps = st.enter_context(nc.psum_tensor(px + "ps", [128, 8, 512], F32))
        cnt = {}

        def nxt(k, n):
            v = cnt.get(k, 0) % n
            cnt[k] = cnt.get(k, 0) + 1
            return v

        for i in range(3):
            for hf in range(2):
                P.dma('sp', wst[0:64], wp_d[i][:, :, hf * 512:(hf + 1) * 512], writes=['wst'], key='wst')
                P.op('act', lambda e, i=i, hf=hf: e.copy(wp[i][:, :, hf * 512:(hf + 1) * 512], wst[0:64]), reads=['wst'], writes=[('wp', i)])
        for hf in range(2):
            P.dma('sp', wst[:], wo_d[:, :, hf * 512:(hf + 1) * 512], writes=['wst'], key='wst')
            P.op('act', lambda e, hf=hf: e.copy(wo[:, :, hf * 512:(hf + 1) * 512], wst[:]), reads=['wst'], writes=['wo'])
        P.dma('sp', gn[:], gn_d, writes=['gn'], key='c0')
        P.dma('sp', lng[:], lng_d.partition_broadcast(128), writes=['lng'], key='c1')
        P.dma('sp', lnb[:], lnb_d.partition_broadcast(128), writes=['lnb'], key='c2')
        P.op('pool', lambda e: e.memset(on64[:], 1.0 / 64.0), writes=['on64'])

        NTC = T // 512

        def load(tc):
            b = 0
            sl = slice(tc * 512, (tc + 1) * 512)
            for br, d in enumerate((oa_d, ob_d, oc_d)):
                P.dma('sp', oT[br][b][:], d[:, :, sl], writes=[('oT', br, b)], key=('oT', br, b))
            P.dma('sp', cg[b][:], cg_d[:, :, sl], writes=[('cg', b)], key=('cg', b))

        def load_gt(tc, mc):
            gi = nxt('gt', 2)
            sl = slice(tc * 512, (tc + 1) * 512)
            for br in range(3):
                P.dma('sp', gt[gi][:, br, :], gt_d[:, br * 8 + mc, sl], writes=[('gt', gi)], key=('gt', gi))
            return gi

        for tc in range(NTC):
            load(tc)
            b = 0
            oc = oT[2][b]
            for h in range(8):
                si = nxt('sq', 2)
                P.op('dve', lambda e, h=h, si=si: e.tensor_tensor(sq[si][:], oc[:, h, :], oc[:, h, :], ALU.mult), reads=[('oT', 2, b)], writes=[('sq', si)])
                g = nxt('g', 4)
                P.mm(ps[0:64, g, :], on64[:], sq[si][:], True, True, reads=['on64', ('sq', si)], writes=[('ps', g)])
                ri = nxt('rs', 2)
                P.op('act', lambda e, g=g, ri=ri: e.activation(rs[ri][:], ps[0:64, g, :], AF.Sqrt, bias=1e-6), reads=[('ps', g)], writes=[('rs', ri)])
                P.op('dve', lambda e, ri=ri: e.reciprocal(rs[ri][:], rs[ri][:]), reads=[('rs', ri)], writes=[('rs', ri)])
                P.op('dve', lambda e, h=h, ri=ri: e.scalar_tensor_tensor(rs[ri][:], oc[:, h, :], gn[:, h:h + 1], rs[ri][:], ALU.mult, ALU.mult),
                     reads=[('oT', 2, b), 'gn', ('rs', ri)], writes=[('rs', ri)])
                P.op('pool', lambda e, h=h, ri=ri: e.tensor_tensor(oc[:, h, :], rs[ri][:], cg[b][:, h, :], ALU.mult),
                     reads=[('rs', ri), ('cg', b)], writes=[('oT', 2, b)])
            gnext = load_gt(tc, 0)
            for mc in range(8):
                gcur = gnext
                if mc + 1 < 8:
                    gnext = load_gt(tc, mc + 1)
                gb = []
                for br in range(3):
                    g = nxt('g', 4)
                    for h in range(8):
                        P.mm(ps[:, g, :], wp[br][:, h, mc * 128:(mc + 1) * 128], oT[br][b][:, h, :], h == 0, h == 7,
                             reads=[('wp', br), ('oT', br, b)], writes=[('ps', g)])
                    gb.append(g)
                ti = nxt('t', 2)
                for br, tt in enumerate((tA, tB, tC)):
                    P.op('dve', lambda e, br=br, tt=tt, ti=ti, g=gb[br], gcur=gcur: e.tensor_tensor(tt[ti][:], ps[:, g, :], gt[gcur][:, br, :], ALU.mult),
                         reads=[('ps', gb[br]), ('gt', gcur)], writes=[('t', br, ti)])
                P.op('pool', lambda e, ti=ti: e.tensor_tensor(tA[ti][:], tA[ti][:], tB[ti][:], ALU.add), reads=[('t', 0, ti), ('t', 1, ti)], writes=[('t', 0, ti)])
                P.op('pool', lambda e, ti=ti, mc=mc: e.tensor_tensor(mT[:, mc, :], tA[ti][:], tC[ti][:], ALU.add), reads=[('t', 0, ti), ('t', 2, ti)], writes=[('mT', mc)])
            for tt in range(4):
                tix = tc * 4 + tt
                xi = nxt('x', 2)
                P.dma('sp', xin[xi][:], x_d[tix * 128:(tix + 1) * 128, :], writes=[('xin', xi)], key=('xin', xi))
                yi = nxt('y', 2)
                for half in range(2):
                    g = 4 + nxt('go', 4)
                    for k in range(8):
                        P.mm(ps[:, g, :], mT[:, k, tt * 128:(tt + 1) * 128], wo[:, k, half * 512:(half + 1) * 512], k == 0, k == 7,
                             reads=[('mT', k), 'wo'], writes=[('ps', g)])
                    P.op('dve', lambda e, g=g, xi=xi, yi=yi, half=half: e.scalar_tensor_tensor(
                        yb[yi][:, half * 512:(half + 1) * 512], xin[xi][:, half * 512:(half + 1) * 512], ALPHA, ps[:, g, :], ALU.mult, ALU.add),
                        reads=[('xin', xi), ('ps', g)], writes=[('yb', yi)])
                hi = nxt('h', 2)
                emit_layernorm(P, yb[yi][:], hb[hi][:], lng[:], lnb[:], stats, mv, rstd, ('yb', yi), ('hb', hi), ('lng', 'lnb'), 'ln')
                P.dma('sp', h_d[tix * 128:(tix + 1) * 128, :], hb[hi][:], reads=[('hb', hi)], key='out')
    return dict(h=h_d)


def _fm64(a_T):
    return np.ascontiguousarray(a_T.reshape(8, 64, -1).transpose(1, 0, 2))


def k3a_inputs(oaT, obT, ocT, cgT, gT, x, wpa, wpb, wpc, wo, gn, lng, lnb):
    T = TOK
    f = lambda w: np.ascontiguousarray(w.reshape(8, 64, D_MODEL).transpose(1, 0, 2)).astype(np.float32)
    return dict(oaT=oaT, obT=obT, ocT=ocT, cgT=_fm64(cgT),
                gT=np.ascontiguousarray(gT.reshape(24, 128, T).transpose(1, 0, 2)),
                x=np.ascontiguousarray(x), wp0=f(wpa), wp1=f(wpb), wp2=f(wpc),
                wo=np.ascontiguousarray(wo.reshape(8, 128, D_MODEL).transpose(1, 0, 2)).astype(np.float32),
                gn=np.ascontiguousarray(gn.reshape(8, 64).T).astype(np.float32),
                lng=np.ascontiguousarray(lng.reshape(1, -1)).astype(np.float32),
                lnb=np.ascontiguousarray(lnb.reshape(1, -1)).astype(np.float32))


def build_k3b(e0=0, ne=N_EXP, first=True, last=True):
    nc = bass.Bass("TRN2", target_bir_lowering=False)
    P = Prog(nc)
    phase_k3b(nc, P, '', e0, ne, first, last)
    P.emit(final_keys=['out'])
    return nc, P


def phase_k3b(nc, P, px, e0=0, ne=N_EXP, first=True, last=True, ext=None):
    ext = ext or {}
    T = TOK
    QC = 1024
    NQ = T // QC
    din = lambda name, shape, dt: ext[name] if name in ext else nc.dram_tensor(px + name, shape, dt, kind="ExternalInput").ap()
    h_d = din("h", [T, D_MODEL], F32)
    wr_d = din("wr", [128, 8, N_EXP], F32)
    br_d = din("br", [1, N_EXP], F32)
    wgu_d = din("wgu", [ne, D_MODEL, 2 * D_MODEL], F32)
    bgu_d = din("bgu", [128, N_EXP, 16], F32)
    wd_d = din("wd", [ne, D_MODEL, D_MODEL], F32)
    accin_d = None if first else din("acc_in", [T, D_MODEL], F32)
    bd_d = din("bd", [N_EXP, D_MODEL], F32)
    lng_d = din("lng", [1, D_MODEL], F32)
    lnb_d = din("lnb", [1, D_MODEL], F32)
    ident_d = din("ident", [128, 128], F32)
    out_d = nc.dram_tensor(px + ("out" if last else "acc_out"), [T, D_MODEL], F32, kind="ExternalOutput").ap()

    from contextlib import ExitStack
    with ExitStack() as st:
        def sb(name, shape, dt):
            return st.enter_context(nc.sbuf_tensor(px + name, shape, dt))
        hTc = sb("hTc", [128, 8, QC], BF16)
        acc = sb("acc", [128, QC // 128, D_MODEL], F32)
        actT = sb("actT", [128, 8, QC], BF16)
        gst = [sb("gst%d" % i, [128, 8, 512], F32) for i in range(2)]
        gbf = [sb("gbf%d" % i, [128, 8, 512], BF16) for i in range(2)]
        dst = gst
        dbf = [sb("dbf%d" % i, [128, 8, D_MODEL], BF16) for i in range(2)]
        big = [sb("big%d" % i, [128, D_MODEL], F32) for i in range(4)]
        hT32 = sb("hT32", [128, 8, 128], F32)
        wr = sb("wrs", [128, 8, N_EXP], F32)
        brb = sb("brb", [128, N_EXP], F32)
        bgu = sb("bgus", [128, N_EXP, 16], F32)
        bd = sb("bds", [N_EXP, D_MODEL], F32)
        lng = sb("lngs", [128, D_MODEL], F32)
        lnb = sb("lnbs", [128, D_MODEL], F32)
        ident = sb("idents", [128, 128], F32)
        Gc = sb("Gc", [128, QC // 128, N_EXP], F32)
        GTc = sb("GTc", [N_EXP, QC], F32)
        lg = sb("lg", [128, N_EXP], F32)
        ex = sb("ex", [128, N_EXP], F32)
        m8 = sb("m8", [128, 8], F32)
        sm = sb("sm", [128, 4], F32)
        tG = [sb("tG%d" % i, [128, 512], F32) for i in range(2)]
        tS = [sb("tS%d" % i, [128, 512], F32) for i in range(2)]
        tU = [sb("tU%d" % i, [128, 512], F32) for i in range(2)]
        stats = sb("stats", [128, 2, 6], F32)
        mv = sb("mv", [128, 2], F32)
        rstd = sb("rstd", [128, 1], F32)
        ps = st.enter_context(nc.psum_tensor(px + "ps", [128, 8, 512], F32))
        cnt = {}

        def nxt(k, n):
            v = cnt.get(k, 0) % n
            cnt[k] = cnt.get(k, 0) + 1
            return v

        for i, (t, d, kn) in enumerate([(wr, wr_d, 'wrs'), (bgu, bgu_d, 'bgus'), (bd, bd_d, 'bds'), (ident, ident_d, 'idents')]):
            P.dma('sp', t[:], d, writes=[kn], key='c%d' % i)
        P.dma('sp', brb[:], br_d.partition_broadcast(128), writes=['brb'], key='c4')
        P.dma('sp', lng[:], lng_d.partition_broadcast(128), writes=['lng'], key='c5')
        P.dma('sp', lnb[:], lnb_d.partition_broadcast(128), writes=['lnb'], key='c6')

        wgu_v = wgu_d.rearrange("e (c p) n -> e p c n", p=128)
        wd_v = wd_d.rearrange("e (c p) n -> e p c n", p=128)

        def gu_unit(q, e, f2):
            def load():
                b = nxt('gw', 2)
                s_ = nxt('ds', 2)
                P.dma('sp', gst[s_][:, :, 0:256], wgu_v[e - e0, :, :, f2 * 256:(f2 + 1) * 256], writes=[('gst', s_)], key=('gst', s_))
                P.dma('sp', gst[s_][:, :, 256:512], wgu_v[e - e0, :, :, 1024 + f2 * 256:1024 + (f2 + 1) * 256], writes=[('gst', s_)], key=('gst', s_))
                P.op('act', lambda e_: e_.copy(gbf[b][:], gst[s_][:]), reads=[('gst', s_)], writes=[('gbf', b)])
                return b

            def comp(b):
                for fl in range(2):
                    fc = f2 * 2 + fl
                    for tcc in range(QC // 512):
                        tsl = slice(tcc * 512, (tcc + 1) * 512)
                        g1 = nxt('g', 4)
                        for k in range(8):
                            P.mm(ps[:, g1, :], gbf[b][:, k, fl * 128:(fl + 1) * 128], hTc[:, k, tsl], k == 0, k == 7,
                                 reads=[('gbf', b), 'hTc'], writes=[('ps', g1)])
                        g2 = nxt('g', 4)
                        for k in range(8):
                            P.mm(ps[:, g2, :], gbf[b][:, k, 256 + fl * 128:256 + (fl + 1) * 128], hTc[:, k, tsl], k == 0, k == 7,
                                 reads=[('gbf', b), 'hTc'], writes=[('ps', g2)])
                        ti = nxt('t', 2)
                        P.op('dve', lambda e_, g1=g1, ti=ti, fc=fc: e_.tensor_scalar(tG[ti][:], ps[:, g1, :], bgu[:, e, fc:fc + 1], 7.0, ALU.add, ALU.min),
                             reads=[('ps', g1), 'bgus'], writes=[('tG', ti)])
                        P.op('act', lambda e_, ti=ti: e_.activation(tS[ti][:], tG[ti][:], AF.Sigmoid, scale=1.702), reads=[('tG', ti)], writes=[('tS', ti)])
                        P.op('dve', lambda e_, g2=g2, ti=ti, fc=fc: e_.tensor_scalar(tU[ti][:], ps[:, g2, :], bgu[:, e, 8 + fc:9 + fc], 7.0, ALU.add, ALU.min),
                             reads=[('ps', g2), 'bgus'], writes=[('tU', ti)])
                        P.op('dve', lambda e_, ti=ti: e_.tensor_scalar(tU[ti][:], tU[ti][:], -7.0, 1.0, ALU.max, ALU.add), reads=[('tU', ti)], writes=[('tU', ti)])
                        P.op('pool', lambda e_, ti=ti: e_.tensor_tensor(tG[ti][:], tG[ti][:], tS[ti][:], ALU.mult), reads=[('tG', ti), ('tS', ti)], writes=[('tG', ti)])
                        P.op('pool', lambda e_, ti=ti, fc=fc, tsl=tsl: e_.tensor_tensor(actT[:, fc, tsl], tG[ti][:], tU[ti][:], ALU.mult),
                             reads=[('tG', ti), ('tU', ti)], writes=[('actT', fc)])
            return load, comp

        def down_unit(q, e):
            def load():
                b = nxt('dw', 2)
                for half in range(2):
                    s_ = nxt('ds', 2)
                    P.dma('sp', dst[s_][:], wd_v[e - e0, :, :, half * 512:(half + 1) * 512], writes=[('gst', s_)], key=('gst', s_))
                    P.op('act', lambda e_, s_=s_, half=half: e_.copy(dbf[b][:, :, half * 512:(half + 1) * 512], dst[s_][:]), reads=[('gst', s_)], writes=[('dbf', b)])
                return b

            def comp(b):
                for tt in range(QC // 128):
                    for half in range(2):
                        g = 4 + nxt('gd', 4)
                        for fc in range(8):
                            P.mm(ps[:, g, :], actT[:, fc, tt * 128:(tt + 1) * 128], dbf[b][:, fc, half * 512:(half + 1) * 512], fc == 0, fc == 7,
                                 reads=[('actT', fc), ('dbf', b)], writes=[('ps', g)])
                        P.op('dve', lambda e_, g=g, tt=tt, half=half: e_.scalar_tensor_tensor(
                            acc[:, tt, half * 512:(half + 1) * 512], ps[:, g, :], Gc[:, tt, e:e + 1], acc[:, tt, half * 512:(half + 1) * 512], ALU.mult, ALU.add),
                            reads=[('ps', g), 'Gc', ('acc', tt)], writes=[('acc', tt)])
            return load, comp

        units = []
        for q in range(NQ):
            for e in range(e0, e0 + ne):
                for f2 in range(4):
                    units.append(('gu', q, e, f2) + gu_unit(q, e, f2))
                units.append(('dn', q, e, None) + down_unit(q, e))

        def prologue(q):
            for tt in range(QC // 128):
                tix = q * (QC // 128) + tt
                bi = nxt('big', 4)
                P.dma('sp', big[bi][:], h_d[tix * 128:(tix + 1) * 128, :], writes=[('big', bi)], key=('big', bi))
                for half in range(2):
                    g = nxt('g', 4)
                    for j in range(4):
                        k = half * 4 + j
                        P.tr(ps[:, g, j * 128:(j + 1) * 128], big[bi][:, k * 128:(k + 1) * 128], ident[:], reads=[('big', bi), 'idents'], writes=[('ps', g)])
                    P.op('act', lambda e_, g=g, half=half: e_.copy(hT32[:, half * 4:(half + 1) * 4, :], ps[:, g, :].rearrange("p (j t) -> p j t", j=4)),
                         reads=[('ps', g)], writes=[('hT32', half)])
                    P.op('dve', lambda e_, half=half, tt=tt: e_.tensor_copy(hTc[:, half * 4:(half + 1) * 4, tt * 128:(tt + 1) * 128],
                                                                          hT32[:, half * 4:(half + 1) * 4, :]),
                         reads=[('hT32', half)], writes=['hTc'])
                g = nxt('g', 4)
                for k in range(8):
                    P.mm(ps[:, g, 0:N_EXP], hT32[:, k, :], wr[:, k, :], k == 0, k == 7, reads=[('hT32', k // 4), 'wrs'], writes=[('ps', g)])
                P.op('dve', lambda e_, g=g: e_.tensor_tensor(lg[:], ps[:, g, 0:N_EXP], brb[:], ALU.add), reads=[('ps', g), 'brb'], writes=['lg'])
                P.op('dve', lambda e_: e_.max(m8[:], lg[:]), reads=['lg'], writes=['m8'])
                P.op('dve', lambda e_: e_.tensor_scalar(sm[:, 0:1], m8[:, 0:1], -1.0, None, ALU.mult), reads=['m8'], writes=['sm0'])
                P.op('act', lambda e_: e_.activation(ex[:], lg[:], AF.Exp, bias=sm[:, 0:1]), reads=['lg', 'sm0'], writes=['ex'])
                P.op('dve', lambda e_: e_.scalar_tensor_tensor(ex[:], lg[:], m8[:, 3:4], ex[:], ALU.is_ge, ALU.mult), reads=['lg', 'm8', 'ex'], writes=['ex'])
                P.op('dve', lambda e_: e_.reduce_sum(sm[:, 1:2], ex[:], AX.X), reads=['ex'], writes=['sm1'])
                P.op('dve', lambda e_: e_.reciprocal(sm[:, 1:2], sm[:, 1:2]), reads=['sm1'], writes=['sm1'])
                P.op('dve', lambda e_, tt=tt: e_.tensor_scalar(Gc[:, tt, :], ex[:], sm[:, 1:2], None, ALU.mult), reads=['ex', 'sm1'], writes=['Gc'])
                g = nxt('g', 4)
                P.tr(ps[0:N_EXP, g, 0:128], Gc[:, tt, :], ident[:], reads=['Gc', 'idents'], writes=[('ps', g)])
                P.op('act', lambda e_, g=g, tt=tt: e_.copy(GTc[:, tt * 128:(tt + 1) * 128], ps[0:N_EXP, g, 0:128]), reads=[('ps', g)], writes=['GTc'])
                if first:
                    for half in range(2):
                        g = 4 + nxt('gd', 4)
                        P.mm(ps[:, g, :], GTc[:, tt * 128:(tt + 1) * 128], bd[:, half * 512:(half + 1) * 512], True, True, reads=['GTc', 'bds'], writes=[('ps', g)])
                        P.op('act', lambda e_, g=g, tt=tt, half=half: e_.copy(acc[:, tt, half * 512:(half + 1) * 512], ps[:, g, :]), reads=[('ps', g)], writes=[('acc', tt)])
                else:
                    P.dma('sp', acc[:, tt, :], accin_d[tix * 128:(tix + 1) * 128, :], writes=[('acc', tt)], key=('accin', tt))

        def epilogue(q):
            for tt in range(QC // 128):
                tix = q * (QC // 128) + tt
                if not last:
                    P.dma('sp', out_d[tix * 128:(tix + 1) * 128, :], acc[:, tt, :], reads=[('acc', tt)], key='out')
                    continue
                bi = nxt('big', 4)
                P.dma('sp', big[bi][:], h_d[tix * 128:(tix + 1) * 128, :], writes=[('big', bi)], key=('big', bi))
                yi = nxt('big', 4)
                P.op('dve', lambda e_, bi=bi, yi=yi, tt=tt: e_.scalar_tensor_tensor(big[yi][:], big[bi][:], ALPHA, acc[:, tt, :], ALU.mult, ALU.add),
                     reads=[('big', bi), ('acc', tt)], writes=[('big', yi)])
                oi = nxt('big', 4)
                emit_layernorm(P, big[yi][:], big[oi][:], lng[:], lnb[:], stats, mv, rstd, ('big', yi), ('big', oi), ('lng', 'lnb'), 'ln')
                P.dma('sp', out_d[tix * 128:(tix + 1) * 128, :], big[oi][:], reads=[('big', oi)], key='out')

        nb = units[0][4]()
        for ui in range(len(units)):
            kind, q, e, f2, load, comp = units[ui]
            cur = nb
            if kind == 'gu' and e == e0 and f2 == 0:
                prologue(q)
            if ui + 1 < len(units):
                nb = units[ui + 1][4]()
            comp(cur)
            if kind == 'dn' and e == e0 + ne - 1:
                epilogue(q)
    return dict(out=out_d)


def k3b_inputs(h, wr, br, wgu, bgu, wd, bd, lng, lnb, acc_in=None):
    d = _k3b_inputs(h, wr, br, wgu, bgu, wd, bd, lng, lnb)
    if acc_in is not None:
        d['acc_in'] = np.ascontiguousarray(acc_in)
    return d


def _k3b_inputs(h, wr, br, wgu, bgu, wd, bd, lng, lnb):
    return dict(h=np.ascontiguousarray(h), wr=np.ascontiguousarray(wr.reshape(8, 128, N_EXP).transpose(1, 0, 2)).astype(np.float32),
                br=np.ascontiguousarray(br.reshape(1, -1)).astype(np.float32), wgu=wgu,
                bgu=np.ascontiguousarray(bgu.reshape(N_EXP, 16, 128).transpose(2, 0, 1)).astype(np.float32),
                wd=wd, bd=np.ascontiguousarray(bd).astype(np.float32),
                lng=np.ascontiguousarray(lng.reshape(1, -1)).astype(np.float32), lnb=np.ascontiguousarray(lnb.reshape(1, -1)).astype(np.float32),
                ident=np.eye(128, dtype=np.float32))


def build_l2():
    nc = bass.Bass("TRN2", target_bir_lowering=False)
    P = Prog(nc)
    phase_k2a(nc, P, 'a_')
    P.barrier()
    phase_k2b(nc, P, 'b_')
    P.emit(final_keys=['out'])
    return nc, P


def build_l3(next_layer=None):
    nc = bass.Bass("TRN2", target_bir_lowering=False)
    P = Prog(nc)
    o3a = phase_k3a(nc, P, 'a_')
    P.barrier()
    o3b = phase_k3b(nc, P, 'b_', 0, N_EXP, True, True, ext={'h': o3a['h']})
    if next_layer is not None:
        P.barrier()
        phase_k1(nc, P, 'k_', next_layer, ext={'x': o3b['out']})
    P.emit(final_keys=['out'])
    return nc, P


def _launch(nc, in_maps):
    res = run_bass_kernel_spmd(nc, in_maps, core_ids=list(range(NCORES)))
    return res.results


def _pref(px, d):
    return {px + k: v for k, v in d.items()}


def kernel(x, w_in, b_fox_f, hgrn_lb_logits, hgrn_norm_g, w_branch_a, w_branch_b, w_branch_c, w_out,
           ln1_g, ln1_b, w_router, b_router, w_gu, b_gu, w_down, b_down, ln2_g, ln2_b):
    f32 = lambda a: np.ascontiguousarray(np.asarray(a, dtype=np.float32))
    x_cur = f32(x).reshape(-1, D_MODEL)
    lbl = f32(hgrn_lb_logits)
    w_perm = np.ascontiguousarray(f32(w_in[0])[:, K1_COLS])
    r1 = _launch(build_k1(0)[0], [k1_inputs(c, x_cur[c * TOK:(c + 1) * TOK], w_perm, f32(b_fox_f[0]), lbl) for c in range(NCORES)])
    del w_perm
    for l in range(DEPTH):
        catT = lambda nm, b: np.concatenate([r1[2 * b][nm], r1[2 * b + 1][nm]], axis=1)
        cat0 = lambda nm, b: np.concatenate([r1[2 * b][nm], r1[2 * b + 1][nm]], axis=0)
        in2 = []
        for b in range(BATCH):
            f = dict(aqT=catT('aqT', b), akT=catT('akT', b), av=cat0('av', b), fqT=catT('fqT', b), fkT=catT('fkT', b),
                     fv=cat0('fv', b), iqT=catT('iqT', b), ikT=catT('ikT', b), small=cat0('small', b))
            h = dict(qpT=catT('qpT', b), kpT=catT('kpT', b), kp=cat0('kp', b), cv=cat0('cv', b), em=catT('em', b), g=catT('g', b))
            for j in range(2):
                d = _pref('a_', k2a_inputs(j, f['aqT'], f['akT'], f['av'], f['fqT'], f['fkT'], f['fv'], f['iqT'], f['ikT'], f['small']))
                d.update(_pref('b_', k2b_inputs(j, h['qpT'], h['kpT'], h['kp'], h['cv'], h['em'], h['g'])))
                in2.append(d)
            del f, h
        r2 = _launch(build_l2()[0], in2)
        del in2
        nxt_l = l + 1 if l + 1 < DEPTH else None
        wgu_l, wd_l = f32(w_gu[l]), f32(w_down[l])
        if nxt_l is not None:
            w_perm = np.ascontiguousarray(f32(w_in[nxt_l])[:, K1_COLS])
        in3 = []
        for b in range(BATCH):
            oa = np.empty((64, 8, SEQ), dtype=NPBF)
            ob = np.empty((64, 8, SEQ), dtype=NPBF)
            for j in range(2):
                cols = _slot_cols(j)
                oa[:, :, cols] = r2[2 * b + j]['a_oaT']
                ob[:, :, cols] = r2[2 * b + j]['a_obT']
            oc = np.concatenate([r2[2 * b]['b_ocT'], r2[2 * b + 1]['b_ocT']], axis=1)
            for cc in range(2):
                c = 2 * b + cc
                sl = slice(cc * TOK, (cc + 1) * TOK)
                d = _pref('a_', k3a_inputs(np.ascontiguousarray(oa[:, :, sl]), np.ascontiguousarray(ob[:, :, sl]), np.ascontiguousarray(oc[:, :, sl]),
                                           r1[c]['cgT'], r1[c]['gT'], x_cur[c * TOK:(c + 1) * TOK],
                                           f32(w_branch_a[l]), f32(w_branch_b[l]), f32(w_branch_c[l]), f32(w_out[l]),
                                           f32(hgrn_norm_g[l]), f32(ln1_g[l]), f32(ln1_b[l])))
                d3b = _k3b_inputs(np.zeros((1, 1), np.float32), f32(w_router[l]), f32(b_router[l]), wgu_l, f32(b_gu[l]), wd_l, f32(b_down[l]),
                                  f32(ln2_g[l]), f32(ln2_b[l]))
                del d3b['h']
                d.update(_pref('b_', d3b))
                if nxt_l is not None:
                    d1 = k1_inputs(c, np.zeros((1, 1), np.float32), w_perm, f32(b_fox_f[nxt_l]), lbl)
                    del d1['x']
                    d.update(_pref('k_', d1))
                in3.append(d)
        del r1, r2
        r3 = _launch(build_l3(nxt_l)[0], in3)
        del in3
        x_cur = np.concatenate([np.asarray(r3[c]['b_out'], dtype=np.float32) for c in range(NCORES)], axis=0)
        if nxt_l is not None:
            r1 = [{k[2:]: v for k, v in r3[c].items() if k.startswith('k_')} for c in range(NCORES)]
        del r3
    return x_cur.reshape(BATCH, SEQ, D_MODEL)
```

```python
import numpy as np
import ml_dtypes
import concourse.bass as bass
import concourse.mybir as mybir
from concourse.bass_utils import run_bass_kernel_spmd

F32 = mybir.dt.float32
BF16 = mybir.dt.bfloat16
I32 = mybir.dt.int32
ALU = mybir.AluOpType
AF = mybir.ActivationFunctionType
AX = mybir.AxisListType
NPBF = ml_dtypes.bfloat16

D_MODEL = 1024
SEQ = 8192
BATCH = 4
DEPTH = 2
NCORES = 8
TOK = 4096
ALPHA = (2 * DEPTH) ** 0.25
N_EXP = 32

class Prog:
    def __init__(self, nc):
        self.nc = nc
        self.ops = []
        self.last_w = {}
        self.readers = {}

    def op(self, eng, fn, reads=(), writes=(), dma=None):
        idx = len(self.ops)
        deps = set()
        for b in reads:
            w = self.last_w.get(b)
            if w is not None:
                deps.add(w)
        for b in writes:
            w = self.last_w.get(b)
            if w is not None:
                deps.add(w)
            for r in self.readers.get(b, ()):
                deps.add(r)
        for b in writes:
            self.last_w[b] = idx
            self.readers[b] = []
        for b in reads:
            if b not in writes:
                self.readers.setdefault(b, []).append(idx)
        deps.discard(idx)
        self.ops.append(dict(eng=eng, fn=fn, deps=deps, dma=dma))
        return idx

    def dma(self, q, out, in_, reads=(), writes=(), key=None, **kw):
        assert key is not None
        return self.op(q, lambda e: e.dma_start(out=out, in_=in_, **kw), reads, writes, dma=key)

    def mm(self, out, lhsT, rhs, start, stop, reads=(), writes=()):
        return self.op('pe', lambda e: e.matmul(out, lhsT, rhs, start=start, stop=stop), reads, writes)

    def tr(self, out, in_, ident, reads=(), writes=()):
        return self.op('pe', lambda e: e.transpose(out, in_, ident), reads, writes)

    def barrier(self):
        last = {}
        for i, o in enumerate(self.ops):
            if o['fn'] is None:
                continue
            last[('dma', o['dma']) if o['dma'] is not None else ('eng', o['eng'])] = i
        deps = set(last.values())
        for eng in ('pe', 'act', 'dve', 'pool', 'sp'):
            self.ops.append(dict(eng=eng, fn=None, deps=set(deps), dma=None))
        self.last_w = {}
        self.readers = {}

    def emit(self, final_keys=()):
        nc = self.nc
        ops = self.ops
        n = len(ops)
        need = [False] * n
        for o in ops:
            best = {}
            ed = set()
            for d in o['deps']:
                po = ops[d]
                if po['eng'] == 'pe' and o['eng'] == 'pe' and po['dma'] is None and o['dma'] is None:
                    continue
                if po['dma'] is not None:
                    ed.add(d)
                else:
                    best[po['eng']] = max(best.get(po['eng'], -1), d)
            ed.update(best.values())
            o['edeps'] = ed
            for d in ed:
                need[d] = True
        for i, o in enumerate(ops):
            if o['dma'] is not None:
                need[i] = True
        eng_cnt = {}
        key_cnt = {}
        sig = [None] * n
        key_before = [None] * n
        key_events = {}
        for i, o in enumerate(ops):
            if o['dma'] is not None:
                k = o['dma']
                key_cnt[k] = key_cnt.get(k, 0) + 1
                key_events.setdefault(k, []).append(i)
                sig[i] = ('dma:' + str(k), 16 * key_cnt[k])
            elif need[i]:
                e = o['eng']
                eng_cnt[e] = eng_cnt.get(e, 0) + 1
                sig[i] = ('eng:' + e, eng_cnt[e])
        semnames = sorted(set(s[0] for s in sig if s is not None))
        import bisect
        from contextlib import ExitStack
        with ExitStack() as st:
            sems = {nm: st.enter_context(nc.semaphore('s%d' % j)) for j, nm in enumerate(semnames)}
            block = st.enter_context(nc.Block())
            by_eng = {}
            for i, o in enumerate(ops):
                by_eng.setdefault(o['eng'], []).append(i)

            def run(engname, e):
                waited = {}
                for i in by_eng.get(engname, []):
                    o = ops[i]
                    wl = {}
                    for d in o['edeps']:
                        po = ops[d]
                        nm, val = sig[d]
                        if po['dma'] is not None:
                            ev = key_events[po['dma']]
                            cnt = bisect.bisect_left(ev, i)
                            val = 16 * cnt
                        if val > wl.get(nm, 0):
                            wl[nm] = val
                    for nm, val in wl.items():
                        if val > waited.get(nm, 0):
                            e.wait_ge(sems[nm], val)
                            waited[nm] = val
                    if o['fn'] is None:
                        continue
                    ins = o['fn'](e)
                    if sig[i] is not None:
                        nm, val = sig[i]
                        ins.then_inc(sems[nm], 16 if o['dma'] is not None else 1)
                for k in final_keys:
                    ev = key_events.get(k, [])
                    if ev and ops[ev[-1]]['eng'] == engname:
                        e.wait_ge(sems['dma:' + str(k)], 16 * len(ev))

            @block.tensor
            def _(e):
                run('pe', e)

            @block.scalar
            def _(e):
                run('act', e)

            @block.vector
            def _(e):
                run('dve', e)

            @block.gpsimd
            def _(e):
                run('pool', e)

            @block.sync
            def _(e):
                run('sp', e)
        self.stats = dict(n_ops=n, eng_cnt=eng_cnt, n_sems=len(semnames))


SPL = dict(aq=(0, 512), ak=(512, 1024), av=(1024, 1536), iq=(1536, 1792), ik=(1792, 1856), iw=(1856, 1860),
           fq=(1860, 2372), fk=(2372, 2884), fv=(2884, 3396), ff=(3396, 3404), cq=(3404, 3916), cf=(3916, 4428),
           ci=(4428, 4940), cg=(4940, 5452), gt=(5452, 8524))


def _rot_cols(lo, hi):
    c = np.arange(lo, hi)
    base = lo + ((c - lo) // 64) * 64
    return base + ((c - base + 32) % 64)


def k1_weight_columns():
    cols = []
    off = {}
    pos = 0

    def add(name, idx):
        nonlocal pos
        off[name] = (pos, len(idx))
        cols.append(np.asarray(idx))
        pos += len(idx)

    for nm in ('aq', 'ak', 'iq', 'ik'):
        lo, hi = SPL[nm]
        add(nm, np.arange(lo, hi))
        add(nm + '_rot', _rot_cols(lo, hi))
    for nm in ('fq', 'fk', 'cq', 'cf', 'cg', 'gt', 'av', 'fv', 'ci'):
        lo, hi = SPL[nm]
        add(nm, np.arange(lo, hi))
    add('small', np.concatenate([np.arange(*SPL['iw']), np.arange(*SPL['ff'])]))
    return np.concatenate(cols), off


K1_COLS, K1_OFF = k1_weight_columns()
K1_NCOLS = len(K1_COLS)


def rope_tables(positions):
    half = 32
    inv = (10000.0 ** (-np.arange(half, dtype=np.float32) / half)).astype(np.float32)
    ang = positions.astype(np.float32)[None, :] * inv[:, None]
    cos = np.cos(ang).astype(np.float32)
    sin = np.sin(ang).astype(np.float32)
    cosT = np.concatenate([cos, cos, cos, cos], axis=0)
    sinT = np.concatenate([-sin, sin, -sin, sin], axis=0)
    return np.ascontiguousarray(cosT), np.ascontiguousarray(sinT)


def build_k1(layer=0):
    nc = bass.Bass("TRN2", target_bir_lowering=False)
    P = Prog(nc)
    phase_k1(nc, P, '', layer)
    P.emit(final_keys=['out'])
    return nc, P


def phase_k1(nc, P, px, layer=0, ext=None):
    ext = ext or {}
    T = TOK
    NT = T // 128
    NTC = T // 512
    x_d = ext['x'] if 'x' in ext else nc.dram_tensor(px + "x", [T, D_MODEL], F32, kind="ExternalInput").ap()
    w_d = nc.dram_tensor(px + "w", [D_MODEL, K1_NCOLS], F32, kind="ExternalInput").ap()
    cos_d = nc.dram_tensor(px + "cosT", [128, T], F32, kind="ExternalInput").ap()
    sin_d = nc.dram_tensor(px + "sinT", [128, T], F32, kind="ExternalInput").ap()
    bfox_d = nc.dram_tensor(px + "bfox", [1, 8], F32, kind="ExternalInput").ap()
    lbl_d = nc.dram_tensor(px + "lbl", [128, 2, 4], F32, kind="ExternalInput").ap()
    ident_d = nc.dram_tensor(px + "ident", [128, 128], F32, kind="ExternalInput").ap()
    rmask_d = nc.dram_tensor(px + "rmask", [128, 512], F32, kind="ExternalInput").ap()

    def out_t(name, shape, dt):
        return nc.dram_tensor(px + name, shape, dt, kind="ExternalOutput").ap()

    o_aqT = out_t("aqT", [512, T], BF16)
    o_akT = out_t("akT", [512, T], BF16)
    o_iqT = out_t("iqT", [256, T], BF16)
    o_ikT = out_t("ikT", [64, T], BF16)
    o_fqT = out_t("fqT", [512, T], BF16)
    o_fkT = out_t("fkT", [512, T], BF16)
    o_av = out_t("av", [T, 512], BF16)
    o_fv = out_t("fv", [T, 512], BF16)
    o_cv = out_t("cv", [T, 512], BF16)
    o_small = out_t("small", [T, 12], F32)
    o_qpT = out_t("qpT", [512, T], BF16)
    o_kpT = out_t("kpT", [512, T], BF16)
    o_kp = out_t("kp", [T, 512], BF16)
    o_em = out_t("em", [512, T // 64], F32)
    o_g = out_t("g", [512, T // 64], F32)
    o_cgT = out_t("cgT", [512, T], BF16)
    o_gT = out_t("gT", [3072, T], BF16)

    from contextlib import ExitStack
    with ExitStack() as st:
        def sb(name, shape, dt):
            return st.enter_context(nc.sbuf_tensor(px + name, shape, dt))
        xT = sb("xT", [128, 8, T], BF16)
        cosT = sb("cosT_s", [128, T], F32)
        sinT = sb("sinT_s", [128, T], F32)
        ident = sb("ident_s", [128, 128], F32)
        identb = sb("identb", [128, 128], BF16)
        rmask = sb("rmask_s", [128, 512], F32)
        lb = sb("lb_s", [128, 4], F32)
        oml = sb("oml", [128, 4], F32)
        noml = sb("noml", [128, 4], F32)
        bfox = sb("bfox_s", [128, 8], F32)
        xin = [sb("xin%d" % i, [128, D_MODEL], F32) for i in range(2)]
        xbf = [sb("xbf%d" % i, [128, D_MODEL], BF16) for i in range(2)]
        wst = [sb("wst%d" % i, [128, 8, 512], F32) for i in range(2)]
        wbf = [sb("wbf%d" % i, [128, 8, 512], BF16) for i in range(2)]
        t1 = [sb("t1_%d" % i, [128, 512], F32) for i in range(2)]
        t2 = [sb("t2_%d" % i, [128, 512], F32) for i in range(2)]
        t3 = [sb("t3_%d" % i, [128, 512], F32) for i in range(2)]
        t4 = [sb("t4_%d" % i, [128, 512], F32) for i in range(2)]
        ob = [sb("ob%d" % i, [128, 512], BF16) for i in range(4)]
        ob2 = [sb("ob2_%d" % i, [128, 512], BF16) for i in range(2)]
        osm = [sb("osm%d" % i, [128, 12], F32) for i in range(2)]
        emg = sb("emg", [128, 2, 4, T // 64], F32)
        ps = st.enter_context(nc.psum_tensor(px + "ps", [128, 8, 512], F32))
        psb = ps
        cnt = dict(ps=0, ob=0, ob2=0, t=0, w=0, x=0, osm=0)

        def nxt(k, n):
            v = cnt[k] % n
            cnt[k] += 1
            return v

        P.dma('sp', cosT[:], cos_d, writes=['cosT'], key='c0')
        P.dma('sp', sinT[:], sin_d, writes=['sinT'], key='c1')
        P.dma('sp', ident[:], ident_d, writes=['ident'], key='c2')
        P.dma('sp', rmask[:], rmask_d, writes=['rmask'], key='c3')
        lbl = sb("lbl_s", [128, 2, 4], F32)
        P.dma('sp', lbl[:], lbl_d, writes=['lbl'], key='c4')
        if layer == 0:
            P.op('dve', lambda e: e.memset(lb[:], 0.0), writes=['lb'])
        else:
            P.op('dve', lambda e: e.tensor_tensor(lb[:], lbl[:, 1, :], lbl[:, 0, :], ALU.subtract), reads=['lbl'], writes=['lb'])
            P.op('act', lambda e: e.activation(lb[:], lb[:], AF.Sigmoid), reads=['lb'], writes=['lb'])
        P.dma('sp', bfox[:], bfox_d.partition_broadcast(128), writes=['bfox'], key='c5')
        P.op('dve', lambda e: e.tensor_copy(identb[:], ident[:]), reads=['ident'], writes=['identb'])
        P.op('dve', lambda e: e.tensor_scalar(oml[:], lb[:], -1.0, 1.0, ALU.mult, ALU.add), reads=['lb'], writes=['oml'])
        P.op('dve', lambda e: e.tensor_scalar(noml[:], lb[:], 1.0, -1.0, ALU.mult, ALU.add), reads=['lb'], writes=['noml'])

        for ti in range(NT):
            b = nxt('x', 2)
            P.dma('sp', xin[b][:], x_d[ti * 128:(ti + 1) * 128, :], writes=[('xin', b)], key=('xin', b))
            P.op('act', lambda e, b=b: e.copy(xbf[b][:], xin[b][:]), reads=[('xin', b)], writes=[('xbf', b)])
            for half in range(2):
                pb = nxt('ps', 8)
                pst = ps[:, pb, :].bitcast(BF16)
                for j in range(4):
                    k = half * 4 + j
                    P.tr(pst[:, j * 128:(j + 1) * 128], xbf[b][:, k * 128:(k + 1) * 128], identb[:],
                         reads=[('xbf', b), 'identb'], writes=[('ps', pb)])
                P.op('dve', lambda e, pst=pst, half=half, ti=ti: e.tensor_copy(
                    xT[:, half * 4:(half + 1) * 4, ti * 128:(ti + 1) * 128],
                    pst[:, 0:512].rearrange("p (j t) -> p j t", j=4)),
                    reads=[('ps', pb)], writes=[('xT', ti)])

        w_v = w_d.rearrange("(c p) n -> p c n", p=128)

        def load_w(col0, ncols):
            b = nxt('w', 2)
            P.dma('sp', wst[b][:, :, 0:ncols], w_v[:, :, col0:col0 + ncols], writes=[('wst', b)], key=('wst', b))
            P.op('pool', lambda e: e.tensor_copy(wbf[b][:, :, 0:ncols], wst[b][:, :, 0:ncols]),
                 reads=[('wst', b)], writes=[('wbf', b)])
            return b

        def fm_matmul(wb, c0, m, tc):
            pb = nxt('ps', 8)
            for k in range(8):
                P.mm(ps[0:m, pb, :], wbf[wb][:, k, c0:c0 + m], xT[:, k, tc * 512:(tc + 1) * 512], k == 0, k == 7,
                     reads=[('wbf', wb)] + [('xT', tc * 4 + i) for i in range(4)], writes=[('ps', pb)])
            return pb

        all_xT = [('xT', i) for i in range(NT)]

        jobs = []
        def rope_group(nm, o_d):
            s0, n = K1_OFF[nm]
            r0, _ = K1_OFF[nm + '_rot']
            nblk = max(1, n // 128)
            m = min(n, 128)
            for bi in range(nblk):
              def load(bi=bi):
                b = nxt('w', 2)
                P.dma('sp', wst[b][:, :, 0:m], w_v[:, :, s0 + bi * 128:s0 + bi * 128 + m], writes=[('wst', b)], key=('wst', b))
                P.dma('sp', wst[b][:, :, 128:128 + m], w_v[:, :, r0 + bi * 128:r0 + bi * 128 + m], writes=[('wst', b)], key=('wst', b))
                P.op('pool', lambda e, b=b: e.tensor_copy(wbf[b][:, :, 0:256], wst[b][:, :, 0:256]),
                     reads=[('wst', b)], writes=[('wbf', b)])
                return b
              def comp(b, bi=bi):
                for tc in range(NTC):
                    p0 = fm_matmul(b, 0, m, tc)
                    p1 = fm_matmul(b, 128, m, tc)
                    tb = nxt('t', 2)
                    sl = slice(tc * 512, (tc + 1) * 512)
                    P.op('dve', lambda e, p0=p0, tb=tb, sl=sl: e.tensor_tensor(t1[tb][0:m, :], ps[0:m, p0, :], cosT[0:m, sl], ALU.mult),
                         reads=[('ps', p0), 'cosT'], writes=[('t1', tb)])
                    P.op('dve', lambda e, p1=p1, tb=tb, sl=sl: e.tensor_tensor(t2[tb][0:m, :], ps[0:m, p1, :], sinT[0:m, sl], ALU.mult),
                         reads=[('ps', p1), 'sinT'], writes=[('t2', tb)])
                    o = nxt('ob', 4)
                    P.op('pool', lambda e, tb=tb, o=o: e.tensor_tensor(ob[o][0:m, :], t1[tb][0:m, :], t2[tb][0:m, :], ALU.add),
                         reads=[('t1', tb), ('t2', tb)], writes=[('ob', o)])
                    P.dma('sp', o_d[bi * 128:bi * 128 + m, sl], ob[o][0:m, :], reads=[('ob', o)], key='out')
              jobs.append((load, comp))

        rope_group('aq', o_aqT)
        rope_group('ak', o_akT)
        rope_group('iq', o_iqT)
        rope_group('ik', o_ikT)

        def plain_group(nm, o_d, func):
            s0, n = K1_OFF[nm]
            for g0 in range(0, n, 512):
                gn = min(512, n - g0)
                def load(g0=g0, gn=gn):
                    return load_w(s0 + g0, gn)
                def comp(b, g0=g0, gn=gn):
                  for bi in range(gn // 128):
                    for tc in range(NTC):
                        p0 = fm_matmul(b, bi * 128, 128, tc)
                        o = nxt('ob', 4)
                        sl = slice(tc * 512, (tc + 1) * 512)
                        P.op('act', lambda e, p0=p0, o=o: e.activation(ob[o][:], ps[:, p0, :], func),
                             reads=[('ps', p0)], writes=[('ob', o)])
                        r0 = g0 + bi * 128
                        P.dma('sp', o_d[r0:r0 + 128, sl], ob[o][:], reads=[('ob', o)], key='out')
                jobs.append((load, comp))

        plain_group('fq', o_fqT, AF.Copy)
        plain_group('fk', o_fkT, AF.Copy)
        plain_group('cg', o_cgT, AF.Sigmoid)
        plain_group('gt', o_gT, AF.Sigmoid)

        sq0, _ = K1_OFF['cq']
        sf0, _ = K1_OFF['cf']
        for pr in range(4):
          def load(pr=pr):
            b = nxt('w', 2)
            P.dma('sp', wst[b][:, :, 0:128], w_v[:, :, sq0 + pr * 128:sq0 + (pr + 1) * 128], writes=[('wst', b)], key=('wst', b))
            P.dma('sp', wst[b][:, :, 128:256], w_v[:, :, sf0 + pr * 128:sf0 + (pr + 1) * 128], writes=[('wst', b)], key=('wst', b))
            P.op('pool', lambda e, b=b: e.tensor_copy(wbf[b][:, :, 0:256], wst[b][:, :, 0:256]),
                 reads=[('wst', b)], writes=[('wbf', b)])
            return b
          def comp(b, pr=pr):
            for tc in range(NTC):
                pq = fm_matmul(b, 0, 128, tc)
                pf = fm_matmul(b, 128, 128, tc)
                tb = nxt('t', 2)
                sl = slice(tc * 512, (tc + 1) * 512)
                P.op('act', lambda e, pf=pf, tb=tb: e.activation(t1[tb][:], ps[:, pf, :], AF.Sigmoid),
                     reads=[('ps', pf)], writes=[('t1', tb)])
                P.op('act', lambda e, tb=tb, pr=pr: e.activation(t2[tb][:], t1[tb][:], AF.Ln, bias=lb[:, pr:pr + 1], scale=oml[:, pr:pr + 1]),
                     reads=[('t1', tb), 'lb', 'oml'], writes=[('t2', tb)])
                P.op('pool', lambda e, tb=tb, pr=pr: e.tensor_scalar(t3[tb][:], t1[tb][:], noml[:, pr:pr + 1], oml[:, pr:pr + 1], ALU.mult, ALU.add),
                     reads=[('t1', tb), 'oml', 'noml'], writes=[('t3', tb)])
                P.op('dve', lambda e, tb=tb: e.tensor_tensor_scan(t4[tb][:], rmask[:], t2[tb][:], 0.0, ALU.mult, ALU.add),
                     reads=[('t2', tb), 'rmask'], writes=[('t4', tb)])
                b3 = t4[tb][:].rearrange("p (c t) -> p c t", t=64)
                P.op('act', lambda e, b3=b3, pr=pr, tc=tc: e.activation(emg[:, 0, pr, tc * 8:(tc + 1) * 8], b3[:, :, 31], AF.Exp),
                     reads=[('t4', tb)], writes=[('emg', pr)])
                P.op('dve', lambda e, tb=tb, b3=b3: e.tensor_tensor(t2[tb][:].rearrange("p (c t) -> p c t", t=64), b3,
                                                                   b3[:, :, 31:32].to_broadcast([128, 8, 64]), ALU.subtract),
                     reads=[('t4', tb)], writes=[('t2', tb)])
                P.op('act', lambda e, tb=tb: e.activation(t4[tb][:], t2[tb][:], AF.Exp),
                     reads=[('t2', tb)], writes=[('t4', tb)])
                P.op('act', lambda e, tb=tb: e.activation(t1[tb][:], t2[tb][:], AF.Exp, scale=-1.0),
                     reads=[('t2', tb)], writes=[('t1', tb)])
                P.op('pool', lambda e, tb=tb, pr=pr, tc=tc: e.tensor_copy(emg[:, 1, pr, tc * 8:(tc + 1) * 8],
                                                                       t4[tb][:].rearrange("p (c t) -> p c t", t=64)[:, :, 63]),
                     reads=[('t4', tb)], writes=[('emg', pr)])
                o = nxt('ob', 4)
                P.op('dve', lambda e, pq=pq, tb=tb, o=o: e.tensor_tensor(ob[o][:], ps[:, pq, :], t4[tb][:], ALU.mult),
                     reads=[('ps', pq), ('t4', tb)], writes=[('ob', o)])
                P.dma('sp', o_qpT[pr * 128:(pr + 1) * 128, sl], ob[o][:], reads=[('ob', o)], key='out')
                o2 = nxt('ob', 4)
                P.op('pool', lambda e, tb=tb, o2=o2: e.tensor_tensor(ob[o2][:], t3[tb][:], t1[tb][:], ALU.mult),
                     reads=[('t3', tb), ('t1', tb)], writes=[('ob', o2)])
                P.dma('sp', o_kpT[pr * 128:(pr + 1) * 128, sl], ob[o2][:], reads=[('ob', o2)], key='out')
                pb = nxt('ps', 8)
                pst = ps[:, pb, :].bitcast(BF16)
                for j in range(4):
                    P.tr(pst[:, j * 128:(j + 1) * 128], ob[o2][:, j * 128:(j + 1) * 128], identb[:],
                         reads=[('ob', o2), 'identb'], writes=[('ps', pb)])
                o3 = nxt('ob2', 2)
                P.op('act', lambda e, pst=pst, o3=o3: e.copy(ob2[o3][:], pst[:, 0:512]),
                     reads=[('ps', pb)], writes=[('ob2', o3)])
                P.dma('sp', o_kp[tc * 512:(tc + 1) * 512, pr * 128:(pr + 1) * 128].rearrange("(j t) c -> t j c", t=128),
                      ob2[o3][:].rearrange("t (j c) -> t j c", j=4), reads=[('ob2', o3)], key='out')
            P.dma('sp', o_em[pr * 128:(pr + 1) * 128, :], emg[:, 0, pr, :], reads=[('emg', pr)], key='out')
            P.dma('sp', o_g[pr * 128:(pr + 1) * 128, :], emg[:, 1, pr, :], reads=[('emg', pr)], key='out')
          jobs.append((load, comp))

        for nm, o_d in (('av', o_av), ('fv', o_fv), ('ci', o_cv)):
            s0, n = K1_OFF[nm]
            def load(s0=s0):
                return load_w(s0, 512)
            def comp(b, o_d=o_d):
              for ti in range(NT):
                pb = nxt('ps', 8)
                for k in range(8):
                    P.mm(ps[:, pb, :], xT[:, k, ti * 128:(ti + 1) * 128], wbf[b][:, k, :], k == 0, k == 7,
                         reads=[('wbf', b), ('xT', ti)], writes=[('ps', pb)])
                o = nxt('ob', 4)
                P.op('act', lambda e, pb=pb, o=o: e.copy(ob[o][:], ps[:, pb, :]), reads=[('ps', pb)], writes=[('ob', o)])
                P.dma('sp', o_d[ti * 128:(ti + 1) * 128, :], ob[o][:], reads=[('ob', o)], key='out')
            jobs.append((load, comp))

        s0, n = K1_OFF['small']
        def load(s0=s0):
            return load_w(s0, 12)
        def comp(b):
          for ti in range(NT):
            pb = nxt('ps', 8)
            for k in range(8):
                P.mm(ps[:, pb, 0:12], xT[:, k, ti * 128:(ti + 1) * 128], wbf[b][:, k, 0:12], k == 0, k == 7,
                     reads=[('wbf', b), ('xT', ti)], writes=[('ps', pb)])
            o = nxt('osm', 2)
            P.op('dve', lambda e, pb=pb, o=o: e.tensor_scalar(osm[o][:, 0:4], ps[:, pb, 0:4], 1.0 / 16.0, None, ALU.mult),
                 reads=[('ps', pb)], writes=[('osm', o)])
            P.op('dve', lambda e, pb=pb, o=o: e.tensor_tensor(osm[o][:, 4:12], ps[:, pb, 4:12], bfox[:], ALU.add),
                 reads=[('ps', pb), 'bfox'], writes=[('osm', o)])
            P.op('act', lambda e, o=o: e.activation(osm[o][:, 4:12], osm[o][:, 4:12], AF.Sigmoid),
                 reads=[('osm', o)], writes=[('osm', o)])
            P.op('act', lambda e, o=o: e.activation(osm[o][:, 4:12], osm[o][:, 4:12], AF.Ln),
                 reads=[('osm', o)], writes=[('osm', o)])
            P.dma('sp', o_small[ti * 128:(ti + 1) * 128, :], osm[o][:], reads=[('osm', o)], key='out')
        jobs.append((load, comp))

        nb = jobs[0][0]()
        for ji in range(len(jobs)):
            cur = nb
            if ji + 1 < len(jobs):
                nb = jobs[ji + 1][0]()
            jobs[ji][1](cur)

    return dict(aqT=o_aqT)


def k1_inputs(core, x_slice, w_perm, b_fox, lb_logits):
    pos = (np.arange(TOK) + (core % 2) * TOK).astype(np.float32)
    cosT, sinT = rope_tables(pos)
    rmask = np.ones((128, 512), np.float32)
    rmask[:, ::64] = 0.0
    lbl = np.ascontiguousarray(lb_logits.reshape(2, 4, 128).transpose(2, 0, 1)).astype(np.float32)
    return dict(x=np.ascontiguousarray(x_slice), w=w_perm, cosT=cosT, sinT=sinT,
                bfox=np.ascontiguousarray(b_fox.reshape(1, 8)).astype(np.float32), lbl=lbl,
                ident=np.eye(128, dtype=np.float32), rmask=rmask)


NSLOT = 8
NBIS = 16
TOPK = 256.0
NEG = -1.0e30


def build_k2a():
    nc = bass.Bass("TRN2", target_bir_lowering=False)
    P = Prog(nc)
    phase_k2a(nc, P, '')
    P.emit(final_keys=['out'])
    return nc, P


def phase_k2a(nc, P, px, ext=None):
    ext = ext or {}
    S = SEQ
    QT = NSLOT * 512
    din = lambda name, shape, dt: ext[name] if name in ext else nc.dram_tensor(px + name, shape, dt, kind="ExternalInput").ap()
    aq_d = din("aq", [4, 128, QT], BF16)
    ak_d = din("ak", [4, 128, S], BF16)
    av_d = din("av", [4, 128, 64, 130], BF16)
    fq_d = din("fq", [4, 128, QT], BF16)
    fk_d = din("fk", [4, 128, S], BF16)
    fv_d = din("fv", [4, 128, 64, 130], BF16)
    iq_d = din("iq", [128, 2, QT], BF16)
    ik_d = din("ik2", [128, S], BF16)
    iw_d = din("iw", [128, NSLOT * 4, 4], F32)
    lf_d = din("lf", [128, 64, 8], F32)
    adm_d = din("adm", [4, 128, 1024], F32)
    cm_d = din("cm", [128, 8, 512], BF16)
    jf_d = din("jflag", [128, 1], F32)
    tri_d = din("tri", [128, 128], F32)
    ident_d = din("identb", [128, 128], BF16)
    pw_d = din("pw2", [128, NBIS], F32)
    oa_d = nc.dram_tensor(px + "oaT", [64, 8, QT], BF16, kind="ExternalOutput").ap()
    ob_d = nc.dram_tensor(px + "obT", [64, 8, QT], BF16, kind="ExternalOutput").ap()

    from contextlib import ExitStack
    with ExitStack() as st:
        def sb(name, shape, dt):
            return st.enter_context(nc.sbuf_tensor(px + name, shape, dt))
        Sc = sb("Sc", [128, S], F32)
        Mk = sb("Mk", [128, S], BF16)
        MT = sb("MT", [128, 64, 512], BF16)
        ik2 = sb("ik2s", [128, S], BF16)
        KT = [sb("KT%d" % i, [128, 2048], BF16) for i in range(2)]
        VA = [sb("VA%d" % i, [128, 16, 130], BF16) for i in range(2)]
        qT = [sb("qT%d" % i, [128, 512], BF16) for i in range(2)]
        iqc = sb("iqc", [128, 2, 512], BF16)
        iw = sb("iws", [128, NSLOT * 4, 4], F32)
        lf = sb("lfs", [128, 64, 8], F32)
        cw = sb("cw", [128, 64, 8], F32)
        offs = sb("offs", [128, 65, 8], F32)
        tot = sb("tot", [128, 64, 8], F32)
        adm = [sb("adm%d" % i, [128, 1024], F32) for i in range(2)]
        cm = sb("cms", [128, 8, 512], BF16)
        jf = sb("jfs", [128, 1], F32)
        tri = sb("tris", [128, 128], F32)
        ones = sb("ones", [128, 128], F32)
        identb = sb("identbs", [128, 128], BF16)
        pw = sb("pws", [128, NBIS], F32)
        rl = [sb("rl%d" % i, [128, 512], F32) for i in range(3)]
        pT = [sb("pT%d" % i, [128, 512], BF16) for i in range(6)]
        U = [sb("U%d" % i, [65, 512], F32) for i in range(2)]
        R = [sb("R%d" % i, [65, 512], F32) for i in range(2)]
        oT = [sb("oT%d" % i, [64, 512], BF16) for i in range(2)]
        bfx = [sb("bfx%d" % i, [128, 64], F32) for i in range(2)]
        cref = sb("cref", [128, 8], F32)
        sm = sb("sm", [128, 16], F32)
        dk = sb("dk", [128, NBIS], F32)
        ps = st.enter_context(nc.psum_tensor(px + "ps", [128, 8, 512], F32))
        cnt = {}

        def nxt(k, n):
            v = cnt.get(k, 0) % n
            cnt[k] = cnt.get(k, 0) + 1
            return v

        for i, (t, d, kn) in enumerate([(ik2, ik_d, 'ik2s'), (iw, iw_d, 'iws'), (lf, lf_d, 'lfs'), (cm, cm_d, 'cms'), (jf, jf_d, 'jfs'),
                                        (tri, tri_d, 'tris'), (identb, ident_d, 'identbs'), (pw, pw_d, 'pws')]):
            P.dma('sp', t[:], d, writes=[kn], key='c%d' % i)
        P.op('pool', lambda e: e.memset(ones[:], 1.0), writes=['ones'])

        lf2 = lf[:].rearrange("p b h -> p (b h)")
        P.mm(ps[:, 0, :], tri[:], lf2, True, True, reads=['tris', 'lfs'], writes=[('ps', 0)])
        P.mm(ps[:, 1, :], ones[:], lf2, True, True, reads=['ones', 'lfs'], writes=[('ps', 1)])
        P.op('act', lambda e: e.copy(tot[:].rearrange("p b h -> p (b h)"), ps[:, 1, :]), reads=[('ps', 1)], writes=['tot'])
        P.op('dve', lambda e: e.memset(offs[:, 0, :], 0.0), writes=['offs'])
        for bl in range(64):
            P.op('dve', lambda e, bl=bl: e.tensor_tensor(offs[:, bl + 1, :], offs[:, bl, :], tot[:, bl, :], ALU.add),
                 reads=['offs', 'tot'], writes=['offs'])
        P.op('dve', lambda e: e.tensor_tensor(cw[:], ps[:, 0, :].rearrange("p (b h) -> p b h", h=8), offs[:, 0:64, :], ALU.add),
             reads=[('ps', 0), 'offs'], writes=['cw'])
        P.op('dve', lambda e: e.tensor_scalar(cw[:], cw[:], -1.0, None, ALU.mult), reads=['cw'], writes=['cw'])

        def gbank():
            return nxt('g', 4)

        def attention(slot, branch):
            nblk = (2 * slot + 2) * 4
            q_d, k_d, v_d, o_d = (aq_d, ak_d, av_d, oa_d) if branch == 'a' else (fq_d, fk_d, fv_d, ob_d)
            jobs = []
            for pr in range(4):
                segs = [(s0, min(16, nblk - s0)) for s0 in range(0, nblk, 16)]
                for si, (s0, sn) in enumerate(segs):
                    def load(pr=pr, s0=s0, sn=sn, si=si):
                        b = nxt('kv', 2)
                        P.dma('sp', KT[b][:, 0:sn * 128], k_d[pr, :, s0 * 128:(s0 + sn) * 128], writes=[('KT', b)], key=('KT', b))
                        P.dma('sp', VA[b][:, 0:sn, :], v_d[pr, :, s0:s0 + sn, :], writes=[('VA', b)], key=('VA', b))
                        qb_ = None
                        if si == 0:
                            qb_ = nxt('q', 2)
                            P.dma('sp', qT[qb_][:], q_d[pr, :, slot * 512:(slot + 1) * 512], writes=[('qT', qb_)], key=('qT', qb_))
                        return (b, qb_)

                    def comp(ld, st_, pr=pr, s0=s0, sn=sn, si=si, last=(si == len(segs) - 1)):
                        b, qb_ = ld
                        if si == 0:
                            st_['q'] = qb_
                            st_['acc'] = [4, 5]
                            if branch == 'b':
                                for hh in range(2):
                                    h = pr * 2 + hh
                                    P.op('dve', lambda e, hh=hh, h=h: e.tensor_scalar(bfx[hh][:, 0:nblk], cw[:, 0:nblk, h], cref[:, h:h + 1], 60.0, ALU.subtract, ALU.min),
                                         reads=['cw', 'cref'], writes=[('bfx', hh)])
                        qb = st_['q']
                        LA = 2
                        pend = []

                        def emit_pv(it):
                            hh, kl, pi = it
                            kb = s0 + kl
                            acc = st_['acc'][hh]
                            P.mm(ps[0:65, acc, :], VA[b][:, kl, hh * 65:(hh + 1) * 65], pT[pi][:], kb == 0, kb == nblk - 1,
                                 reads=[('VA', b), ('pT', pi)], writes=[('ps', acc)])
                            if last and kl == sn - 1:
                                h = pr * 2 + hh
                                ui = nxt('U', 2)
                                P.op('act', lambda e, ui=ui, acc=acc: e.copy(U[ui][:], ps[0:65, acc, :]), reads=[('ps', acc)], writes=[('U', ui)])
                                P.op('dve', lambda e, ui=ui: e.reciprocal(R[ui][64:65, :], U[ui][64:65, :]), reads=[('U', ui)], writes=[('R', ui)])
                                P.mm(ps[0:64, 7, :], ones[64:65, 0:64], R[ui][64:65, :], True, True, reads=['ones', ('R', ui)], writes=[('ps', 7)])
                                oi = nxt('oT', 2)
                                P.op('dve', lambda e, ui=ui, oi=oi: e.tensor_tensor(oT[oi][:], U[ui][0:64, :], ps[0:64, 7, :], ALU.mult),
                                     reads=[('U', ui), ('ps', 7)], writes=[('oT', oi)])
                                P.dma('sp', o_d[:, h, slot * 512:(slot + 1) * 512], oT[oi][:], reads=[('oT', oi)], key='out')

                        for hh in range(2):
                            for kl in range(sn):
                                kb = s0 + kl
                                g = gbank()
                                P.mm(ps[:, g, :], KT[b][hh * 64:(hh + 1) * 64, kl * 128:(kl + 1) * 128], qT[qb][hh * 64:(hh + 1) * 64, :], True, True,
                                     reads=[('KT', b), ('qT', qb)], writes=[('ps', g)])
                                pi = nxt('pT', 6)
                                meng = 'pool' if (kb % 2 == 0) else 'dve'
                                if branch == 'a':
                                    P.op('act', lambda e, g=g, pi=pi: e.activation(pT[pi][:], ps[:, g, :], AF.Exp, scale=0.125),
                                         reads=[('ps', g)], writes=[('pT', pi)])
                                    P.op(meng, lambda e, pi=pi, kb=kb: e.tensor_tensor(pT[pi][:], pT[pi][:], MT[:, kb, :], ALU.mult),
                                         reads=[('pT', pi), 'MT'], writes=[('pT', pi)])
                                else:
                                    P.op('act', lambda e, g=g, pi=pi, hh=hh, kb=kb: e.activation(pT[pi][:], ps[:, g, :], AF.Exp, bias=bfx[hh][:, kb:kb + 1], scale=0.125),
                                         reads=[('ps', g), ('bfx', hh)], writes=[('pT', pi)])
                                    if kb >= nblk - 8:
                                        P.op(meng, lambda e, pi=pi, kb=kb: e.tensor_tensor(pT[pi][:], pT[pi][:], cm[:, kb - (nblk - 8), :], ALU.mult),
                                             reads=[('pT', pi), 'cms'], writes=[('pT', pi)])
                                pend.append((hh, kl, pi))
                                if len(pend) > LA:
                                    emit_pv(pend.pop(0))
                        while pend:
                            emit_pv(pend.pop(0))
                    jobs.append((load, comp))
            return jobs

        def run_jobs(jobs):
            st_ = {}
            nb = jobs[0][0]()
            for ji in range(len(jobs)):
                cur = nb
                if ji + 1 < len(jobs):
                    nb = jobs[ji + 1][0]()
                jobs[ji][1](cur, st_)

        for slot in range(NSLOT):
            nblk = (2 * slot + 2) * 4
            n = nblk * 128
            P.dma('sp', iqc[:], iq_d[:, :, slot * 512:(slot + 1) * 512], writes=['iqc'], key='iqc')
            P.op('dve', lambda e, slot=slot: e.tensor_tensor(cref[:], offs[:, 8 * slot + 4, :], offs[:, 8 * slot, :], ALU.subtract),
                 reads=['offs'], writes=['cref'])
            P.op('dve', lambda e, slot=slot: e.scalar_tensor_tensor(cref[:], cref[:], jf[:, 0:1], offs[:, 8 * slot, :], ALU.mult, ALU.add),
                 reads=['cref', 'offs', 'jfs'], writes=['cref'])
            P.op('dve', lambda e: e.tensor_scalar(cref[:], cref[:], -1.0, None, ALU.mult), reads=['cref'], writes=['cref'])
            for qb in range(4):
                qi = slot * 4 + qb
                ai = nxt('adm', 2)
                P.dma('sp', adm[ai][:], adm_d[qb], writes=[('adm', ai)], key=('adm', ai))
                for sc in range(n // 512):
                    cs = slice(sc * 512, (sc + 1) * 512)
                    for h in range(4):
                        g = gbank()
                        hb = (h % 2) * 64
                        P.mm(ps[:, g, :], iqc[hb:hb + 64, h // 2, qb * 128:(qb + 1) * 128], ik2[hb:hb + 64, cs], True, True,
                             reads=['iqc', 'ik2s'], writes=[('ps', g)])
                        ri = nxt('rl', 3)
                        P.op('act', lambda e, g=g, ri=ri: e.activation(rl[ri][:], ps[:, g, :], AF.Relu), reads=[('ps', g)], writes=[('rl', ri)])
                        if h == 0:
                            P.op('dve', lambda e, ri=ri, cs=cs, qi=qi: e.tensor_scalar(Sc[:, cs], rl[ri][:], iw[:, qi, 0:1], None, ALU.mult),
                                 reads=[('rl', ri), 'iws'], writes=[('Sc', sc)])
                        else:
                            P.op('dve', lambda e, ri=ri, cs=cs, qi=qi, h=h: e.scalar_tensor_tensor(Sc[:, cs], rl[ri][:], iw[:, qi, h:h + 1], Sc[:, cs], ALU.mult, ALU.add),
                                 reads=[('rl', ri), 'iws', ('Sc', sc)], writes=[('Sc', sc)])
                scall = [('Sc', sc) for sc in range(n // 512)]
                P.op('dve', lambda e, n=n: e.tensor_reduce(sm[:, 0:1], Sc[:, 0:n], AX.X, ALU.max, apply_absolute_value=True),
                     reads=scall, writes=['sm0'])
                P.op('dve', lambda e: e.tensor_scalar(sm[:, 1:2], sm[:, 0:1], -1.0, -1.0, ALU.mult, ALU.add), reads=['sm0'], writes=['lo'])
                P.op('dve', lambda e: e.tensor_scalar(sm[:, 2:3], sm[:, 0:1], 2.0, 2.0, ALU.mult, ALU.add), reads=['sm0'], writes=['w0'])
                P.op('dve', lambda e: e.tensor_scalar(dk[:], pw[:], sm[:, 2:3], None, ALU.mult), reads=['w0', 'pws'], writes=['dk'])
                P.op('dve', lambda e, n=n, ai=ai: e.tensor_tensor(Sc[:, n - 1024:n], Sc[:, n - 1024:n], adm[ai][:], ALU.add),
                     reads=[('adm', ai)] + scall[-2:], writes=scall[-2:])
                for k in range(NBIS):
                    P.op('dve', lambda e, k=k: e.tensor_tensor(sm[:, 3:4], sm[:, 1:2], dk[:, k:k + 1], ALU.add), reads=['lo', 'dk'], writes=['mid'])
                    P.op('dve', lambda e, n=n: e.tensor_scalar(Mk[:, 0:n], Sc[:, 0:n], sm[:, 3:4], None, ALU.is_ge, ALU.add, accum_out=sm[:, 4:5]),
                         reads=scall + ['mid'], writes=['Mk', 'cnt'])
                    P.op('dve', lambda e, k=k: e.scalar_tensor_tensor(sm[:, 5:6], sm[:, 4:5], TOPK, dk[:, k:k + 1], ALU.is_ge, ALU.mult),
                         reads=['cnt', 'dk'], writes=['stp'])
                    P.op('dve', lambda e: e.tensor_tensor(sm[:, 1:2], sm[:, 1:2], sm[:, 5:6], ALU.add), reads=['lo', 'stp'], writes=['lo'])
                P.op('dve', lambda e, n=n: e.tensor_scalar(Mk[:, 0:n], Sc[:, 0:n], sm[:, 1:2], None, ALU.is_ge),
                     reads=scall + ['lo'], writes=['Mk'])
                pst = ps[:, 6, :].bitcast(BF16)
                for k0 in range(0, nblk, 4):
                    for j in range(4):
                        P.tr(pst[:, j * 128:(j + 1) * 128], Mk[:, (k0 + j) * 128:(k0 + j + 1) * 128], identb[:],
                             reads=['Mk', 'identbs'], writes=[('ps', 6)])
                    P.op('act', lambda e, k0=k0, qb=qb: e.copy(MT[:, k0:k0 + 4, qb * 128:(qb + 1) * 128],
                                                              pst[:, 0:512].rearrange("p (j t) -> p j t", j=4)),
                         reads=[('ps', 6)], writes=['MT'])
            run_jobs(attention(slot, 'a') + attention(slot, 'b'))

    return dict(oaT=oa_d, obT=ob_d)


def _slot_cols(j):
    return np.concatenate([np.arange((2 * i + j) * 512, (2 * i + j + 1) * 512) for i in range(NSLOT)])


def k2a_consts(j):
    p = np.arange(128)
    col = np.arange(1024)
    adm = np.zeros((4, 128, 1024), np.float32)
    for qb in range(4):
        lim = j * 512 + qb * 128 + (p // 64) * 64 + 64
        adm[qb] = np.where(col[None, :] < lim[:, None], 0.0, NEG)
    kbl = np.arange(8)
    q = np.arange(512)
    cm = ((kbl[None, :, None] * 128 + p[:, None, None]) <= (j * 512 + q[None, None, :])).astype(np.float32).astype(NPBF)
    tri = (p[:, None] <= p[None, :]).astype(np.float32)
    pw = np.tile((2.0 ** -(np.arange(NBIS, dtype=np.float32) + 1))[None, :], (128, 1)).astype(np.float32)
    return dict(adm=adm, cm=np.ascontiguousarray(cm), jflag=np.full((128, 1), float(j), np.float32), tri=tri,
                identb=np.eye(128, dtype=np.float32).astype(NPBF), pw2=pw)


def _vaug(v_full):
    S = v_full.shape[0]
    va = np.ones((S, 8, 65), dtype=v_full.dtype)
    va[:, :, :64] = v_full.reshape(S, 8, 64)
    return np.ascontiguousarray(va.reshape(S // 128, 128, 4, 130).transpose(2, 1, 0, 3))


def k2a_inputs(j, aqT, akT, av, fqT, fkT, fv, iqT, ikT, small):
    cols = _slot_cols(j)
    S = SEQ
    d = dict(
        aq=np.ascontiguousarray(aqT.reshape(4, 128, S)[:, :, cols]),
        ak=np.ascontiguousarray(akT.reshape(4, 128, S)),
        av=_vaug(av),
        fq=np.ascontiguousarray(fqT.reshape(4, 128, S)[:, :, cols]),
        fk=np.ascontiguousarray(fkT.reshape(4, 128, S)),
        fv=_vaug(fv),
        iq=np.ascontiguousarray(iqT.reshape(2, 128, S)[:, :, cols].transpose(1, 0, 2)),
        ik2=np.ascontiguousarray(np.concatenate([ikT, ikT], axis=0)),
        iw=np.ascontiguousarray(small[cols, 0:4].reshape(NSLOT * 4, 128, 4).transpose(1, 0, 2)),
        lf=np.ascontiguousarray(small[:, 4:12].reshape(64, 128, 8).transpose(1, 0, 2)),
    )
    d.update(k2a_consts(j))
    return d


def build_k2b():
    nc = bass.Bass("TRN2", target_bir_lowering=False)
    P = Prog(nc)
    phase_k2b(nc, P, '')
    P.emit(final_keys=['out'])
    return nc, P


def phase_k2b(nc, P, px, ext=None):
    ext = ext or {}
    S = SEQ
    NCH = S // 64
    GRP = 16
    din = lambda name, shape, dt: ext[name] if name in ext else nc.dram_tensor(px + name, shape, dt, kind="ExternalInput").ap()
    qp_d = din("qpT", [64, 4, S], BF16)
    kpT_d = din("kpT", [64, 4, S], BF16)
    kp_d = din("kp", [64, NCH, 256], BF16)
    v_d = din("v", [64, NCH, 256], BF16)
    em_d = din("em", [64, 4, NCH], F32)
    g_d = din("g", [64, 4, NCH], F32)
    triu_d = din("triu", [64, 64], F32)
    oc_d = nc.dram_tensor(px + "ocT", [64, 4, S], BF16, kind="ExternalOutput").ap()

    from contextlib import ExitStack
    with ExitStack() as st:
        def sb(name, shape, dt):
            return st.enter_context(nc.sbuf_tensor(px + name, shape, dt))
        qp = [sb("qp%d" % i, [64, 4, GRP * 64], BF16) for i in range(2)]
        kpT = [sb("kpT%d" % i, [64, 4, GRP * 64], BF16) for i in range(2)]
        kp = [sb("kp%d" % i, [64, GRP, 256], BF16) for i in range(2)]
        vv = [sb("vv%d" % i, [64, GRP, 256], BF16) for i in range(2)]
        ocb = [sb("ocb%d" % i, [64, 4, GRP * 64], BF16) for i in range(2)]
        em = sb("ems", [64, 4, NCH], F32)
        gg = sb("ggs", [64, 4, NCH], F32)
        triu = sb("trius", [64, 64], F32)
        Sm = sb("Sm", [64, 4, 64], F32)
        Smb = sb("Smb", [64, 4, 64], BF16)
        tmp = sb("tmp", [64, 4, 64], F32)
        scm = [sb("scm%d" % i, [64, 4, 64], BF16) for i in range(2)]
        ps = st.enter_context(nc.psum_tensor(px + "ps", [128, 8, 512], F32))
        cnt = {}

        def nxt(k, n):
            v = cnt.get(k, 0) % n
            cnt[k] = cnt.get(k, 0) + 1
            return v

        P.dma('sp', em[:], em_d, writes=['em'], key='c0')
        P.dma('sp', gg[:], g_d, writes=['gg'], key='c1')
        P.dma('sp', triu[:], triu_d, writes=['triu'], key='c2')
        P.op('dve', lambda e: e.tensor_tensor(gg[:, :, 0:NCH - 1], gg[:, :, 0:NCH - 1], em[:, :, 1:NCH], ALU.mult), reads=['em', 'gg'], writes=['gg'])
        P.op('dve', lambda e: e.memset(Sm[:], 0.0), writes=['Sm'])
        P.op('dve', lambda e: e.memset(Smb[:], 0.0), writes=['Smb'])

        ngrp = NCH // GRP

        def load(gi):
            b = gi % 2
            ts_ = slice(gi * GRP * 64, (gi + 1) * GRP * 64)
            cs_ = slice(gi * GRP, (gi + 1) * GRP)
            P.dma('sp', qp[b][:], qp_d[:, :, ts_], writes=[('qp', b)], key=('qp', b))
            P.dma('sp', kpT[b][:], kpT_d[:, :, ts_], writes=[('kpT', b)], key=('kpT', b))
            P.dma('sp', kp[b][:], kp_d[:, cs_, :], writes=[('kp', b)], key=('kp', b))
            P.dma('sp', vv[b][:], v_d[:, cs_, :], writes=[('vv', b)], key=('vv', b))

        load(0)
        for gi in range(ngrp):
            if gi + 1 < ngrp:
                load(gi + 1)
            b = gi % 2
            for cl in range(GRP):
                c = gi * GRP + cl
                tsl = slice(cl * 64, (cl + 1) * 64)
                gs = nxt('sc', 2)
                for hl in range(4):
                    P.mm(ps[0:64, gs, hl * 64:(hl + 1) * 64], kpT[b][:, hl, tsl], qp[b][:, hl, tsl], True, True,
                         reads=[('kpT', b), ('qp', b)], writes=[('ps', gs)])
                si = nxt('scm', 2)
                P.op('dve', lambda e, gs=gs, si=si: e.tensor_tensor(scm[si][:], ps[0:64, gs, 0:256].rearrange("p (h t) -> p h t", h=4),
                                                                  triu[:].unsqueeze(1).to_broadcast([64, 4, 64]), ALU.mult),
                     reads=[('ps', gs), 'triu'], writes=[('scm', si)])
                go = 2 + nxt('o', 2)
                for hl in range(4):
                    P.mm(ps[0:64, go, hl * 64:(hl + 1) * 64], vv[b][:, cl, hl * 64:(hl + 1) * 64], scm[si][:, hl, :], True, False,
                         reads=[('vv', b), ('scm', si)], writes=[('ps', go)])
                    P.mm(ps[0:64, go, hl * 64:(hl + 1) * 64], Smb[:, hl, :], qp[b][:, hl, tsl], False, True,
                         reads=['Smb', ('qp', b)], writes=[('ps', go)])
                P.op('act', lambda e, go=go, b=b, tsl=tsl: e.copy(ocb[b][:, :, tsl], ps[0:64, go, 0:256].rearrange("p (h t) -> p h t", h=4)),
                     reads=[('ps', go)], writes=[('ocb', b)])
                gk = 4 + nxt('kv', 2)
                for hl in range(4):
                    P.mm(ps[0:64, gk, hl * 64:(hl + 1) * 64], kp[b][:, cl, hl * 64:(hl + 1) * 64], vv[b][:, cl, hl * 64:(hl + 1) * 64], True, True,
                         reads=[('kp', b), ('vv', b)], writes=[('ps', gk)])
                if c < NCH - 1:
                    P.op('dve', lambda e, gk=gk: e.tensor_tensor(tmp[:], ps[0:64, gk, 0:256].rearrange("p (h t) -> p h t", h=4), Sm[:], ALU.add),
                         reads=[('ps', gk), 'Sm'], writes=['tmp'])
                    P.op('dve', lambda e, c=c: e.tensor_tensor(Sm[:], tmp[:], gg[:, :, c:c + 1].to_broadcast([64, 4, 64]), ALU.mult),
                         reads=['tmp', 'gg'], writes=['Sm'])
                    P.op('dve', lambda e, c=c: e.tensor_tensor(Smb[:], tmp[:], gg[:, :, c:c + 1].to_broadcast([64, 4, 64]), ALU.mult),
                         reads=['tmp', 'gg'], writes=['Smb'])
            P.dma('sp', oc_d[:, :, gi * GRP * 64:(gi + 1) * GRP * 64], ocb[b][:], reads=[('ocb', b)], key='out')
    return dict(ocT=oc_d)


def k2b_inputs(j, qpT, kpT, kp, cv, em, g):
    S = SEQ
    hs = slice(4 * j, 4 * j + 4)
    cs = slice(256 * j, 256 * j + 256)
    return dict(
        qpT=np.ascontiguousarray(qpT.reshape(8, 64, S)[hs].transpose(1, 0, 2)),
        kpT=np.ascontiguousarray(kpT.reshape(8, 64, S)[hs].transpose(1, 0, 2)),
        kp=np.ascontiguousarray(kp[:, cs].reshape(S // 64, 64, 256).transpose(1, 0, 2)),
        v=np.ascontiguousarray(cv[:, cs].reshape(S // 64, 64, 256).transpose(1, 0, 2)),
        em=np.ascontiguousarray(em.reshape(8, 64, S // 64)[hs].transpose(1, 0, 2)).astype(np.float32),
        g=np.ascontiguousarray(g.reshape(8, 64, S // 64)[hs].transpose(1, 0, 2)).astype(np.float32),
        triu=np.triu(np.ones((64, 64), np.float32)),
    )


def emit_layernorm(P, y, out, lng, lnb, stats, mv, rstd, ykey, okey, gkeys, tag):
    for c in range(2):
        P.op('dve', lambda e, c=c: e.bn_stats(stats[:, c, :], y[:, c * 512:(c + 1) * 512]), reads=[ykey], writes=[(tag, 'st', c)])
    P.op('dve', lambda e: e.bn_aggr(mv[:], stats[:].rearrange("p c s -> p (c s)")), reads=[(tag, 'st', 0), (tag, 'st', 1)], writes=[(tag, 'mv')])
    P.op('act', lambda e: e.activation(rstd[:], mv[:, 1:2], AF.Sqrt, bias=1e-5), reads=[(tag, 'mv')], writes=[(tag, 'rs')])
    P.op('dve', lambda e: e.reciprocal(rstd[:], rstd[:]), reads=[(tag, 'rs')], writes=[(tag, 'rs')])
    P.op('dve', lambda e: e.tensor_scalar(out, y, mv[:, 0:1], rstd[:, 0:1], ALU.subtract, ALU.mult), reads=[ykey, (tag, 'mv'), (tag, 'rs')], writes=[okey])
    P.op('pool', lambda e: e.tensor_tensor(out, out, lng, ALU.mult), reads=[okey, gkeys[0]], writes=[okey])
    P.op('pool', lambda e: e.tensor_tensor(out, out, lnb, ALU.add), reads=[okey, gkeys[1]], writes=[okey])


def build_k3a():
    nc = bass.Bass("TRN2", target_bir_lowering=False)
    P = Prog(nc)
    phase_k3a(nc, P, '')
    P.emit(final_keys=['out'])
    return nc, P


def phase_k3a(nc, P, px, ext=None):
    ext = ext or {}
    T = TOK
    din = lambda name, shape, dt: ext[name] if name in ext else nc.dram_tensor(px + name, shape, dt, kind="ExternalInput").ap()
    oa_d = din("oaT", [64, 8, T], BF16)
    ob_d = din("obT", [64, 8, T], BF16)
    oc_d = din("ocT", [64, 8, T], BF16)
    cg_d = din("cgT", [64, 8, T], BF16)
    gt_d = din("gT", [128, 24, T], BF16)
    x_d = din("x", [T, D_MODEL], F32)
    wp_d = [din("wp%d" % i, [64, 8, D_MODEL], F32) for i in range(3)]
    wo_d = din("wo", [128, 8, D_MODEL], F32)
    gn_d = din("gn", [64, 8], F32)
    lng_d = din("lng", [1, D_MODEL], F32)
    lnb_d = din("lnb", [1, D_MODEL], F32)
    h_d = nc.dram_tensor(px + "h", [T, D_MODEL], F32, kind="ExternalOutput").ap()

    from contextlib import ExitStack
    with ExitStack() as st:
        def sb(name, shape, dt):
            return st.enter_context(nc.sbuf_tensor(px + name, shape, dt))
        wst = sb("wst", [128, 8, 512], F32)
        wp = [sb("wpb%d" % i, [64, 8, D_MODEL], BF16) for i in range(3)]
        wo = sb("wob", [128, 8, D_MODEL], BF16)
        gn = sb("gns", [64, 8], F32)
        lng = sb("lngs", [128, D_MODEL], F32)
        lnb = sb("lnbs", [128, D_MODEL], F32)
        on64 = sb("on64", [64, 64], BF16)
        oT = [[sb("oT%d_%d" % (br, i), [64, 8, 512], BF16) for i in range(1)] for br in range(3)]
        cg = [sb("cg%d" % i, [64, 8, 512], BF16) for i in range(1)]
        gt = [sb("gt%d" % i, [128, 3, 512], BF16) for i in range(2)]
        mT = sb("mT", [128, 8, 512], BF16)
        sq = [sb("sq%d" % i, [64, 512], BF16) for i in range(2)]
        rs = [sb("rs%d" % i, [64, 512], F32) for i in range(2)]
        tA = [sb("tA%d" % i, [128, 512], F32) for i in range(2)]
        tB = [sb("tB%d" % i, [128, 512], F32) for i in range(2)]
        tC = [sb("tC%d" % i, [128, 512], F32) for i in range(2)]
        xin = [sb("xin%d" % i, [128, D_MODEL], F32) for i in range(2)]
        yb = [sb("yb%d" % i, [128, D_MODEL], F32) for i in range(2)]
        hb = [sb("hb%d" % i, [128, D_MODEL], F32) for i in range(2)]
        stats = sb("stats", [128, 2, 6], F32)
        mv = sb("mv", [128, 2], F32)
        rstd = sb("rstd", [128, 1], F32)
        ps = st.enter_context(nc.psum_tensor(px + "ps", [128, 8, 512], F32))
        cnt = {}

        def nxt(k, n):
            v = cnt.get(k, 0) % n
            cnt[k] = cnt.get(k, 0) + 1
            return v

        for i in range(3):
            for hf in range(2):
                P.dma('sp', wst[0:64], wp_d[i][:, :, hf * 512:(hf + 1) * 512], writes=['wst'], key='wst')
                P.op('act', lambda e, i=i, hf=hf: e.copy(wp[i][:, :, hf * 512:(hf + 1) * 512], wst[0:64]), reads=['wst'], writes=[('wp', i)])
        for hf in range(2):
            P.dma('sp', wst[:], wo_d[:, :, hf * 512:(hf + 1) * 512], writes=['wst'], key='wst')
            P.op('act', lambda e, hf=hf: e.copy(wo[:, :, hf * 512:(hf + 1) * 512], wst[:]), reads=['wst'], writes=['wo'])
        P.dma('sp', gn[:], gn_d, writes=['gn'], key='c0')
        P.dma('sp', lng[:], lng_d.partition_broadcast(128), writes=['lng'], key='c1')
        P.dma('sp', lnb[:], lnb_d.partition_broadcast(128), writes=['lnb'], key='c2')
        P.op('pool', lambda e: e.memset(on64[:], 1.0 / 64.0), writes=['on64'])

        NTC = T // 512

        def load(tc):
            b = 0
            sl = slice(tc * 512, (tc + 1) * 512)
            for br, d in enumerate((oa_d, ob_d, oc_d)):
                P.dma('sp', oT[br][b][:], d[:, :, sl], writes=[('oT', br, b)], key=('oT', br, b))
            P.dma('sp', cg[b][:], cg_d[:, :, sl], writes=[('cg', b)], key=('cg', b))

        def load_gt(tc, mc):
            gi = nxt('gt', 2)
            sl = slice(tc * 512, (tc + 1) * 512)
            for br in range(3):
                P.dma('sp', gt[gi][:, br, :], gt_d[:, br * 8 + mc, sl], writes=[('gt', gi)], key=('gt', gi))
            return gi

        for tc in range(NTC):
            load(tc)
            b = 0
            oc = oT[2][b]
            for h in range(8):
                si = nxt('sq', 2)
                P.op('dve', lambda e, h=h, si=si: e.tensor_tensor(sq[si][:], oc[:, h, :], oc[:, h, :], ALU.mult), reads=[('oT', 2, b)], writes=[('sq', si)])
                g = nxt('g', 4)
                P.mm(ps[0:64, g, :], on64[:], sq[si][:], True, True, reads=['on64', ('sq', si)], writes=[('ps', g)])
                ri = nxt('rs', 2)
                P.op('act', lambda e, g=g, ri=ri: e.activation(rs[ri][:], ps[0:64, g, :], AF.Sqrt, bias=1e-6), reads=[('ps', g)], writes=[('rs', ri)])
                P.op('dve', lambda e, ri=ri: e.reciprocal(rs[ri][:], rs[ri][:]), reads=[('rs', ri)], writes=[('rs', ri)])
                P.op('dve', lambda e, h=h, ri=ri: e.scalar_tensor_tensor(rs[ri][:], oc[:, h, :], gn[:, h:h + 1], rs[ri][:], ALU.mult, ALU.mult),
                     reads=[('oT', 2, b), 'gn', ('rs', ri)], writes=[('rs', ri)])
                P.op('pool', lambda e, h=h, ri=ri: e.tensor_tensor(oc[:, h, :], rs[ri][:], cg[b][:, h, :], ALU.mult),
                     reads=[('rs', ri), ('cg', b)], writes=[('oT', 2, b)])
            gnext = load_gt(tc, 0)
            for mc in range(8):
                gcur = gnext
                if mc + 1 < 8:
                    gnext = load_gt(tc, mc + 1)
                gb = []
                for br in range(3):
                    g = nxt('g', 4)
                    for h in range(8):
                        P.mm(ps[:, g, :], wp[br][:, h, mc * 128:(mc + 1) * 128], oT[br][b][:, h, :], h == 0, h == 7,
                             reads=[('wp', br), ('oT', br, b)], writes=[('ps', g)])
                    gb.append(g)
                ti = nxt('t', 2)
                for br, tt in enumerate((tA, tB, tC)):
                    P.op('dve', lambda e, br=br, tt=tt, ti=ti, g=gb[br], gcur=gcur: e.tensor_tensor(tt[ti][:], ps[:, g, :], gt[gcur][:, br, :], ALU.mult),
                         reads=[('ps', gb[br]), ('gt', gcur)], writes=[('t', br, ti)])
                P.op('pool', lambda e, ti=ti: e.tensor_tensor(tA[ti][:], tA[ti][:], tB[ti][:], ALU.add), reads=[('t', 0, ti), ('t', 1, ti)], writes=[('t', 0, ti)])
                P.op('pool', lambda e, ti=ti, mc=mc: e.tensor_tensor(mT[:, mc, :], tA[ti][:], tC[ti][:], ALU.add), reads=[('t', 0, ti), ('t', 2, ti)], writes=[('mT', mc)])
            for tt in range(4):
                tix = tc * 4 + tt
                xi = nxt('x', 2)
                P.dma('sp', xin[xi][:], x_d[tix * 128:(tix + 1) * 128, :], writes=[('xin', xi)], key=('xin', xi))
                yi = nxt('y', 2)
                for half in range(2):
                    g = 4 + nxt('go', 4)
                    for k in range(8):
                        P.mm(ps[:, g, :], mT[:, k, tt * 128:(tt + 1) * 128], wo[:, k, half * 512:(half + 1) * 512], k == 0, k == 7,
                             reads=[('mT', k), 'wo'], writes=[('ps', g)])
                    P.op('dve', lambda e, g=g, xi=xi, yi=yi, half=half: e.scalar_tensor_tensor(
                        yb[yi][:, half * 512:(half + 1) * 512], xin[xi][:, half * 512:(half + 1) * 512], ALPHA, ps[:, g, :], ALU.mult, ALU.add),
                        reads=[('xin', xi), ('ps', g)], writes=[('yb', yi)])
                hi = nxt('h', 2)
                emit_layernorm(P, yb[yi][:], hb[hi][:], lng[:], lnb[:], stats, mv, rstd, ('yb', yi), ('hb', hi), ('lng', 'lnb'), 'ln')
                P.dma('sp', h_d[tix * 128:(tix + 1) * 128, :], hb[hi][:], reads=[('hb', hi)], key='out')
    return dict(h=h_d)


def _fm64(a_T):
    return np.ascontiguousarray(a_T.reshape(8, 64, -1).transpose(1, 0, 2))


def k3a_inputs(oaT, obT, ocT, cgT, gT, x, wpa, wpb, wpc, wo, gn, lng, lnb):
    T = TOK
    f = lambda w: np.ascontiguousarray(w.reshape(8, 64, D_MODEL).transpose(1, 0, 2)).astype(np.float32)
    return dict(oaT=oaT, obT=obT, ocT=ocT, cgT=_fm64(cgT),
                gT=np.ascontiguousarray(gT.reshape(24, 128, T).transpose(1, 0, 2)),
                x=np.ascontiguousarray(x), wp0=f(wpa), wp1=f(wpb), wp2=f(wpc),
                wo=np.ascontiguousarray(wo.reshape(8, 128, D_MODEL).transpose(1, 0, 2)).astype(np.float32),
                gn=np.ascontiguousarray(gn.reshape(8, 64).T).astype(np.float32),
                lng=np.ascontiguousarray(lng.reshape(1, -1)).astype(np.float32),
                lnb=np.ascontiguousarray(lnb.reshape(1, -1)).astype(np.float32))


def build_k3b(e0=0, ne=N_EXP, first=True, last=True):
    nc = bass.Bass("TRN2", target_bir_lowering=False)
    P = Prog(nc)
    phase_k3b(nc, P, '', e0, ne, first, last)
    P.emit(final_keys=['out'])
    return nc, P


def phase_k3b(nc, P, px, e0=0, ne=N_EXP, first=True, last=True, ext=None):
    ext = ext or {}
    T = TOK
    QC = 1024
    NQ = T // QC
    din = lambda name, shape, dt: ext[name] if name in ext else nc.dram_tensor(px + name, shape, dt, kind="ExternalInput").ap()
    h_d = din("h", [T, D_MODEL], F32)
    wr_d = din("wr", [128, 8, N_EXP], F32)
    br_d = din("br", [1, N_EXP], F32)
    wgu_d = din("wgu", [ne, D_MODEL, 2 * D_MODEL], F32)
    bgu_d = din("bgu", [128, N_EXP, 16], F32)
    wd_d = din("wd", [ne, D_MODEL, D_MODEL], F32)
    accin_d = None if first else din("acc_in", [T, D_MODEL], F32)
    bd_d = din("bd", [N_EXP, D_MODEL], F32)
    lng_d = din("lng", [1, D_MODEL], F32)
    lnb_d = din("lnb", [1, D_MODEL], F32)
    ident_d = din("ident", [128, 128], F32)
    out_d = nc.dram_tensor(px + ("out" if last else "acc_out"), [T, D_MODEL], F32, kind="ExternalOutput").ap()

    from contextlib import ExitStack
    with ExitStack() as st:
        def sb(name, shape, dt):
            return st.enter_context(nc.sbuf_tensor(px + name, shape, dt))
        hTc = sb("hTc", [128, 8, QC], BF16)
        acc = sb("acc", [128, QC // 128, D_MODEL], F32)
        actT = sb("actT", [128, 8, QC], BF16)
        gst = [sb("gst%d" % i, [128, 8, 512], F32) for i in range(2)]
        gbf = [sb("gbf%d" % i, [128, 8, 512], BF16) for i in range(2)]
        dst = gst
        dbf = [sb("dbf%d" % i, [128, 8, D_MODEL], BF16) for i in range(2)]
        big = [sb("big%d" % i, [128, D_MODEL], F32) for i in range(4)]
        hT32 = sb("hT32", [128, 8, 128], F32)
        wr = sb("wrs", [128, 8, N_EXP], F32)
        brb = sb("brb", [128, N_EXP], F32)
        bgu = sb("bgus", [128, N_EXP, 16], F32)
        bd = sb("bds", [N_EXP, D_MODEL], F32)
        lng = sb("lngs", [128, D_MODEL], F32)
        lnb = sb("lnbs", [128, D_MODEL], F32)
        ident = sb("idents", [128, 128], F32)
        Gc = sb("Gc", [128, QC // 128, N_EXP], F32)
        GTc = sb("GTc", [N_EXP, QC], F32)
        lg = sb("lg", [128, N_EXP], F32)
        ex = sb("ex", [128, N_EXP], F32)
        m8 = sb("m8", [128, 8], F32)
        sm = sb("sm", [128, 4], F32)
        tG = [sb("tG%d" % i, [128, 512], F32) for i in range(2)]
        tS = [sb("tS%d" % i, [128, 512], F32) for i in range(2)]
        tU = [sb("tU%d" % i, [128, 512], F32) for i in range(2)]
        stats = sb("stats", [128, 2, 6], F32)
        mv = sb("mv", [128, 2], F32)
        rstd = sb("rstd", [128, 1], F32)
        ps = st.enter_context(nc.psum_tensor(px + "ps", [128, 8, 512], F32))
        cnt = {}

        def nxt(k, n):
            v = cnt.get(k, 0) % n
            cnt[k] = cnt.get(k, 0) + 1
            return v

        for i, (t, d, kn) in enumerate([(wr, wr_d, 'wrs'), (bgu, bgu_d, 'bgus'), (bd, bd_d, 'bds'), (ident, ident_d, 'idents')]):
            P.dma('sp', t[:], d, writes=[kn], key='c%d' % i)
        P.dma('sp', brb[:], br_d.partition_broadcast(128), writes=['brb'], key='c4')
        P.dma('sp', lng[:], lng_d.partition_broadcast(128), writes=['lng'], key='c5')
        P.dma('sp', lnb[:], lnb_d.partition_broadcast(128), writes=['lnb'], key='c6')

        wgu_v = wgu_d.rearrange("e (c p) n -> e p c n", p=128)
        wd_v = wd_d.rearrange("e (c p) n -> e p c n", p=128)

        def gu_unit(q, e, f2):
            def load():
                b = nxt('gw', 2)
                s_ = nxt('ds', 2)
                P.dma('sp', gst[s_][:, :, 0:256], wgu_v[e - e0, :, :, f2 * 256:(f2 + 1) * 256], writes=[('gst', s_)], key=('gst', s_))
                P.dma('sp', gst[s_][:, :, 256:512], wgu_v[e - e0, :, :, 1024 + f2 * 256:1024 + (f2 + 1) * 256], writes=[('gst', s_)], key=('gst', s_))
                P.op('act', lambda e_: e_.copy(gbf[b][:], gst[s_][:]), reads=[('gst', s_)], writes=[('gbf', b)])
                return b

            def comp(b):
                for fl in range(2):
                    fc = f2 * 2 + fl
                    for tcc in range(QC // 512):
                        tsl = slice(tcc * 512, (tcc + 1) * 512)
                        g1 = nxt('g', 4)
                        for k in range(8):
                            P.mm(ps[:, g1, :], gbf[b][:, k, fl * 128:(fl + 1) * 128], hTc[:, k, tsl], k == 0, k == 7,
                                 reads=[('gbf', b), 'hTc'], writes=[('ps', g1)])
                        g2 = nxt('g', 4)
                        for k in range(8):
                            P.mm(ps[:, g2, :], gbf[b][:, k, 256 + fl * 128:256 + (fl + 1) * 128], hTc[:, k, tsl], k == 0, k == 7,
                                 reads=[('gbf', b), 'hTc'], writes=[('ps', g2)])
                        ti = nxt('t', 2)
                        P.op('dve', lambda e_, g1=g1, ti=ti, fc=fc: e_.tensor_scalar(tG[ti][:], ps[:, g1, :], bgu[:, e, fc:fc + 1], 7.0, ALU.add, ALU.min),
                             reads=[('ps', g1), 'bgus'], writes=[('tG', ti)])
                        P.op('act', lambda e_, ti=ti: e_.activation(tS[ti][:], tG[ti][:], AF.Sigmoid, scale=1.702), reads=[('tG', ti)], writes=[('tS', ti)])
                        P.op('dve', lambda e_, g2=g2, ti=ti, fc=fc: e_.tensor_scalar(tU[ti][:], ps[:, g2, :], bgu[:, e, 8 + fc:9 + fc], 7.0, ALU.add, ALU.min),
                             reads=[('ps', g2), 'bgus'], writes=[('tU', ti)])
                        P.op('dve', lambda e_, ti=ti: e_.tensor_scalar(tU[ti][:], tU[ti][:], -7.0, 1.0, ALU.max, ALU.add), reads=[('tU', ti)], writes=[('tU', ti)])
                        P.op('pool', lambda e_, ti=ti: e_.tensor_tensor(tG[ti][:], tG[ti][:], tS[ti][:], ALU.mult), reads=[('tG', ti), ('tS', ti)], writes=[('tG', ti)])
                        P.op('pool', lambda e_, ti=ti, fc=fc, tsl=tsl: e_.tensor_tensor(actT[:, fc, tsl], tG[ti][:], tU[ti][:], ALU.mult),
                             reads=[('tG', ti), ('tU', ti)], writes=[('actT', fc)])
            return load, comp

        def down_unit(q, e):
            def load():
                b = nxt('dw', 2)
                for half in range(2):
                    s_ = nxt('ds', 2)
                    P.dma('sp', dst[s_][:], wd_v[e - e0, :, :, half * 512:(half + 1) * 512], writes=[('gst', s_)], key=('gst', s_))
                    P.op('act', lambda e_, s_=s_, half=half: e_.copy(dbf[b][:, :, half * 512:(half + 1) * 512], dst[s_][:]), reads=[('gst', s_)], writes=[('dbf', b)])
                return b

            def comp(b):
                for tt in range(QC // 128):
                    for half in range(2):
                        g = 4 + nxt('gd', 4)
                        for fc in range(8):
                            P.mm(ps[:, g, :], actT[:, fc, tt * 128:(tt + 1) * 128], dbf[b][:, fc, half * 512:(half + 1) * 512], fc == 0, fc == 7,
                                 reads=[('actT', fc), ('dbf', b)], writes=[('ps', g)])
                        P.op('dve', lambda e_, g=g, tt=tt, half=half: e_.scalar_tensor_tensor(
                            acc[:, tt, half * 512:(half + 1) * 512], ps[:, g, :], Gc[:, tt, e:e + 1], acc[:, tt, half * 512:(half + 1) * 512], ALU.mult, ALU.add),
                            reads=[('ps', g), 'Gc', ('acc', tt)], writes=[('acc', tt)])
            return load, comp

        units = []
        for q in range(NQ):
            for e in range(e0, e0 + ne):
                for f2 in range(4):
                    units.append(('gu', q, e, f2) + gu_unit(q, e, f2))
                units.append(('dn', q, e, None) + down_unit(q, e))

        def prologue(q):
            for tt in range(QC // 128):
                tix = q * (QC // 128) + tt
                bi = nxt('big', 4)
                P.dma('sp', big[bi][:], h_d[tix * 128:(tix + 1) * 128, :], writes=[('big', bi)], key=('big', bi))
                for half in range(2):
                    g = nxt('g', 4)
                    for j in range(4):
                        k = half * 4 + j
                        P.tr(ps[:, g, j * 128:(j + 1) * 128], big[bi][:, k * 128:(k + 1) * 128], ident[:], reads=[('big', bi), 'idents'], writes=[('ps', g)])
                    P.op('act', lambda e_, g=g, half=half: e_.copy(hT32[:, half * 4:(half + 1) * 4, :], ps[:, g, :].rearrange("p (j t) -> p j t", j=4)),
                         reads=[('ps', g)], writes=[('hT32', half)])
                    P.op('dve', lambda e_, half=half, tt=tt: e_.tensor_copy(hTc[:, half * 4:(half + 1) * 4, tt * 128:(tt + 1) * 128],
                                                                          hT32[:, half * 4:(half + 1) * 4, :]),
                         reads=[('hT32', half)], writes=['hTc'])
                g = nxt('g', 4)
                for k in range(8):
                    P.mm(ps[:, g, 0:N_EXP], hT32[:, k, :], wr[:, k, :], k == 0, k == 7, reads=[('hT32', k // 4), 'wrs'], writes=[('ps', g)])
                P.op('dve', lambda e_, g=g: e_.tensor_tensor(lg[:], ps[:, g, 0:N_EXP], brb[:], ALU.add), reads=[('ps', g), 'brb'], writes=['lg'])
                P.op('dve', lambda e_: e_.max(m8[:], lg[:]), reads=['lg'], writes=['m8'])
                P.op('dve', lambda e_: e_.tensor_scalar(sm[:, 0:1], m8[:, 0:1], -1.0, None, ALU.mult), reads=['m8'], writes=['sm0'])
                P.op('act', lambda e_: e_.activation(ex[:], lg[:], AF.Exp, bias=sm[:, 0:1]), reads=['lg', 'sm0'], writes=['ex'])
                P.op('dve', lambda e_: e_.scalar_tensor_tensor(ex[:], lg[:], m8[:, 3:4], ex[:], ALU.is_ge, ALU.mult), reads=['lg', 'm8', 'ex'], writes=['ex'])
                P.op('dve', lambda e_: e_.reduce_sum(sm[:, 1:2], ex[:], AX.X), reads=['ex'], writes=['sm1'])
                P.op('dve', lambda e_: e_.reciprocal(sm[:, 1:2], sm[:, 1:2]), reads=['sm1'], writes=['sm1'])
                P.op('dve', lambda e_, tt=tt: e_.tensor_scalar(Gc[:, tt, :], ex[:], sm[:, 1:2], None, ALU.mult), reads=['ex', 'sm1'], writes=['Gc'])
                g = nxt('g', 4)
                P.tr(ps[0:N_EXP, g, 0:128], Gc[:, tt, :], ident[:], reads=['Gc', 'idents'], writes=[('ps', g)])
                P.op('act', lambda e_, g=g, tt=tt: e_.copy(GTc[:, tt * 128:(tt + 1) * 128], ps[0:N_EXP, g, 0:128]), reads=[('ps', g)], writes=['GTc'])
                if first:
                    for half in range(2):
                        g = 4 + nxt('gd', 4)
                        P.mm(ps[:, g, :], GTc[:, tt * 128:(tt + 1) * 128], bd[:, half * 512:(half + 1) * 512], True, True, reads=['GTc', 'bds'], writes=[('ps', g)])
                        P.op('act', lambda e_, g=g, tt=tt, half=half: e_.copy(acc[:, tt, half * 512:(half + 1) * 512], ps[:, g, :]), reads=[('ps', g)], writes=[('acc', tt)])
                else:
                    P.dma('sp', acc[:, tt, :], accin_d[tix * 128:(tix + 1) * 128, :], writes=[('acc', tt)], key=('accin', tt))

        def epilogue(q):
            for tt in range(QC // 128):
                tix = q * (QC // 128) + tt
                if not last:
                    P.dma('sp', out_d[tix * 128:(tix + 1) * 128, :], acc[:, tt, :], reads=[('acc', tt)], key='out')
                    continue
                bi = nxt('big', 4)
                P.dma('sp', big[bi][:], h_d[tix * 128:(tix + 1) * 128, :], writes=[('big', bi)], key=('big', bi))
                yi = nxt('big', 4)
                P.op('dve', lambda e_, bi=bi, yi=yi, tt=tt: e_.scalar_tensor_tensor(big[yi][:], big[bi][:], ALPHA, acc[:, tt, :], ALU.mult, ALU.add),
                     reads=[('big', bi), ('acc', tt)], writes=[('big', yi)])
                oi = nxt('big', 4)
                emit_layernorm(P, big[yi][:], big[oi][:], lng[:], lnb[:], stats, mv, rstd, ('big', yi), ('big', oi), ('lng', 'lnb'), 'ln')
                P.dma('sp', out_d[tix * 128:(tix + 1) * 128, :], big[oi][:], reads=[('big', oi)], key='out')

        nb = units[0][4]()
        for ui in range(len(units)):
            kind, q, e, f2, load, comp = units[ui]
            cur = nb
            if kind == 'gu' and e == e0 and f2 == 0:
                prologue(q)
            if ui + 1 < len(units):
                nb = units[ui + 1][4]()
            comp(cur)
            if kind == 'dn' and e == e0 + ne - 1:
                epilogue(q)
    return dict(out=out_d)


def k3b_inputs(h, wr, br, wgu, bgu, wd, bd, lng, lnb, acc_in=None):
    d = _k3b_inputs(h, wr, br, wgu, bgu, wd, bd, lng, lnb)
    if acc_in is not None:
        d['acc_in'] = np.ascontiguousarray(acc_in)
    return d


def _k3b_inputs(h, wr, br, wgu, bgu, wd, bd, lng, lnb):
    return dict(h=np.ascontiguousarray(h), wr=np.ascontiguousarray(wr.reshape(8, 128, N_EXP).transpose(1, 0, 2)).astype(np.float32),
                br=np.ascontiguousarray(br.reshape(1, -1)).astype(np.float32), wgu=wgu,
                bgu=np.ascontiguousarray(bgu.reshape(N_EXP, 16, 128).transpose(2, 0, 1)).astype(np.float32),
                wd=wd, bd=np.ascontiguousarray(bd).astype(np.float32),
                lng=np.ascontiguousarray(lng.reshape(1, -1)).astype(np.float32), lnb=np.ascontiguousarray(lnb.reshape(1, -1)).astype(np.float32),
                ident=np.eye(128, dtype=np.float32))


def build_l2():
    nc = bass.Bass("TRN2", target_bir_lowering=False)
    P = Prog(nc)
    phase_k2a(nc, P, 'a_')
    P.barrier()
    phase_k2b(nc, P, 'b_')
    P.emit(final_keys=['out'])
    return nc, P


def build_l3(next_layer=None):
    nc = bass.Bass("TRN2", target_bir_lowering=False)
    P = Prog(nc)
    o3a = phase_k3a(nc, P, 'a_')
    P.barrier()
    o3b = phase_k3b(nc, P, 'b_', 0, N_EXP, True, True, ext={'h': o3a['h']})
    if next_layer is not None:
        P.barrier()
        phase_k1(nc, P, 'k_', next_layer, ext={'x': o3b['out']})
    P.emit(final_keys=['out'])
    return nc, P


def _launch(nc, in_maps):
    res = run_bass_kernel_spmd(nc, in_maps, core_ids=list(range(NCORES)))
    return res.results


def _pref(px, d):
    return {px + k: v for k, v in d.items()}


def kernel(x, w_in, b_fox_f, hgrn_lb_logits, hgrn_norm_g, w_branch_a, w_branch_b, w_branch_c, w_out,
           ln1_g, ln1_b, w_router, b_router, w_gu, b_gu, w_down, b_down, ln2_g, ln2_b):
    f32 = lambda a: np.ascontiguousarray(np.asarray(a, dtype=np.float32))
    x_cur = f32(x).reshape(-1, D_MODEL)
    lbl = f32(hgrn_lb_logits)
    w_perm = np.ascontiguousarray(f32(w_in[0])[:, K1_COLS])
    r1 = _launch(build_k1(0)[0], [k1_inputs(c, x_cur[c * TOK:(c + 1) * TOK], w_perm, f32(b_fox_f[0]), lbl) for c in range(NCORES)])
    del w_perm
    for l in range(DEPTH):
        catT = lambda nm, b: np.concatenate([r1[2 * b][nm], r1[2 * b + 1][nm]], axis=1)
        cat0 = lambda nm, b: np.concatenate([r1[2 * b][nm], r1[2 * b + 1][nm]], axis=0)
        in2 = []
        for b in range(BATCH):
            f = dict(aqT=catT('aqT', b), akT=catT('akT', b), av=cat0('av', b), fqT=catT('fqT', b), fkT=catT('fkT', b),
                     fv=cat0('fv', b), iqT=catT('iqT', b), ikT=catT('ikT', b), small=cat0('small', b))
            h = dict(qpT=catT('qpT', b), kpT=catT('kpT', b), kp=cat0('kp', b), cv=cat0('cv', b), em=catT('em', b), g=catT('g', b))
            for j in range(2):
                d = _pref('a_', k2a_inputs(j, f['aqT'], f['akT'], f['av'], f['fqT'], f['fkT'], f['fv'], f['iqT'], f['ikT'], f['small']))
                d.update(_pref('b_', k2b_inputs(j, h['qpT'], h['kpT'], h['kp'], h['cv'], h['em'], h['g'])))
                in2.append(d)
            del f, h
        r2 = _launch(build_l2()[0], in2)
        del in2
        nxt_l = l + 1 if l + 1 < DEPTH else None
        wgu_l, wd_l = f32(w_gu[l]), f32(w_down[l])
        if nxt_l is not None:
            w_perm = np.ascontiguousarray(f32(w_in[nxt_l])[:, K1_COLS])
        in3 = []
        for b in range(BATCH):
            oa = np.empty((64, 8, SEQ), dtype=NPBF)
            ob = np.empty((64, 8, SEQ), dtype=NPBF)
            for j in range(2):
                cols = _slot_cols(j)
                oa[:, :, cols] = r2[2 * b + j]['a_oaT']
                ob[:, :, cols] = r2[2 * b + j]['a_obT']
            oc = np.concatenate([r2[2 * b]['b_ocT'], r2[2 * b + 1]['b_ocT']], axis=1)
            for cc in range(2):
                c = 2 * b + cc
                sl = slice(cc * TOK, (cc + 1) * TOK)
                d = _pref('a_', k3a_inputs(np.ascontiguousarray(oa[:, :, sl]), np.ascontiguousarray(ob[:, :, sl]), np.ascontiguousarray(oc[:, :, sl]),
                                           r1[c]['cgT'], r1[c]['gT'], x_cur[c * TOK:(c + 1) * TOK],
                                           f32(w_branch_a[l]), f32(w_branch_b[l]), f32(w_branch_c[l]), f32(w_out[l]),
                                           f32(hgrn_norm_g[l]), f32(ln1_g[l]), f32(ln1_b[l])))
                d3b = _k3b_inputs(np.zeros((1, 1), np.float32), f32(w_router[l]), f32(b_router[l]), wgu_l, f32(b_gu[l]), wd_l, f32(b_down[l]),
                                  f32(ln2_g[l]), f32(ln2_b[l]))
                del d3b['h']
                d.update(_pref('b_', d3b))
                if nxt_l is not None:
                    d1 = k1_inputs(c, np.zeros((1, 1), np.float32), w_perm, f32(b_fox_f[nxt_l]), lbl)
                    del d1['x']
                    d.update(_pref('k_', d1))
                in3.append(d)
        del r1, r2
        r3 = _launch(build_l3(nxt_l)[0], in3)
        del in3
        x_cur = np.concatenate([np.asarray(r3[c]['b_out'], dtype=np.float32) for c in range(NCORES)], axis=0)
        if nxt_l is not None:
            r1 = [{k[2:]: v for k, v in r3[c].items() if k.startswith('k_')} for c in range(NCORES)]
        del r3
    return x_cur.reshape(BATCH, SEQ, D_MODEL)
```

```python
import numpy as np
import ml_dtypes
import concourse.bass as bass
import concourse.mybir as mybir
from concourse.bass_utils import run_bass_kernel_spmd

F32 = mybir.dt.float32
BF16 = mybir.dt.bfloat16
I32 = mybir.dt.int32
ALU = mybir.AluOpType
AF = mybir.ActivationFunctionType
AX = mybir.AxisListType
NPBF = ml_dtypes.bfloat16

D_MODEL = 1024
SEQ = 8192
BATCH = 4
DEPTH = 2
NCORES = 8
TOK = 4096
ALPHA = (2 * DEPTH) ** 0.25
N_EXP = 32

class Prog:
    def __init__(self, nc):
        self.nc = nc
        self.ops = []
        self.last_w = {}
        self.readers = {}

    def op(self, eng, fn, reads=(), writes=(), dma=None):
        idx = len(self.ops)
        deps = set()
        for b in reads:
            w = self.last_w.get(b)
            if w is not None:
                deps.add(w)
        for b in writes:
            w = self.last_w.get(b)
            if w is not None:
                deps.add(w)
            for r in self.readers.get(b, ()):
                deps.add(r)
        for b in writes:
            self.last_w[b] = idx
            self.readers[b] = []
        for b in reads:
            if b not in writes:
                self.readers.setdefault(b, []).append(idx)
        deps.discard(idx)
        self.ops.append(dict(eng=eng, fn=fn, deps=deps, dma=dma))
        return idx

    def dma(self, q, out, in_, reads=(), writes=(), key=None, **kw):
        assert key is not None
        return self.op(q, lambda e: e.dma_start(out=out, in_=in_, **kw), reads, writes, dma=key)

    def mm(self, out, lhsT, rhs, start, stop, reads=(), writes=()):
        return self.op('pe', lambda e: e.matmul(out, lhsT, rhs, start=start, stop=stop), reads, writes)

    def tr(self, out, in_, ident, reads=(), writes=()):
        return self.op('pe', lambda e: e.transpose(out, in_, ident), reads, writes)

    def barrier(self):
        last = {}
        for i, o in enumerate(self.ops):
            if o['fn'] is None:
                continue
            last[('dma', o['dma']) if o['dma'] is not None else ('eng', o['eng'])] = i
        deps = set(last.values())
        for eng in ('pe', 'act', 'dve', 'pool', 'sp'):
            self.ops.append(dict(eng=eng, fn=None, deps=set(deps), dma=None))
        self.last_w = {}
        self.readers = {}

    def emit(self, final_keys=()):
        nc = self.nc
        ops = self.ops
        n = len(ops)
        need = [False] * n
        for o in ops:
            best = {}
            ed = set()
            for d in o['deps']:
                po = ops[d]
                if po['eng'] == 'pe' and o['eng'] == 'pe' and po['dma'] is None and o['dma'] is None:
                    continue
                if po['dma'] is not None:
                    ed.add(d)
                else:
                    best[po['eng']] = max(best.get(po['eng'], -1), d)
            ed.update(best.values())
            o['edeps'] = ed
            for d in ed:
                need[d] = True
        for i, o in enumerate(ops):
            if o['dma'] is not None:
                need[i] = True
        eng_cnt = {}
        key_cnt = {}
        sig = [None] * n
        key_before = [None] * n
        key_events = {}
        for i, o in enumerate(ops):
            if o['dma'] is not None:
                k = o['dma']
                key_cnt[k] = key_cnt.get(k, 0) + 1
                key_events.setdefault(k, []).append(i)
                sig[i] = ('dma:' + str(k), 16 * key_cnt[k])
            elif need[i]:
                e = o['eng']
                eng_cnt[e] = eng_cnt.get(e, 0) + 1
                sig[i] = ('eng:' + e, eng_cnt[e])
        semnames = sorted(set(s[0] for s in sig if s is not None))
        import bisect
        from contextlib import ExitStack
        with ExitStack() as st:
            sems = {nm: st.enter_context(nc.semaphore('s%d' % j)) for j, nm in enumerate(semnames)}
            block = st.enter_context(nc.Block())
            by_eng = {}
            for i, o in enumerate(ops):
                by_eng.setdefault(o['eng'], []).append(i)

            def run(engname, e):
                waited = {}
                for i in by_eng.get(engname, []):
                    o = ops[i]
                    wl = {}
                    for d in o['edeps']:
                        po = ops[d]
                        nm, val = sig[d]
                        if po['dma'] is not None:
                            ev = key_events[po['dma']]
                            cnt = bisect.bisect_left(ev, i)
                            val = 16 * cnt
                        if val > wl.get(nm, 0):
                            wl[nm] = val
                    for nm, val in wl.items():
                        if val > waited.get(nm, 0):
                            e.wait_ge(sems[nm], val)
                            waited[nm] = val
                    if o['fn'] is None:
                        continue
                    ins = o['fn'](e)
                    if sig[i] is not None:
                        nm, val = sig[i]
                        ins.then_inc(sems[nm], 16 if o['dma'] is not None else 1)
                for k in final_keys:
                    ev = key_events.get(k, [])
                    if ev and ops[ev[-1]]['eng'] == engname:
                        e.wait_ge(sems['dma:' + str(k)], 16 * len(ev))

            @block.tensor
            def _(e):
                run('pe', e)

            @block.scalar
            def _(e):
                run('act', e)

            @block.vector
            def _(e):
                run('dve', e)

            @block.gpsimd
            def _(e):
                run('pool', e)

            @block.sync
            def _(e):
                run('sp', e)
        self.stats = dict(n_ops=n, eng_cnt=eng_cnt, n_sems=len(semnames))


SPL = dict(aq=(0, 512), ak=(512, 1024), av=(1024, 1536), iq=(1536, 1792), ik=(1792, 1856), iw=(1856, 1860),
           fq=(1860, 2372), fk=(2372, 2884), fv=(2884, 3396), ff=(3396, 3404), cq=(3404, 3916), cf=(3916, 4428),
           ci=(4428, 4940), cg=(4940, 5452), gt=(5452, 8524))


def _rot_cols(lo, hi):
    c = np.arange(lo, hi)
    base = lo + ((c - lo) // 64) * 64
    return base + ((c - base + 32) % 64)


def k1_weight_columns():
    cols = []
    off = {}
    pos = 0

    def add(name, idx):
        nonlocal pos
        off[name] = (pos, len(idx))
        cols.append(np.asarray(idx))
        pos += len(idx)

    for nm in ('aq', 'ak', 'iq', 'ik'):
        lo, hi = SPL[nm]
        add(nm, np.arange(lo, hi))
        add(nm + '_rot', _rot_cols(lo, hi))
    for nm in ('fq', 'fk', 'cq', 'cf', 'cg', 'gt', 'av', 'fv', 'ci'):
        lo, hi = SPL[nm]
        add(nm, np.arange(lo, hi))
    add('small', np.concatenate([np.arange(*SPL['iw']), np.arange(*SPL['ff'])]))
    return np.concatenate(cols), off


K1_COLS, K1_OFF = k1_weight_columns()
K1_NCOLS = len(K1_COLS)


def rope_tables(positions):
    half = 32
    inv = (10000.0 ** (-np.arange(half, dtype=np.float32) / half)).astype(np.float32)
    ang = positions.astype(np.float32)[None, :] * inv[:, None]
    cos = np.cos(ang).astype(np.float32)
    sin = np.sin(ang).astype(np.float32)
    cosT = np.concatenate([cos, cos, cos, cos], axis=0)
    sinT = np.concatenate([-sin, sin, -sin, sin], axis=0)
    return np.ascontiguousarray(cosT), np.ascontiguousarray(sinT)


def build_k1(layer=0):
    nc = bass.Bass("TRN2", target_bir_lowering=False)
    P = Prog(nc)
    phase_k1(nc, P, '', layer)
    P.emit(final_keys=['out'])
    return nc, P


def phase_k1(nc, P, px, layer=0, ext=None):
    ext = ext or {}
    T = TOK
    NT = T // 128
    NTC = T // 512
    x_d = ext['x'] if 'x' in ext else nc.dram_tensor(px + "x", [T, D_MODEL], F32, kind="ExternalInput").ap()
    w_d = nc.dram_tensor(px + "w", [D_MODEL, K1_NCOLS], F32, kind="ExternalInput").ap()
    cos_d = nc.dram_tensor(px + "cosT", [128, T], F32, kind="ExternalInput").ap()
    sin_d = nc.dram_tensor(px + "sinT", [128, T], F32, kind="ExternalInput").ap()
    bfox_d = nc.dram_tensor(px + "bfox", [1, 8], F32, kind="ExternalInput").ap()
    lbl_d = nc.dram_tensor(px + "lbl", [128, 2, 4], F32, kind="ExternalInput").ap()
    ident_d = nc.dram_tensor(px + "ident", [128, 128], F32, kind="ExternalInput").ap()
    rmask_d = nc.dram_tensor(px + "rmask", [128, 512], F32, kind="ExternalInput").ap()

    def out_t(name, shape, dt):
        return nc.dram_tensor(px + name, shape, dt, kind="ExternalOutput").ap()

    o_aqT = out_t("aqT", [512, T], BF16)
    o_akT = out_t("akT", [512, T], BF16)
    o_iqT = out_t("iqT", [256, T], BF16)
    o_ikT = out_t("ikT", [64, T], BF16)
    o_fqT = out_t("fqT", [512, T], BF16)
    o_fkT = out_t("fkT", [512, T], BF16)
    o_av = out_t("av", [T, 512], BF16)
    o_fv = out_t("fv", [T, 512], BF16)
    o_cv = out_t("cv", [T, 512], BF16)
    o_small = out_t("small", [T, 12], F32)
    o_qpT = out_t("qpT", [512, T], BF16)
    o_kpT = out_t("kpT", [512, T], BF16)
    o_kp = out_t("kp", [T, 512], BF16)
    o_em = out_t("em", [512, T // 64], F32)
    o_g = out_t("g", [512, T // 64], F32)
    o_cgT = out_t("cgT", [512, T], BF16)
    o_gT = out_t("gT", [3072, T], BF16)

    from contextlib import ExitStack
    with ExitStack() as st:
        def sb(name, shape, dt):
            return st.enter_context(nc.sbuf_tensor(px + name, shape, dt))
        xT = sb("xT", [128, 8, T], BF16)
        cosT = sb("cosT_s", [128, T], F32)
        sinT = sb("sinT_s", [128, T], F32)
        ident = sb("ident_s", [128, 128], F32)
        identb = sb("identb", [128, 128], BF16)
        rmask = sb("rmask_s", [128, 512], F32)
        lb = sb("lb_s", [128, 4], F32)
        oml = sb("oml", [128, 4], F32)
        noml = sb("noml", [128, 4], F32)
        bfox = sb("bfox_s", [128, 8], F32)
        xin = [sb("xin%d" % i, [128, D_MODEL], F32) for i in range(2)]
        xbf = [sb("xbf%d" % i, [128, D_MODEL], BF16) for i in range(2)]
        wst = [sb("wst%d" % i, [128, 8, 512], F32) for i in range(2)]
        wbf = [sb("wbf%d" % i, [128, 8, 512], BF16) for i in range(2)]
        t1 = [sb("t1_%d" % i, [128, 512], F32) for i in range(2)]
        t2 = [sb("t2_%d" % i, [128, 512], F32) for i in range(2)]
        t3 = [sb("t3_%d" % i, [128, 512], F32) for i in range(2)]
        t4 = [sb("t4_%d" % i, [128, 512], F32) for i in range(2)]
        ob = [sb("ob%d" % i, [128, 512], BF16) for i in range(4)]
        ob2 = [sb("ob2_%d" % i, [128, 512], BF16) for i in range(2)]
        osm = [sb("osm%d" % i, [128, 12], F32) for i in range(2)]
        emg = sb("emg", [128, 2, 4, T // 64], F32)
        ps = st.enter_context(nc.psum_tensor(px + "ps", [128, 8, 512], F32))
        psb = ps
        cnt = dict(ps=0, ob=0, ob2=0, t=0, w=0, x=0, osm=0)

        def nxt(k, n):
            v = cnt[k] % n
            cnt[k] += 1
            return v

        P.dma('sp', cosT[:], cos_d, writes=['cosT'], key='c0')
        P.dma('sp', sinT[:], sin_d, writes=['sinT'], key='c1')
        P.dma('sp', ident[:], ident_d, writes=['ident'], key='c2')
        P.dma('sp', rmask[:], rmask_d, writes=['rmask'], key='c3')
        lbl = sb("lbl_s", [128, 2, 4], F32)
        P.dma('sp', lbl[:], lbl_d, writes=['lbl'], key='c4')
        if layer == 0:
            P.op('dve', lambda e: e.memset(lb[:], 0.0), writes=['lb'])
        else:
            P.op('dve', lambda e: e.tensor_tensor(lb[:], lbl[:, 1, :], lbl[:, 0, :], ALU.subtract), reads=['lbl'], writes=['lb'])
            P.op('act', lambda e: e.activation(lb[:], lb[:], AF.Sigmoid), reads=['lb'], writes=['lb'])
        P.dma('sp', bfox[:], bfox_d.partition_broadcast(128), writes=['bfox'], key='c5')
        P.op('dve', lambda e: e.tensor_copy(identb[:], ident[:]), reads=['ident'], writes=['identb'])
        P.op('dve', lambda e: e.tensor_scalar(oml[:], lb[:], -1.0, 1.0, ALU.mult, ALU.add), reads=['lb'], writes=['oml'])
        P.op('dve', lambda e: e.tensor_scalar(noml[:], lb[:], 1.0, -1.0, ALU.mult, ALU.add), reads=['lb'], writes=['noml'])

        for ti in range(NT):
            b = nxt('x', 2)
            P.dma('sp', xin[b][:], x_d[ti * 128:(ti + 1) * 128, :], writes=[('xin', b)], key=('xin', b))
            P.op('act', lambda e, b=b: e.copy(xbf[b][:], xin[b][:]), reads=[('xin', b)], writes=[('xbf', b)])
            for half in range(2):
                pb = nxt('ps', 8)
                pst = ps[:, pb, :].bitcast(BF16)
                for j in range(4):
                    k = half * 4 + j
                    P.tr(pst[:, j * 128:(j + 1) * 128], xbf[b][:, k * 128:(k + 1) * 128], identb[:],
                         reads=[('xbf', b), 'identb'], writes=[('ps', pb)])
                P.op('dve', lambda e, pst=pst, half=half, ti=ti: e.tensor_copy(
                    xT[:, half * 4:(half + 1) * 4, ti * 128:(ti + 1) * 128],
                    pst[:, 0:512].rearrange("p (j t) -> p j t", j=4)),
                    reads=[('ps', pb)], writes=[('xT', ti)])

        w_v = w_d.rearrange("(c p) n -> p c n", p=128)

        def load_w(col0, ncols):
            b = nxt('w', 2)
            P.dma('sp', wst[b][:, :, 0:ncols], w_v[:, :, col0:col0 + ncols], writes=[('wst', b)], key=('wst', b))
            P.op('pool', lambda e: e.tensor_copy(wbf[b][:, :, 0:ncols], wst[b][:, :, 0:ncols]),
                 reads=[('wst', b)], writes=[('wbf', b)])
            return b

        def fm_matmul(wb, c0, m, tc):
            pb = nxt('ps', 8)
            for k in range(8):
                P.mm(ps[0:m, pb, :], wbf[wb][:, k, c0:c0 + m], xT[:, k, tc * 512:(tc + 1) * 512], k == 0, k == 7,
                     reads=[('wbf', wb)] + [('xT', tc * 4 + i) for i in range(4)], writes=[('ps', pb)])
            return pb

        all_xT = [('xT', i) for i in range(NT)]

        jobs = []
        def rope_group(nm, o_d):
            s0, n = K1_OFF[nm]
            r0, _ = K1_OFF[nm + '_rot']
            nblk = max(1, n // 128)
            m = min(n, 128)
            for bi in range(nblk):
              def load(bi=bi):
                b = nxt('w', 2)
                P.dma('sp', wst[b][:, :, 0:m], w_v[:, :, s0 + bi * 128:s0 + bi * 128 + m], writes=[('wst', b)], key=('wst', b))
                P.dma('sp', wst[b][:, :, 128:128 + m], w_v[:, :, r0 + bi * 128:r0 + bi * 128 + m], writes=[('wst', b)], key=('wst', b))
                P.op('pool', lambda e, b=b: e.tensor_copy(wbf[b][:, :, 0:256], wst[b][:, :, 0:256]),
                     reads=[('wst', b)], writes=[('wbf', b)])
                return b
              def comp(b, bi=bi):
                for tc in range(NTC):
                    p0 = fm_matmul(b, 0, m, tc)
                    p1 = fm_matmul(b, 128, m, tc)
                    tb = nxt('t', 2)
                    sl = slice(tc * 512, (tc + 1) * 512)
                    P.op('dve', lambda e, p0=p0, tb=tb, sl=sl: e.tensor_tensor(t1[tb][0:m, :], ps[0:m, p0, :], cosT[0:m, sl], ALU.mult),
                         reads=[('ps', p0), 'cosT'], writes=[('t1', tb)])
                    P.op('dve', lambda e, p1=p1, tb=tb, sl=sl: e.tensor_tensor(t2[tb][0:m, :], ps[0:m, p1, :], sinT[0:m, sl], ALU.mult),
                         reads=[('ps', p1), 'sinT'], writes=[('t2', tb)])
                    o = nxt('ob', 4)
                    P.op('pool', lambda e, tb=tb, o=o: e.tensor_tensor(ob[o][0:m, :], t1[tb][0:m, :], t2[tb][0:m, :], ALU.add),
                         reads=[('t1', tb), ('t2', tb)], writes=[('ob', o)])
                    P.dma('sp', o_d[bi * 128:bi * 128 + m, sl], ob[o][0:m, :], reads=[('ob', o)], key='out')
              jobs.append((load, comp))

        rope_group('aq', o_aqT)
        rope_group('ak', o_akT)
        rope_group('iq', o_iqT)
        rope_group('ik', o_ikT)

        def plain_group(nm, o_d, func):
            s0, n = K1_OFF[nm]
            for g0 in range(0, n, 512):
                gn = min(512, n - g0)
                def load(g0=g0, gn=gn):
                    return load_w(s0 + g0, gn)
                def comp(b, g0=g0, gn=gn):
                  for bi in range(gn // 128):
                    for tc in range(NTC):
                        p0 = fm_matmul(b, bi * 128, 128, tc)
                        o = nxt('ob', 4)
                        sl = slice(tc * 512, (tc + 1) * 512)
                        P.op('act', lambda e, p0=p0, o=o: e.activation(ob[o][:], ps[:, p0, :], func),
                             reads=[('ps', p0)], writes=[('ob', o)])
                        r0 = g0 + bi * 128
                        P.dma('sp', o_d[r0:r0 + 128, sl], ob[o][:], reads=[('ob', o)], key='out')
                jobs.append((load, comp))

        plain_group('fq', o_fqT, AF.Copy)
        plain_group('fk', o_fkT, AF.Copy)
        plain_group('cg', o_cgT, AF.Sigmoid)
        plain_group('gt', o_gT, AF.Sigmoid)

        sq0, _ = K1_OFF['cq']
        sf0, _ = K1_OFF['cf']
        for pr in range(4):
          def load(pr=pr):
            b = nxt('w', 2)
            P.dma('sp', wst[b][:, :, 0:128], w_v[:, :, sq0 + pr * 128:sq0 + (pr + 1) * 128], writes=[('wst', b)], key=('wst', b))
            P.dma('sp', wst[b][:, :, 128:256], w_v[:, :, sf0 + pr * 128:sf0 + (pr + 1) * 128], writes=[('wst', b)], key=('wst', b))
            P.op('pool', lambda e, b=b: e.tensor_copy(wbf[b][:, :, 0:256], wst[b][:, :, 0:256]),
                 reads=[('wst', b)], writes=[('wbf', b)])
            return b
          def comp(b, pr=pr):
            for tc in range(NTC):
                pq = fm_matmul(b, 0, 128, tc)
                pf = fm_matmul(b, 128, 128, tc)
                tb = nxt('t', 2)
                sl = slice(tc * 512, (tc + 1) * 512)
                P.op('act', lambda e, pf=pf, tb=tb: e.activation(t1[tb][:], ps[:, pf, :], AF.Sigmoid),
                     reads=[('ps', pf)], writes=[('t1', tb)])
                P.op('act', lambda e, tb=tb, pr=pr: e.activation(t2[tb][:], t1[tb][:], AF.Ln, bias=lb[:, pr:pr + 1], scale=oml[:, pr:pr + 1]),
                     reads=[('t1', tb), 'lb', 'oml'], writes=[('t2', tb)])
                P.op('pool', lambda e, tb=tb, pr=pr: e.tensor_scalar(t3[tb][:], t1[tb][:], noml[:, pr:pr + 1], oml[:, pr:pr + 1], ALU.mult, ALU.add),
                     reads=[('t1', tb), 'oml', 'noml'], writes=[('t3', tb)])
                P.op('dve', lambda e, tb=tb: e.tensor_tensor_scan(t4[tb][:], rmask[:], t2[tb][:], 0.0, ALU.mult, ALU.add),
                     reads=[('t2', tb), 'rmask'], writes=[('t4', tb)])
                b3 = t4[tb][:].rearrange("p (c t) -> p c t", t=64)
                P.op('act', lambda e, b3=b3, pr=pr, tc=tc: e.activation(emg[:, 0, pr, tc * 8:(tc + 1) * 8], b3[:, :, 31], AF.Exp),
                     reads=[('t4', tb)], writes=[('emg', pr)])
                P.op('dve', lambda e, tb=tb, b3=b3: e.tensor_tensor(t2[tb][:].rearrange("p (c t) -> p c t", t=64), b3,
                                                                   b3[:, :, 31:32].to_broadcast([128, 8, 64]), ALU.subtract),
                     reads=[('t4', tb)], writes=[('t2', tb)])
                P.op('act', lambda e, tb=tb: e.activation(t4[tb][:], t2[tb][:], AF.Exp),
                     reads=[('t2', tb)], writes=[('t4', tb)])
                P.op('act', lambda e, tb=tb: e.activation(t1[tb][:], t2[tb][:], AF.Exp, scale=-1.0),
                     reads=[('t2', tb)], writes=[('t1', tb)])
                P.op('pool', lambda e, tb=tb, pr=pr, tc=tc: e.tensor_copy(emg[:, 1, pr, tc * 8:(tc + 1) * 8],
                                                                       t4[tb][:].rearrange("p (c t) -> p c t", t=64)[:, :, 63]),
                     reads=[('t4', tb)], writes=[('emg', pr)])
                o = nxt('ob', 4)
                P.op('dve', lambda e, pq=pq, tb=tb, o=o: e.tensor_tensor(ob[o][:], ps[:, pq, :], t4[tb][:], ALU.mult),
                     reads=[('ps', pq), ('t4', tb)], writes=[('ob', o)])
                P.dma('sp', o_qpT[pr * 128:(pr + 1) * 128, sl], ob[o][:], reads=[('ob', o)], key='out')
                o2 = nxt('ob', 4)
                P.op('pool', lambda e, tb=tb, o2=o2: e.tensor_tensor(ob[o2][:], t3[tb][:], t1[tb][:], ALU.mult),
                     reads=[('t3', tb), ('t1', tb)], writes=[('ob', o2)])
                P.dma('sp', o_kpT[pr * 128:(pr + 1) * 128, sl], ob[o2][:], reads=[('ob', o2)], key='out')
                pb = nxt('ps', 8)
                pst = ps[:, pb, :].bitcast(BF16)
                for j in range(4):
                    P.tr(pst[:, j * 128:(j + 1) * 128], ob[o2][:, j * 128:(j + 1) * 128], identb[:],
                         reads=[('ob', o2), 'identb'], writes=[('ps', pb)])
                o3 = nxt('ob2', 2)
                P.op('act', lambda e, pst=pst, o3=o3: e.copy(ob2[o3][:], pst[:, 0:512]),
                     reads=[('ps', pb)], writes=[('ob2', o3)])
                P.dma('sp', o_kp[tc * 512:(tc + 1) * 512, pr * 128:(pr + 1) * 128].rearrange("(j t) c -> t j c", t=128),
                      ob2[o3][:].rearrange("t (j c) -> t j c", j=4), reads=[('ob2', o3)], key='out')
            P.dma('sp', o_em[pr * 128:(pr + 1) * 128, :], emg[:, 0, pr, :], reads=[('emg', pr)], key='out')
            P.dma('sp', o_g[pr * 128:(pr + 1) * 128, :], emg[:, 1, pr, :], reads=[('emg', pr)], key='out')
          jobs.append((load, comp))

        for nm, o_d in (('av', o_av), ('fv', o_fv), ('ci', o_cv)):
            s0, n = K1_OFF[nm]
            def load(s0=s0):
                return load_w(s0, 512)
            def comp(b, o_d=o_d):
              for ti in range(NT):
                pb = nxt('ps', 8)
                for k in range(8):
                    P.mm(ps[:, pb, :], xT[:, k, ti * 128:(ti + 1) * 128], wbf[b][:, k, :], k == 0, k == 7,
                         reads=[('wbf', b), ('xT', ti)], writes=[('ps', pb)])
                o = nxt('ob', 4)
                P.op('act', lambda e, pb=pb, o=o: e.copy(ob[o][:], ps[:, pb, :]), reads=[('ps', pb)], writes=[('ob', o)])
                P.dma('sp', o_d[ti * 128:(ti + 1) * 128, :], ob[o][:], reads=[('ob', o)], key='out')
            jobs.append((load, comp))

        s0, n = K1_OFF['small']
        def load(s0=s0):
            return load_w(s0, 12)
        def comp(b):
          for ti in range(NT):
            pb = nxt('ps', 8)
            for k in range(8):
                P.mm(ps[:, pb, 0:12], xT[:, k, ti * 128:(ti + 1) * 128], wbf[b][:, k, 0:12], k == 0, k == 7,
                     reads=[('wbf', b), ('xT', ti)], writes=[('ps', pb)])
            o = nxt('osm', 2)
            P.op('dve', lambda e, pb=pb, o=o: e.tensor_scalar(osm[o][:, 0:4], ps[:, pb, 0:4], 1.0 / 16.0, None, ALU.mult),
                 reads=[('ps', pb)], writes=[('osm', o)])
            P.op('dve', lambda e, pb=pb, o=o: e.tensor_tensor(osm[o][:, 4:12], ps[:, pb, 4:12], bfox[:], ALU.add),
                 reads=[('ps', pb), 'bfox'], writes=[('osm', o)])
            P.op('act', lambda e, o=o: e.activation(osm[o][:, 4:12], osm[o][:, 4:12], AF.Sigmoid),
                 reads=[('osm', o)], writes=[('osm', o)])
            P.op('act', lambda e, o=o: e.activation(osm[o][:, 4:12], osm[o][:, 4:12], AF.Ln),
                 reads=[('osm', o)], writes=[('osm', o)])
            P.dma('sp', o_small[ti * 128:(ti + 1) * 128, :], osm[o][:], reads=[('osm', o)], key='out')
        jobs.append((load, comp))

        nb = jobs[0][0]()
        for ji in range(len(jobs)):
            cur = nb
            if ji + 1 < len(jobs):
                nb = jobs[ji + 1][0]()
            jobs[ji][1](cur)

    return dict(aqT=o_aqT)


def k1_inputs(core, x_slice, w_perm, b_fox, lb_logits):
    pos = (np.arange(TOK) + (core % 2) * TOK).astype(np.float32)
    cosT, sinT = rope_tables(pos)
    rmask = np.ones((128, 512), np.float32)
    rmask[:, ::64] = 0.0
    lbl = np.ascontiguousarray(lb_logits.reshape(2, 4, 128).transpose(2, 0, 1)).astype(np.float32)
    return dict(x=np.ascontiguousarray(x_slice), w=w_perm, cosT=cosT, sinT=sinT,
                bfox=np.ascontiguousarray(b_fox.reshape(1, 8)).astype(np.float32), lbl=lbl,
                ident=np.eye(128, dtype=np.float32), rmask=rmask)


NSLOT = 8
NBIS = 16
TOPK = 256.0
NEG = -1.0e30


def build_k2a():
    nc = bass.Bass("TRN2", target_bir_lowering=False)
    P = Prog(nc)
    phase_k2a(nc, P, '')
    P.emit(final_keys=['out'])
    return nc, P


def phase_k2a(nc, P, px, ext=None):
    ext = ext or {}
    S = SEQ
    QT = NSLOT * 512
    din = lambda name, shape, dt: ext[name] if name in ext else nc.dram_tensor(px + name, shape, dt, kind="ExternalInput").ap()
    aq_d = din("aq", [4, 128, QT], BF16)
    ak_d = din("ak", [4, 128, S], BF16)
    av_d = din("av", [4, 128, 64, 130], BF16)
    fq_d = din("fq", [4, 128, QT], BF16)
    fk_d = din("fk", [4, 128, S], BF16)
    fv_d = din("fv", [4, 128, 64, 130], BF16)
    iq_d = din("iq", [128, 2, QT], BF16)
    ik_d = din("ik2", [128, S], BF16)
    iw_d = din("iw", [128, NSLOT * 4, 4], F32)
    lf_d = din("lf", [128, 64, 8], F32)
    adm_d = din("adm", [4, 128, 1024], F32)
    cm_d = din("cm", [128, 8, 512], BF16)
    jf_d = din("jflag", [128, 1], F32)
    tri_d = din("tri", [128, 128], F32)
    ident_d = din("identb", [128, 128], BF16)
    pw_d = din("pw2", [128, NBIS], F32)
    oa_d = nc.dram_tensor(px + "oaT", [64, 8, QT], BF16, kind="ExternalOutput").ap()
    ob_d = nc.dram_tensor(px + "obT", [64, 8, QT], BF16, kind="ExternalOutput").ap()

    from contextlib import ExitStack
    with ExitStack() as st:
        def sb(name, shape, dt):
            return st.enter_context(nc.sbuf_tensor(px + name, shape, dt))
        Sc = sb("Sc", [128, S], F32)
        Mk = sb("Mk", [128, S], BF16)
        MT = sb("MT", [128, 64, 512], BF16)
        ik2 = sb("ik2s", [128, S], BF16)
        KT = [sb("KT%d" % i, [128, 2048], BF16) for i in range(2)]
        VA = [sb("VA%d" % i, [128, 16, 130], BF16) for i in range(2)]
        qT = [sb("qT%d" % i, [128, 512], BF16) for i in range(2)]
        iqc = sb("iqc", [128, 2, 512], BF16)
        iw = sb("iws", [128, NSLOT * 4, 4], F32)
        lf = sb("lfs", [128, 64, 8], F32)
        cw = sb("cw", [128, 64, 8], F32)
        offs = sb("offs", [128, 65, 8], F32)
        tot = sb("tot", [128, 64, 8], F32)
        adm = [sb("adm%d" % i, [128, 1024], F32) for i in range(2)]
        cm = sb("cms", [128, 8, 512], BF16)
        jf = sb("jfs", [128, 1], F32)
        tri = sb("tris", [128, 128], F32)
        ones = sb("ones", [128, 128], F32)
        identb = sb("identbs", [128, 128], BF16)
        pw = sb("pws", [128, NBIS], F32)
        rl = [sb("rl%d" % i, [128, 512], F32) for i in range(3)]
        pT = [sb("pT%d" % i, [128, 512], BF16) for i in range(6)]
        U = [sb("U%d" % i, [65, 512], F32) for i in range(4)]
        R = [sb("R%d" % i, [65, 512], F32) for i in range(4)]
        oT = [sb("oT%d" % i, [64, 512], BF16) for i in range(2)]
        bfx = [sb("bfx%d" % i, [128, 64], F32) for i in range(2)]
        cref2 = [sb("cref%d" % i, [128, 8], F32) for i in range(2)]
        sm = sb("sm", [128, 16], F32)
        dk = sb("dk", [128, NBIS], F32)
        ps = st.enter_context(nc.psum_tensor(px + "ps", [128, 8, 512], F32))
        cnt = {}

        def nxt(k, n):
            v = cnt.get(k, 0) % n
            cnt[k] = cnt.get(k, 0) + 1
            return v

        for i, (t, d, kn) in enumerate([(ik2, ik_d, 'ik2s'), (iw, iw_d, 'iws'), (lf, lf_d, 'lfs'), (cm, cm_d, 'cms'), (jf, jf_d, 'jfs'),
                                        (tri, tri_d, 'tris'), (identb, ident_d, 'identbs'), (pw, pw_d, 'pws')]):
            P.dma('sp', t[:], d, writes=[kn], key='c%d' % i)
        P.op('pool', lambda e: e.memset(ones[:], 1.0), writes=['ones'])

        lf2 = lf[:].rearrange("p b h -> p (b h)")
        P.mm(ps[:, 0, :], tri[:], lf2, True, True, reads=['tris', 'lfs'], writes=[('ps', 0)])
        P.mm(ps[:, 1, :], ones[:], lf2, True, True, reads=['ones', 'lfs'], writes=[('ps', 1)])
        P.op('act', lambda e: e.copy(tot[:].rearrange("p b h -> p (b h)"), ps[:, 1, :]), reads=[('ps', 1)], writes=['tot'])
        P.op('dve', lambda e: e.memset(offs[:, 0, :], 0.0), writes=['offs'])
        for bl in range(64):
            P.op('dve', lambda e, bl=bl: e.tensor_tensor(offs[:, bl + 1, :], offs[:, bl, :], tot[:, bl, :], ALU.add),
                 reads=['offs', 'tot'], writes=['offs'])
        P.op('dve', lambda e: e.tensor_tensor(cw[:], ps[:, 0, :].rearrange("p (b h) -> p b h", h=8), offs[:, 0:64, :], ALU.add),
             reads=[('ps', 0), 'offs'], writes=['cw'])
        P.op('dve', lambda e: e.tensor_scalar(cw[:], cw[:], -1.0, None, ALU.mult), reads=['cw'], writes=['cw'])

        def gbank():
            return nxt('g', 4)

        def attention(slot, branch):
            nblk = (2 * slot + 2) * 4
            q_d, k_d, v_d, o_d = (aq_d, ak_d, av_d, oa_d) if branch == 'a' else (fq_d, fk_d, fv_d, ob_d)
            jobs = []
            for pr in range(4):
                segs = [(s0, min(16, nblk - s0)) for s0 in range(0, nblk, 16)]
                for si, (s0, sn) in enumerate(segs):
                    def load(pr=pr, s0=s0, sn=sn, si=si):
                        b = nxt('kv', 2)
                        P.dma('sp', KT[b][:, 0:sn * 128], k_d[pr, :, s0 * 128:(s0 + sn) * 128], writes=[('KT', b)], key=('KT', b))
                        P.dma('sp', VA[b][:, 0:sn, :], v_d[pr, :, s0:s0 + sn, :], writes=[('VA', b)], key=('VA', b))
                        qb_ = None
                        if si == 0:
                            qb_ = nxt('q', 2)
                            P.dma('sp', qT[qb_][:], q_d[pr, :, slot * 512:(slot + 1) * 512], writes=[('qT', qb_)], key=('qT', qb_))
                        return (b, qb_)

                    def comp(ld, st_, pr=pr, s0=s0, sn=sn, si=si, last=(si == len(segs) - 1)):
                        b, qb_ = ld
                        if si == 0:
                            st_['q'] = qb_
                            st_['acc'] = [4, 5]
                            if branch == 'b':
                                for hh in range(2):
                                    h = pr * 2 + hh
                                    P.op('pool', lambda e, hh=hh, h=h: e.tensor_scalar(bfx[hh][:, 0:nblk], cw[:, 0:nblk, h], cref2[slot % 2][:, h:h + 1], 60.0, ALU.subtract, ALU.min),
                                         reads=['cw', ('cref', slot % 2)], writes=[('bfx', hh)])
                        qb = st_['q']
                        LA = 2
                        pend = []

                        def emit_pv(it):
                            hh, kl, pi = it
                            kb = s0 + kl
                            acc = st_['acc'][hh]
                            P.mm(ps[0:65, acc, :], VA[b][:, kl, hh * 65:(hh + 1) * 65], pT[pi][:], kb == 0, kb == nblk - 1,
                                 reads=[('VA', b), ('pT', pi)], writes=[('ps', acc)])
                            if last and kl == sn - 1:
                                h = pr * 2 + hh
                                ui = nxt('U', 4)
                                P.op('act', lambda e, ui=ui, acc=acc: e.copy(U[ui][:], ps[0:65, acc, :]), reads=[('ps', acc)], writes=[('U', ui)])

                                def norm(ui=ui, h=h):
                                    P.op('dve', lambda e, ui=ui: e.reciprocal(R[ui][64:65, :], U[ui][64:65, :]), reads=[('U', ui)], writes=[('R', ui)])
                                    P.mm(ps[0:64, 7, :], ones[64:65, 0:64], R[ui][64:65, :], True, True, reads=['ones', ('R', ui)], writes=[('ps', 7)])
                                    oi = nxt('oT', 2)
                                    P.op('dve', lambda e, ui=ui, oi=oi: e.tensor_tensor(oT[oi][:], U[ui][0:64, :], ps[0:64, 7, :], ALU.mult),
                                         reads=[('U', ui), ('ps', 7)], writes=[('oT', oi)])
                                    P.dma('sp', o_d[:, h, slot * 512:(slot + 1) * 512], oT[oi][:], reads=[('oT', oi)], key='out')
                                st_.setdefault('dcur', []).append(norm)

                        for hh in range(2):
                            for kl in range(sn):
                                kb = s0 + kl
                                g = gbank()
                                P.mm(ps[:, g, :], KT[b][hh * 64:(hh + 1) * 64, kl * 128:(kl + 1) * 128], qT[qb][hh * 64:(hh + 1) * 64, :], True, True,
                                     reads=[('KT', b), ('qT', qb)], writes=[('ps', g)])
                                pi = nxt('pT', 6)
                                meng = 'pool' if (kb % 2 == 0 or branch == 'b') else 'dve'
                                if branch == 'a':
                                    P.op('act', lambda e, g=g, pi=pi: e.activation(pT[pi][:], ps[:, g, :], AF.Exp, scale=0.125),
                                         reads=[('ps', g)], writes=[('pT', pi)])
                                    P.op(meng, lambda e, pi=pi, kb=kb: e.tensor_tensor(pT[pi][:], pT[pi][:], MT[:, kb, :], ALU.mult),
                                         reads=[('pT', pi), 'MT'], writes=[('pT', pi)])
                                else:
                                    P.op('act', lambda e, g=g, pi=pi, hh=hh, kb=kb: e.activation(pT[pi][:], ps[:, g, :], AF.Exp, bias=bfx[hh][:, kb:kb + 1], scale=0.125),
                                         reads=[('ps', g), ('bfx', hh)], writes=[('pT', pi)])
                                    if kb >= nblk - 8:
                                        P.op(meng, lambda e, pi=pi, kb=kb: e.tensor_tensor(pT[pi][:], pT[pi][:], cm[:, kb - (nblk - 8), :], ALU.mult),
                                             reads=[('pT', pi), 'cms'], writes=[('pT', pi)])
                                pend.append((hh, kl, pi))
                                if len(pend) > LA:
                                    emit_pv(pend.pop(0))
                        while pend:
                            emit_pv(pend.pop(0))
                        for fn_ in st_.get('dprev', []):
                            fn_()
                        st_['dprev'] = st_.get('dcur', [])
                        st_['dcur'] = []
                    jobs.append((load, comp))
            return jobs

        def jobs_gen(jobs):
            st_ = {}
            nb = jobs[0][0]()
            for ji in range(len(jobs)):
                cur = nb
                if ji + 1 < len(jobs):
                    nb = jobs[ji + 1][0]()
                jobs[ji][1](cur, st_)
                if ji == len(jobs) - 1:
                    for fn_ in st_.get('dprev', []) + st_.get('dcur', []):
                        fn_()
                    st_['dprev'] = st_['dcur'] = []
                yield

        def run_jobs(jobs):
            for _ in jobs_gen(jobs):
                pass

        def mask_gen(slot):
            cref = cref2[slot % 2]
            nblk = (2 * slot + 2) * 4
            n = nblk * 128
            P.dma('sp', iqc[:], iq_d[:, :, slot * 512:(slot + 1) * 512], writes=['iqc'], key='iqc')
            P.op('dve', lambda e, slot=slot: e.tensor_tensor(cref[:], offs[:, 8 * slot + 4, :], offs[:, 8 * slot, :], ALU.subtract),
                 reads=['offs'], writes=[('cref', slot % 2)])
            P.op('dve', lambda e, slot=slot: e.scalar_tensor_tensor(cref[:], cref[:], jf[:, 0:1], offs[:, 8 * slot, :], ALU.mult, ALU.add),
                 reads=[('cref', slot % 2), 'offs', 'jfs'], writes=[('cref', slot % 2)])
            P.op('dve', lambda e: e.tensor_scalar(cref[:], cref[:], -1.0, None, ALU.mult), reads=[('cref', slot % 2)], writes=[('cref', slot % 2)])
            for qb in range(4):
                qi = slot * 4 + qb
                ai = nxt('adm', 2)
                P.dma('sp', adm[ai][:], adm_d[qb], writes=[('adm', ai)], key=('adm', ai))
                for sc in range(n // 512):
                    cs = slice(sc * 512, (sc + 1) * 512)
                    for h in range(4):
                        g = gbank()
                        hb = (h % 2) * 64
                        P.mm(ps[:, g, :], iqc[hb:hb + 64, h // 2, qb * 128:(qb + 1) * 128], ik2[hb:hb + 64, cs], True, True,
                             reads=['iqc', 'ik2s'], writes=[('ps', g)])
                        ri = nxt('rl', 3)
                        P.op('act', lambda e, g=g, ri=ri: e.activation(rl[ri][:], ps[:, g, :], AF.Relu), reads=[('ps', g)], writes=[('rl', ri)])
                        if h == 0:
                            P.op('dve', lambda e, ri=ri, cs=cs, qi=qi: e.tensor_scalar(Sc[:, cs], rl[ri][:], iw[:, qi, 0:1], None, ALU.mult),
                                 reads=[('rl', ri), 'iws'], writes=[('Sc', sc)])
                        else:
                            P.op('dve', lambda e, ri=ri, cs=cs, qi=qi, h=h: e.scalar_tensor_tensor(Sc[:, cs], rl[ri][:], iw[:, qi, h:h + 1], Sc[:, cs], ALU.mult, ALU.add),
                                 reads=[('rl', ri), 'iws', ('Sc', sc)], writes=[('Sc', sc)])
                scall = [('Sc', sc) for sc in range(n // 512)]
                P.op('dve', lambda e, n=n: e.tensor_reduce(sm[:, 0:1], Sc[:, 0:n], AX.X, ALU.max, apply_absolute_value=True),
                     reads=scall, writes=['sm0'])
                P.op('dve', lambda e: e.tensor_scalar(sm[:, 1:2], sm[:, 0:1], -1.0, -1.0, ALU.mult, ALU.add), reads=['sm0'], writes=['lo'])
                P.op('dve', lambda e: e.tensor_scalar(sm[:, 2:3], sm[:, 0:1], 2.0, 2.0, ALU.mult, ALU.add), reads=['sm0'], writes=['w0'])
                P.op('dve', lambda e: e.tensor_scalar(dk[:], pw[:], sm[:, 2:3], None, ALU.mult), reads=['w0', 'pws'], writes=['dk'])
                P.op('dve', lambda e, n=n, ai=ai: e.tensor_tensor(Sc[:, n - 1024:n], Sc[:, n - 1024:n], adm[ai][:], ALU.add),
                     reads=[('adm', ai)] + scall[-2:], writes=scall[-2:])
                for k in range(NBIS):
                    P.op('dve', lambda e, k=k: e.tensor_tensor(sm[:, 3:4], sm[:, 1:2], dk[:, k:k + 1], ALU.add), reads=['lo', 'dk'], writes=['mid'])
                    P.op('dve', lambda e, n=n: e.tensor_scalar(Mk[:, 0:n], Sc[:, 0:n], sm[:, 3:4], None, ALU.is_ge, ALU.add, accum_out=sm[:, 4:5]),
                         reads=scall + ['mid'], writes=['Mk', 'cnt'])
                    P.op('dve', lambda e, k=k: e.scalar_tensor_tensor(sm[:, 5:6], sm[:, 4:5], TOPK, dk[:, k:k + 1], ALU.is_ge, ALU.mult),
                         reads=['cnt', 'dk'], writes=['stp'])
                    P.op('dve', lambda e: e.tensor_tensor(sm[:, 1:2], sm[:, 1:2], sm[:, 5:6], ALU.add), reads=['lo', 'stp'], writes=['lo'])
                P.op('dve', lambda e, n=n: e.tensor_scalar(Mk[:, 0:n], Sc[:, 0:n], sm[:, 1:2], None, ALU.is_ge),
                     reads=scall + ['lo'], writes=['Mk'])
                yield
                pst = ps[:, 6, :].bitcast(BF16)
                for k0 in range(0, nblk, 4):
                    for j in range(4):
                        P.tr(pst[:, j * 128:(j + 1) * 128], Mk[:, (k0 + j) * 128:(k0 + j + 1) * 128], identb[:],
                             reads=['Mk', 'identbs'], writes=[('ps', 6)])
                    P.op('act', lambda e, k0=k0, qb=qb: e.copy(MT[:, k0:k0 + 4, qb * 128:(qb + 1) * 128],
                                                              pst[:, 0:512].rearrange("p (j t) -> p j t", j=4)),
                         reads=[('ps', 6)], writes=['MT'])
                yield

        def drain(g):
            for _ in g:
                pass

        drain(mask_gen(0))
        for slot in range(NSLOT):
            run_jobs(attention(slot, 'a'))
            gf = jobs_gen(attention(slot, 'b'))
            gm = mask_gen(slot + 1) if slot + 1 < NSLOT else iter(())
            fa = ma = True
            while fa or ma:
                if fa:
                    fa = next(gf, 'end') != 'end'
                if ma:
                    ma = next(gm, 'end') != 'end'


    return dict(oaT=oa_d, obT=ob_d)


def _slot_cols(j):
    return np.concatenate([np.arange((2 * i + j) * 512, (2 * i + j + 1) * 512) for i in range(NSLOT)])


def k2a_consts(j):
    p = np.arange(128)
    col = np.arange(1024)
    adm = np.zeros((4, 128, 1024), np.float32)
    for qb in range(4):
        lim = j * 512 + qb * 128 + (p // 64) * 64 + 64
        adm[qb] = np.where(col[None, :] < lim[:, None], 0.0, NEG)
    kbl = np.arange(8)
    q = np.arange(512)
    cm = ((kbl[None, :, None] * 128 + p[:, None, None]) <= (j * 512 + q[None, None, :])).astype(np.float32).astype(NPBF)
    tri = (p[:, None] <= p[None, :]).astype(np.float32)
    pw = np.tile((2.0 ** -(np.arange(NBIS, dtype=np.float32) + 1))[None, :], (128, 1)).astype(np.float32)
    return dict(adm=adm, cm=np.ascontiguousarray(cm), jflag=np.full((128, 1), float(j), np.float32), tri=tri,
                identb=np.eye(128, dtype=np.float32).astype(NPBF), pw2=pw)


def _vaug(v_full):
    S = v_full.shape[0]
    va = np.ones((S, 8, 65), dtype=v_full.dtype)
    va[:, :, :64] = v_full.reshape(S, 8, 64)
    return np.ascontiguousarray(va.reshape(S // 128, 128, 4, 130).transpose(2, 1, 0, 3))


def k2a_inputs(j, aqT, akT, av, fqT, fkT, fv, iqT, ikT, small):
    cols = _slot_cols(j)
    S = SEQ
    d = dict(
        aq=np.ascontiguousarray(aqT.reshape(4, 128, S)[:, :, cols]),
        ak=np.ascontiguousarray(akT.reshape(4, 128, S)),
        av=_vaug(av),
        fq=np.ascontiguousarray(fqT.reshape(4, 128, S)[:, :, cols]),
        fk=np.ascontiguousarray(fkT.reshape(4, 128, S)),
        fv=_vaug(fv),
        iq=np.ascontiguousarray(iqT.reshape(2, 128, S)[:, :, cols].transpose(1, 0, 2)),
        ik2=np.ascontiguousarray(np.concatenate([ikT, ikT], axis=0)),
        iw=np.ascontiguousarray(small[cols, 0:4].reshape(NSLOT * 4, 128, 4).transpose(1, 0, 2)),
        lf=np.ascontiguousarray(small[:, 4:12].reshape(64, 128, 8).transpose(1, 0, 2)),
    )
    d.update(k2a_consts(j))
    return d


def build_k2b():
    nc = bass.Bass("TRN2", target_bir_lowering=False)
    P = Prog(nc)
    phase_k2b(nc, P, '')
    P.emit(final_keys=['out'])
    return nc, P


def phase_k2b(nc, P, px, ext=None):
    ext = ext or {}
    S = SEQ
    NCH = S // 64
    GRP = 16
    din = lambda name, shape, dt: ext[name] if name in ext else nc.dram_tensor(px + name, shape, dt, kind="ExternalInput").ap()
    qp_d = din("qpT", [64, 4, S], BF16)
    kpT_d = din("kpT", [64, 4, S], BF16)
    kp_d = din("kp", [64, NCH, 256], BF16)
    v_d = din("v", [64, NCH, 256], BF16)
    em_d = din("em", [64, 4, NCH], F32)
    g_d = din("g", [64, 4, NCH], F32)
    triu_d = din("triu", [64, 64], F32)
    oc_d = nc.dram_tensor(px + "ocT", [64, 4, S], BF16, kind="ExternalOutput").ap()

    from contextlib import ExitStack
    with ExitStack() as st:
        def sb(name, shape, dt):
            return st.enter_context(nc.sbuf_tensor(px + name, shape, dt))
        qp = [sb("qp%d" % i, [64, 4, GRP * 64], BF16) for i in range(2)]
        kpT = [sb("kpT%d" % i, [64, 4, GRP * 64], BF16) for i in range(2)]
        kp = [sb("kp%d" % i, [64, GRP, 256], BF16) for i in range(2)]
        vv = [sb("vv%d" % i, [64, GRP, 256], BF16) for i in range(2)]
        ocb = [sb("ocb%d" % i, [64, 4, GRP * 64], BF16) for i in range(2)]
        em = sb("ems", [64, 4, NCH], F32)
        gg = sb("ggs", [64, 4, NCH], F32)
        triu = sb("trius", [64, 64], F32)
        Sm = sb("Sm", [64, 4, 64], F32)
        Smb = sb("Smb", [64, 4, 64], BF16)
        tmp = sb("tmp", [64, 4, 64], F32)
        scm = [sb("scm%d" % i, [64, 4, 64], BF16) for i in range(2)]
        ps = st.enter_context(nc.psum_tensor(px + "ps", [128, 8, 512], F32))
        cnt = {}

        def nxt(k, n):
            v = cnt.get(k, 0) % n
            cnt[k] = cnt.get(k, 0) + 1
            return v

        P.dma('sp', em[:], em_d, writes=['em'], key='c0')
        P.dma('sp', gg[:], g_d, writes=['gg'], key='c1')
        P.dma('sp', triu[:], triu_d, writes=['triu'], key='c2')
        P.op('dve', lambda e: e.tensor_tensor(gg[:, :, 0:NCH - 1], gg[:, :, 0:NCH - 1], em[:, :, 1:NCH], ALU.mult), reads=['em', 'gg'], writes=['gg'])
        P.op('dve', lambda e: e.memset(Sm[:], 0.0), writes=['Sm'])
        P.op('dve', lambda e: e.memset(Smb[:], 0.0), writes=['Smb'])

        ngrp = NCH // GRP

        def load(gi):
            b = gi % 2
            ts_ = slice(gi * GRP * 64, (gi + 1) * GRP * 64)
            cs_ = slice(gi * GRP, (gi + 1) * GRP)
            P.dma('sp', qp[b][:], qp_d[:, :, ts_], writes=[('qp', b)], key=('qp', b))
            P.dma('sp', kpT[b][:], kpT_d[:, :, ts_], writes=[('kpT', b)], key=('kpT', b))
            P.dma('sp', kp[b][:], kp_d[:, cs_, :], writes=[('kp', b)], key=('kp', b))
            P.dma('sp', vv[b][:], v_d[:, cs_, :], writes=[('vv', b)], key=('vv', b))

        load(0)
        for gi in range(ngrp):
            if gi + 1 < ngrp:
                load(gi + 1)
            b = gi % 2
            for cl in range(GRP):
                c = gi * GRP + cl
                tsl = slice(cl * 64, (cl + 1) * 64)
                gs = nxt('sc', 2)
                for hl in range(4):
                    P.mm(ps[0:64, gs, hl * 64:(hl + 1) * 64], kpT[b][:, hl, tsl], qp[b][:, hl, tsl], True, True,
                         reads=[('kpT', b), ('qp', b)], writes=[('ps', gs)])
                si = nxt('scm', 2)
                P.op('dve', lambda e, gs=gs, si=si: e.tensor_tensor(scm[si][:], ps[0:64, gs, 0:256].rearrange("p (h t) -> p h t", h=4),
                                                                  triu[:].unsqueeze(1).to_broadcast([64, 4, 64]), ALU.mult),
                     reads=[('ps', gs), 'triu'], writes=[('scm', si)])
                go = 2 + nxt('o', 2)
                for hl in range(4):
                    P.mm(ps[0:64, go, hl * 64:(hl + 1) * 64], vv[b][:, cl, hl * 64:(hl + 1) * 64], scm[si][:, hl, :], True, False,
                         reads=[('vv', b), ('scm', si)], writes=[('ps', go)])
                    P.mm(ps[0:64, go, hl * 64:(hl + 1) * 64], Smb[:, hl, :], qp[b][:, hl, tsl], False, True,
                         reads=['Smb', ('qp', b)], writes=[('ps', go)])
                P.op('act', lambda e, go=go, b=b, tsl=tsl: e.copy(ocb[b][:, :, tsl], ps[0:64, go, 0:256].rearrange("p (h t) -> p h t", h=4)),
                     reads=[('ps', go)], writes=[('ocb', b)])
                gk = 4 + nxt('kv', 2)
                for hl in range(4):
                    P.mm(ps[0:64, gk, hl * 64:(hl + 1) * 64], kp[b][:, cl, hl * 64:(hl + 1) * 64], vv[b][:, cl, hl * 64:(hl + 1) * 64], True, True,
                         reads=[('kp', b), ('vv', b)], writes=[('ps', gk)])
                if c < NCH - 1:
                    P.op('dve', lambda e, gk=gk: e.tensor_tensor(tmp[:], ps[0:64, gk, 0:256].rearrange("p (h t) -> p h t", h=4), Sm[:], ALU.add),
                         reads=[('ps', gk), 'Sm'], writes=['tmp'])
                    P.op('dve', lambda e, c=c: e.tensor_tensor(Sm[:], tmp[:], gg[:, :, c:c + 1].to_broadcast([64, 4, 64]), ALU.mult),
                         reads=['tmp', 'gg'], writes=['Sm'])
                    P.op('dve', lambda e, c=c: e.tensor_tensor(Smb[:], tmp[:], gg[:, :, c:c + 1].to_broadcast([64, 4, 64]), ALU.mult),
                         reads=['tmp', 'gg'], writes=['Smb'])
            P.dma('sp', oc_d[:, :, gi * GRP * 64:(gi + 1) * GRP * 64], ocb[b][:], reads=[('ocb', b)], key='out')
    return dict(ocT=oc_d)


def k2b_inputs(j, qpT, kpT, kp, cv, em, g):
    S = SEQ
    hs = slice(4 * j, 4 * j + 4)
    cs = slice(256 * j, 256 * j + 256)
    return dict(
        qpT=np.ascontiguousarray(qpT.reshape(8, 64, S)[hs].transpose(1, 0, 2)),
        kpT=np.ascontiguousarray(kpT.reshape(8, 64, S)[hs].transpose(1, 0, 2)),
        kp=np.ascontiguousarray(kp[:, cs].reshape(S // 64, 64, 256).transpose(1, 0, 2)),
        v=np.ascontiguousarray(cv[:, cs].reshape(S // 64, 64, 256).transpose(1, 0, 2)),
        em=np.ascontiguousarray(em.reshape(8, 64, S // 64)[hs].transpose(1, 0, 2)).astype(np.float32),
        g=np.ascontiguousarray(g.reshape(8, 64, S // 64)[hs].transpose(1, 0, 2)).astype(np.float32),
        triu=np.triu(np.ones((64, 64), np.float32)),
    )


def emit_layernorm(P, y, out, lng, lnb, stats, mv, rstd, ykey, okey, gkeys, tag):
    for c in range(2):
        P.op('dve', lambda e, c=c: e.bn_stats(stats[:, c, :], y[:, c * 512:(c + 1) * 512]), reads=[ykey], writes=[(tag, 'st', c)])
    P.op('dve', lambda e: e.bn_aggr(mv[:], stats[:].rearrange("p c s -> p (c s)")), reads=[(tag, 'st', 0), (tag, 'st', 1)], writes=[(tag, 'mv')])
    P.op('act', lambda e: e.activation(rstd[:], mv[:, 1:2], AF.Sqrt, bias=1e-5), reads=[(tag, 'mv')], writes=[(tag, 'rs')])
    P.op('dve', lambda e: e.reciprocal(rstd[:], rstd[:]), reads=[(tag, 'rs')], writes=[(tag, 'rs')])
    P.op('dve', lambda e: e.tensor_scalar(out, y, mv[:, 0:1], rstd[:, 0:1], ALU.subtract, ALU.mult), reads=[ykey, (tag, 'mv'), (tag, 'rs')], writes=[okey])
    P.op('pool', lambda e: e.tensor_tensor(out, out, lng, ALU.mult), reads=[okey, gkeys[0]], writes=[okey])
    P.op('pool', lambda e: e.tensor_tensor(out, out, lnb, ALU.add), reads=[okey, gkeys[1]], writes=[okey])


def build_k3a():
    nc = bass.Bass("TRN2", target_bir_lowering=False)
    P = Prog(nc)
    phase_k3a(nc, P, '')
    P.emit(final_keys=['out'])
    return nc, P


def phase_k3a(nc, P, px, ext=None):
    ext = ext or {}
    T = TOK
    din = lambda name, shape, dt: ext[name] if name in ext else nc.dram_tensor(px + name, shape, dt, kind="ExternalInput").ap()
    oa_d = din("oaT", [64, 8, T], BF16)
    ob_d = din("obT", [64, 8, T], BF16)
    oc_d = din("ocT", [64, 8, T], BF16)
    cg_d = din("cgT", [64, 8, T], BF16)
    gt_d = din("gT", [128, 24, T], BF16)
    x_d = din("x", [T, D_MODEL], F32)
    wp_d = [din("wp%d" % i, [64, 8, D_MODEL], F32) for i in range(3)]
    wo_d = din("wo", [128, 8, D_MODEL], F32)
    gn_d = din("gn", [64, 8], F32)
    lng_d = din("lng", [1, D_MODEL], F32)
    lnb_d = din("lnb", [1, D_MODEL], F32)
    h_d = nc.dram_tensor(px + "h", [T, D_MODEL], F32, kind="ExternalOutput").ap()

    from contextlib import ExitStack
    with ExitStack() as st:
        def sb(name, shape, dt):
            return st.enter_context(nc.sbuf_tensor(px + name, shape, dt))
        wst = sb("wst", [128, 8, 512], F32)
        wp = [sb("wpb%d" % i, [64, 8, D_MODEL], BF16) for i in range(3)]
        wo = sb("wob", [128, 8, D_MODEL], BF16)
        gn = sb("gns", [64, 8], F32)
        lng = sb("lngs", [128, D_MODEL], F32)
        lnb = sb("lnbs", [128, D_MODEL], F32)
        on64 = sb("on64", [64, 64], BF16)
        oT = [[sb("oT%d_%d" % (br, i), [64, 8, 512], BF16) for i in range(1)] for br in range(3)]
        cg = [sb("cg%d" % i, [64, 8, 512], BF16) for i in range(1)]
        gt = [sb("gt%d" % i, [128, 3, 512], BF16) for i in range(2)]
        mT = sb("mT", [128, 8, 512], BF16)
        sq = [sb("sq%d" % i, [64, 512], BF16) for i in range(2)]
        rs = [sb("rs%d" % i, [64, 512], F32) for i in range(2)]
        tA = [sb("tA%d" % i, [128, 512], F32) for i in range(2)]
        tB = [sb("tB%d" % i, [128, 512], F32) for i in range(2)]
        tC = [sb("tC%d" % i, [128, 512], F32) for i in range(2)]
        xin = [sb("xin%d" % i, [128, D_MODEL], F32) for i in range(2)]
        yb = [sb("yb%d" % i, [128, D_MODEL], F32) for i in range(2)]
        hb = [sb("hb%d" % i, [128, D_MODEL], F32) for i in range(2)]
        stats = sb("stats", [128, 2, 6], F32)
        mv = sb("mv", [128, 2], F32)
        rstd = sb("rstd", [128, 1], F32)
        ps = st.enter_context(nc.psum_tensor(px + "ps", [128, 8, 512], F32))
        cnt = {}

        def nxt(k, n):
            v = cnt.get(k, 0) % n
            cnt[k] = cnt.get(k, 0) + 1
            return v

        for i in range(3):
            for hf in range(2):
                P.dma('sp', wst[0:64], wp_d[i][:, :, hf * 512:(hf + 1) * 512], writes=['wst'], key='wst')
                P.op('act', lambda e, i=i, hf=hf: e.copy(wp[i][:, :, hf * 512:(hf + 1) * 512], wst[0:64]), reads=['wst'], writes=[('wp', i)])
        for hf in range(2):
            P.dma('sp', wst[:], wo_d[:, :, hf * 512:(hf + 1) * 512], writes=['wst'], key='wst')
            P.op('act', lambda e, hf=hf: e.copy(wo[:, :, hf * 512:(hf + 1) * 512], wst[:]), reads=['wst'], writes=['wo'])
        P.dma('sp', gn[:], gn_d, writes=['gn'], key='c0')
        P.dma('sp', lng[:], lng_d.partition_broadcast(128), writes=['lng'], key='c1')
        P.dma('sp', lnb[:], lnb_d.partition_broadcast(128), writes=['lnb'], key='c2')
        P.op('pool', lambda e: e.memset(on64[:], 1.0 / 64.0), writes=['on64'])

        NTC = T // 512

        def load(tc):
            b = 0
            sl = slice(tc * 512, (tc + 1) * 512)
            for br, d in enumerate((oa_d, ob_d, oc_d)):
                P.dma('sp', oT[br][b][:], d[:, :, sl], writes=[('oT', br, b)], key=('oT', br, b))
            P.dma('sp', cg[b][:], cg_d[:, :, sl], writes=[('cg', b)], key=('cg', b))

        def load_gt(tc, mc):
            gi = nxt('gt', 2)
            sl = slice(tc * 512, (tc + 1) * 512)
            for br in range(3):
                P.dma('sp', gt[gi][:, br, :], gt_d[:, br * 8 + mc, sl], writes=[('gt', gi)], key=('gt', gi))
            return gi

        for tc in range(NTC):
            load(tc)
            b = 0
            oc = oT[2][b]
            for h in range(8):
                si = nxt('sq', 2)
                P.op('dve', lambda e, h=h, si=si: e.tensor_tensor(sq[si][:], oc[:, h, :], oc[:, h, :], ALU.mult), reads=[('oT', 2, b)], writes=[('sq', si)])
                g = nxt('g', 4)
                P.mm(ps[0:64, g, :], on64[:], sq[si][:], True, True, reads=['on64', ('sq', si)], writes=[('ps', g)])
                ri = nxt('rs', 2)
                P.op('act', lambda e, g=g, ri=ri: e.activation(rs[ri][:], ps[0:64, g, :], AF.Sqrt, bias=1e-6), reads=[('ps', g)], writes=[('rs', ri)])
                P.op('dve', lambda e, ri=ri: e.reciprocal(rs[ri][:], rs[ri][:]), reads=[('rs', ri)], writes=[('rs', ri)])
                P.op('dve', lambda e, h=h, ri=ri: e.scalar_tensor_tensor(rs[ri][:], oc[:, h, :], gn[:, h:h + 1], rs[ri][:], ALU.mult, ALU.mult),
                     reads=[('oT', 2, b), 'gn', ('rs', ri)], writes=[('rs', ri)])
                P.op('pool', lambda e, h=h, ri=ri: e.tensor_tensor(oc[:, h, :], rs[ri][:], cg[b][:, h, :], ALU.mult),
                     reads=[('rs', ri), ('cg', b)], writes=[('oT', 2, b)])
            gnext = load_gt(tc, 0)
            for mc in range(8):
                gcur = gnext
                if mc + 1 < 8:
                    gnext = load_gt(tc, mc + 1)
                gb = []
                for br in range(3):
                    g = nxt('g', 4)
                    for h in range(8):
                        P.mm(ps[:, g, :], wp[br][:, h, mc * 128:(mc + 1) * 128], oT[br][b][:, h, :], h == 0, h == 7,
                             reads=[('wp', br), ('oT', br, b)], writes=[('ps', g)])
                    gb.append(g)
                ti = nxt('t', 2)
                for br, tt in enumerate((tA, tB, tC)):
                    P.op('dve', lambda e, br=br, tt=tt, ti=ti, g=gb[br], gcur=gcur: e.tensor_tensor(tt[ti][:], ps[:, g, :], gt[gcur][:, br, :], ALU.mult),
                         reads=[('ps', gb[br]), ('gt', gcur)], writes=[('t', br, ti)])
                P.op('pool', lambda e, ti=ti: e.tensor_tensor(tA[ti][:], tA[ti][:], tB[ti][:], ALU.add), reads=[('t', 0, ti), ('t', 1, ti)], writes=[('t', 0, ti)])
                P.op('pool', lambda e, ti=ti, mc=mc: e.tensor_tensor(mT[:, mc, :], tA[ti][:], tC[ti][:], ALU.add), reads=[('t', 0, ti), ('t', 2, ti)], writes=[('mT', mc)])
            for tt in range(4):
                tix = tc * 4 + tt
                xi = nxt('x', 2)
                P.dma('sp', xin[xi][:], x_d[tix * 128:(tix + 1) * 128, :], writes=[('xin', xi)], key=('xin', xi))
                yi = nxt('y', 2)
                for half in range(2):
                    g = 4 + nxt('go', 4)
                    for k in range(8):
                        P.mm(ps[:, g, :], mT[:, k, tt * 128:(tt + 1) * 128], wo[:, k, half * 512:(half + 1) * 512], k == 0, k == 7,
                             reads=[('mT', k), 'wo'], writes=[('ps', g)])
                    P.op('dve', lambda e, g=g, xi=xi, yi=yi, half=half: e.scalar_tensor_tensor(
                        yb[yi][:, half * 512:(half + 1) * 512], xin[xi][:, half * 512:(half + 1) * 512], ALPHA, ps[:, g, :], ALU.mult, ALU.add),
                        reads=[('xin', xi), ('ps', g)], writes=[('yb', yi)])
                hi = nxt('h', 2)
                emit_layernorm(P, yb[yi][:], hb[hi][:], lng[:], lnb[:], stats, mv, rstd, ('yb', yi), ('hb', hi), ('lng', 'lnb'), 'ln')
                P.dma('sp', h_d[tix * 128:(tix + 1) * 128, :], hb[hi][:], reads=[('hb', hi)], key='out')
    return dict(h=h_d)


def _fm64(a_T):
    return np.ascontiguousarray(a_T.reshape(8, 64, -1).transpose(1, 0, 2))


def k3a_inputs(oaT, obT, ocT, cgT, gT, x, wpa, wpb, wpc, wo, gn, lng, lnb):
    T = TOK
    f = lambda w: np.ascontiguousarray(w.reshape(8, 64, D_MODEL).transpose(1, 0, 2)).astype(np.float32)
    return dict(oaT=oaT, obT=obT, ocT=ocT, cgT=_fm64(cgT),
                gT=np.ascontiguousarray(gT.reshape(24, 128, T).transpose(1, 0, 2)),
                x=np.ascontiguousarray(x), wp0=f(wpa), wp1=f(wpb), wp2=f(wpc),
                wo=np.ascontiguousarray(wo.reshape(8, 128, D_MODEL).transpose(1, 0, 2)).astype(np.float32),
                gn=np.ascontiguousarray(gn.reshape(8, 64).T).astype(np.float32),
                lng=np.ascontiguousarray(lng.reshape(1, -1)).astype(np.float32),
                lnb=np.ascontiguousarray(lnb.reshape(1, -1)).astype(np.float32))


def build_k3b(e0=0, ne=N_EXP, first=True, last=True):
    nc = bass.Bass("TRN2", target_bir_lowering=False)
    P = Prog(nc)
    phase_k3b(nc, P, '', e0, ne, first, last)
    P.emit(final_keys=['out'])
    return nc, P


def phase_k3b(nc, P, px, e0=0, ne=N_EXP, first=True, last=True, ext=None):
    ext = ext or {}
    T = TOK
    QC = 1024
    NQ = T // QC
    din = lambda name, shape, dt: ext[name] if name in ext else nc.dram_tensor(px + name, shape, dt, kind="ExternalInput").ap()
    h_d = din("h", [T, D_MODEL], F32)
    wr_d = din("wr", [128, 8, N_EXP], F32)
    br_d = din("br", [1, N_EXP], F32)
    wgu_d = din("wgu", [ne, D_MODEL, 2 * D_MODEL], F32)
    bgu_d = din("bgu", [128, N_EXP, 16], F32)
    wd_d = din("wd", [ne, D_MODEL, D_MODEL], F32)
    accin_d = None if first else din("acc_in", [T, D_MODEL], F32)
    bd_d = din("bd", [N_EXP, D_MODEL], F32)
    lng_d = din("lng", [1, D_MODEL], F32)
    lnb_d = din("lnb", [1, D_MODEL], F32)
    ident_d = din("ident", [128, 128], F32)
    out_d = nc.dram_tensor(px + ("out" if last else "acc_out"), [T, D_MODEL], F32, kind="ExternalOutput").ap()

    from contextlib import ExitStack
    with ExitStack() as st:
        def sb(name, shape, dt):
            return st.enter_context(nc.sbuf_tensor(px + name, shape, dt))
        hTc = sb("hTc", [128, 8, QC], BF16)
        acc = sb("acc", [128, QC // 128, D_MODEL], F32)
        actT = sb("actT", [128, 8, QC], BF16)
        gst = [sb("gst%d" % i, [128, 8, 512], F32) for i in range(2)]
        gbf = [sb("gbf%d" % i, [128, 8, 512], BF16) for i in range(2)]
        dst = gst
        dbf = [sb("dbf%d" % i, [128, 8, D_MODEL], BF16) for i in range(2)]
        big = [sb("big%d" % i, [128, D_MODEL], F32) for i in range(4)]
        hT32 = sb("hT32", [128, 8, 128], F32)
        wr = sb("wrs", [128, 8, N_EXP], F32)
        brb = sb("brb", [128, N_EXP], F32)
        bgu = sb("bgus", [128, N_EXP, 16], F32)
        bd = sb("bds", [N_EXP, D_MODEL], F32)
        lng = sb("lngs", [128, D_MODEL], F32)
        lnb = sb("lnbs", [128, D_MODEL], F32)
        ident = sb("idents", [128, 128], F32)
        Gc = sb("Gc", [128, QC // 128, N_EXP], F32)
        GTc = sb("GTc", [N_EXP, QC], F32)
        lg = sb("lg", [128, N_EXP], F32)
        ex = sb("ex", [128, N_EXP], F32)
        m8 = sb("m8", [128, 8], F32)
        sm = sb("sm", [128, 4], F32)
        tG = [sb("tG%d" % i, [128, 512], F32) for i in range(2)]
        tS = [sb("tS%d" % i, [128, 512], F32) for i in range(2)]
        tU = [sb("tU%d" % i, [128, 512], F32) for i in range(2)]
        stats = sb("stats", [128, 2, 6], F32)
        mv = sb("mv", [128, 2], F32)
        rstd = sb("rstd", [128, 1], F32)
        ps = st.enter_context(nc.psum_tensor(px + "ps", [128, 8, 512], F32))
        cnt = {}

        def nxt(k, n):
            v = cnt.get(k, 0) % n
            cnt[k] = cnt.get(k, 0) + 1
            return v

        for i, (t, d, kn) in enumerate([(wr, wr_d, 'wrs'), (bgu, bgu_d, 'bgus'), (bd, bd_d, 'bds'), (ident, ident_d, 'idents')]):
            P.dma('sp', t[:], d, writes=[kn], key='c%d' % i)
        P.dma('sp', brb[:], br_d.partition_broadcast(128), writes=['brb'], key='c4')
        P.dma('sp', lng[:], lng_d.partition_broadcast(128), writes=['lng'], key='c5')
        P.dma('sp', lnb[:], lnb_d.partition_broadcast(128), writes=['lnb'], key='c6')

        wgu_v = wgu_d.rearrange("e (c p) n -> e p c n", p=128)
        wd_v = wd_d.rearrange("e (c p) n -> e p c n", p=128)

        def gu_unit(q, e, f2):
            def load():
                b = nxt('gw', 2)
                s_ = nxt('ds', 2)
                P.dma('sp', gst[s_][:, :, 0:256], wgu_v[e - e0, :, :, f2 * 256:(f2 + 1) * 256], writes=[('gst', s_)], key=('gst', s_))
                P.dma('sp', gst[s_][:, :, 256:512], wgu_v[e - e0, :, :, 1024 + f2 * 256:1024 + (f2 + 1) * 256], writes=[('gst', s_)], key=('gst', s_))
                P.op('act', lambda e_: e_.copy(gbf[b][:], gst[s_][:]), reads=[('gst', s_)], writes=[('gbf', b)])
                return b

            def comp(b):
                for fl in range(2):
                    fc = f2 * 2 + fl
                    for tcc in range(QC // 512):
                        tsl = slice(tcc * 512, (tcc + 1) * 512)
                        g1 = nxt('g', 4)
                        for k in range(8):
                            P.mm(ps[:, g1, :], gbf[b][:, k, fl * 128:(fl + 1) * 128], hTc[:, k, tsl], k == 0, k == 7,
                                 reads=[('gbf', b), 'hTc'], writes=[('ps', g1)])
                        g2 = nxt('g', 4)
                        for k in range(8):
                            P.mm(ps[:, g2, :], gbf[b][:, k, 256 + fl * 128:256 + (fl + 1) * 128], hTc[:, k, tsl], k == 0, k == 7,
                                 reads=[('gbf', b), 'hTc'], writes=[('ps', g2)])
                        ti = nxt('t', 2)
                        P.op('dve', lambda e_, g1=g1, ti=ti, fc=fc: e_.tensor_scalar(tG[ti][:], ps[:, g1, :], bgu[:, e, fc:fc + 1], 7.0, ALU.add, ALU.min),
                             reads=[('ps', g1), 'bgus'], writes=[('tG', ti)])
                        P.op('act', lambda e_, ti=ti: e_.activation(tS[ti][:], tG[ti][:], AF.Sigmoid, scale=1.702), reads=[('tG', ti)], writes=[('tS', ti)])
                        P.op('dve', lambda e_, g2=g2, ti=ti, fc=fc: e_.tensor_scalar(tU[ti][:], ps[:, g2, :], bgu[:, e, 8 + fc:9 + fc], 7.0, ALU.add, ALU.min),
                             reads=[('ps', g2), 'bgus'], writes=[('tU', ti)])
                        P.op('dve', lambda e_, ti=ti: e_.tensor_scalar(tU[ti][:], tU[ti][:], -7.0, 1.0, ALU.max, ALU.add), reads=[('tU', ti)], writes=[('tU', ti)])
                        P.op('pool', lambda e_, ti=ti: e_.tensor_tensor(tG[ti][:], tG[ti][:], tS[ti][:], ALU.mult), reads=[('tG', ti), ('tS', ti)], writes=[('tG', ti)])
                        P.op('pool', lambda e_, ti=ti, fc=fc, tsl=tsl: e_.tensor_tensor(actT[:, fc, tsl], tG[ti][:], tU[ti][:], ALU.mult),
                             reads=[('tG', ti), ('tU', ti)], writes=[('actT', fc)])
            return load, comp

        def down_unit(q, e):
            def load():
                b = nxt('dw', 2)
                for half in range(2):
                    s_ = nxt('ds', 2)
                    P.dma('sp', dst[s_][:], wd_v[e - e0, :, :, half * 512:(half + 1) * 512], writes=[('gst', s_)], key=('gst', s_))
                    P.op('act', lambda e_, s_=s_, half=half: e_.copy(dbf[b][:, :, half * 512:(half + 1) * 512], dst[s_][:]), reads=[('gst', s_)], writes=[('dbf', b)])
                return b

            def comp(b):
                for tt in range(QC // 128):
                    for half in range(2):
                        g = 4 + nxt('gd', 4)
                        for fc in range(8):
                            P.mm(ps[:, g, :], actT[:, fc, tt * 128:(tt + 1) * 128], dbf[b][:, fc, half * 512:(half + 1) * 512], fc == 0, fc == 7,
                                 reads=[('actT', fc), ('dbf', b)], writes=[('ps', g)])
                        P.op('dve', lambda e_, g=g, tt=tt, half=half: e_.scalar_tensor_tensor(
                            acc[:, tt, half * 512:(half + 1) * 512], ps[:, g, :], Gc[:, tt, e:e + 1], acc[:, tt, half * 512:(half + 1) * 512], ALU.mult, ALU.add),
                            reads=[('ps', g), 'Gc', ('acc', tt)], writes=[('acc', tt)])
            return load, comp

        units = []
        for q in range(NQ):
            for e in range(e0, e0 + ne):
                for f2 in range(4):
                    units.append(('gu', q, e, f2) + gu_unit(q, e, f2))
                units.append(('dn', q, e, None) + down_unit(q, e))

        def prologue(q):
            for tt in range(QC // 128):
                tix = q * (QC // 128) + tt
                bi = nxt('big', 4)
                P.dma('sp', big[bi][:], h_d[tix * 128:(tix + 1) * 128, :], writes=[('big', bi)], key=('big', bi))
                for half in range(2):
                    g = nxt('g', 4)
                    for j in range(4):
                        k = half * 4 + j
                        P.tr(ps[:, g, j * 128:(j + 1) * 128], big[bi][:, k * 128:(k + 1) * 128], ident[:], reads=[('big', bi), 'idents'], writes=[('ps', g)])
                    P.op('act', lambda e_, g=g, half=half: e_.copy(hT32[:, half * 4:(half + 1) * 4, :], ps[:, g, :].rearrange("p (j t) -> p j t", j=4)),
                         reads=[('ps', g)], writes=[('hT32', half)])
                    P.op('dve', lambda e_, half=half, tt=tt: e_.tensor_copy(hTc[:, half * 4:(half + 1) * 4, tt * 128:(tt + 1) * 128],
                                                                          hT32[:, half * 4:(half + 1) * 4, :]),
                         reads=[('hT32', half)], writes=['hTc'])
                g = nxt('g', 4)
                for k in range(8):
                    P.mm(ps[:, g, 0:N_EXP], hT32[:, k, :], wr[:, k, :], k == 0, k == 7, reads=[('hT32', k // 4), 'wrs'], writes=[('ps', g)])
                P.op('dve', lambda e_, g=g: e_.tensor_tensor(lg[:], ps[:, g, 0:N_EXP], brb[:], ALU.add), reads=[('ps', g), 'brb'], writes=['lg'])
                P.op('dve', lambda e_: e_.max(m8[:], lg[:]), reads=['lg'], writes=['m8'])
                P.op('dve', lambda e_: e_.tensor_scalar(sm[:, 0:1], m8[:, 0:1], -1.0, None, ALU.mult), reads=['m8'], writes=['sm0'])
                P.op('act', lambda e_: e_.activation(ex[:], lg[:], AF.Exp, bias=sm[:, 0:1]), reads=['lg', 'sm0'], writes=['ex'])
                P.op('dve', lambda e_: e_.scalar_tensor_tensor(ex[:], lg[:], m8[:, 3:4], ex[:], ALU.is_ge, ALU.mult), reads=['lg', 'm8', 'ex'], writes=['ex'])
                P.op('dve', lambda e_: e_.reduce_sum(sm[:, 1:2], ex[:], AX.X), reads=['ex'], writes=['sm1'])
                P.op('dve', lambda e_: e_.reciprocal(sm[:, 1:2], sm[:, 1:2]), reads=['sm1'], writes=['sm1'])
                P.op('dve', lambda e_, tt=tt: e_.tensor_scalar(Gc[:, tt, :], ex[:], sm[:, 1:2], None, ALU.mult), reads=['ex', 'sm1'], writes=['Gc'])
                g = nxt('g', 4)
                P.tr(ps[0:N_EXP, g, 0:128], Gc[:, tt, :], ident[:], reads=['Gc', 'idents'], writes=[('ps', g)])
                P.op('act', lambda e_, g=g, tt=tt: e_.copy(GTc[:, tt * 128:(tt + 1) * 128], ps[0:N_EXP, g, 0:128]), reads=[('ps', g)], writes=['GTc'])
                if first:
                    for half in range(2):
                        g = 4 + nxt('gd', 4)
                        P.mm(ps[:, g, :], GTc[:, tt * 128:(tt + 1) * 128], bd[:, half * 512:(half + 1) * 512], True, True, reads=['GTc', 'bds'], writes=[('ps', g)])
                        P.op('act', lambda e_, g=g, tt=tt, half=half: e_.copy(acc[:, tt, half * 512:(half + 1) * 512], ps[:, g, :]), reads=[('ps', g)], writes=[('acc', tt)])
                else:
                    P.dma('sp', acc[:, tt, :], accin_d[tix * 128:(tix + 1) * 128, :], writes=[('acc', tt)], key=('accin', tt))

        def epilogue(q):
            for tt in range(QC // 128):
                tix = q * (QC // 128) + tt
                if not last:
                    P.dma('sp', out_d[tix * 128:(tix + 1) * 128, :], acc[:, tt, :], reads=[('acc', tt)], key='out')
                    continue
                bi = nxt('big', 4)
                P.dma('sp', big[bi][:], h_d[tix * 128:(tix + 1) * 128, :], writes=[('big', bi)], key=('big', bi))
                yi = nxt('big', 4)
                P.op('dve', lambda e_, bi=bi, yi=yi, tt=tt: e_.scalar_tensor_tensor(big[yi][:], big[bi][:], ALPHA, acc[:, tt, :], ALU.mult, ALU.add),
                     reads=[('big', bi), ('acc', tt)], writes=[('big', yi)])
                oi = nxt('big', 4)
                emit_layernorm(P, big[yi][:], big[oi][:], lng[:], lnb[:], stats, mv, rstd, ('big', yi), ('big', oi), ('lng', 'lnb'), 'ln')
                P.dma('sp', out_d[tix * 128:(tix + 1) * 128, :], big[oi][:], reads=[('big', oi)], key='out')

        nb = units[0][4]()
        for ui in range(len(units)):
            kind, q, e, f2, load, comp = units[ui]
            cur = nb
            if kind == 'gu' and e == e0 and f2 == 0:
                prologue(q)
            if ui + 1 < len(units):
                nb = units[ui + 1][4]()
            comp(cur)
            if kind == 'dn' and e == e0 + ne - 1:
                epilogue(q)
    return dict(out=out_d)


def k3b_inputs(h, wr, br, wgu, bgu, wd, bd, lng, lnb, acc_in=None):
    d = _k3b_inputs(h, wr, br, wgu, bgu, wd, bd, lng, lnb)
    if acc_in is not None:
        d['acc_in'] = np.ascontiguousarray(acc_in)
    return d


def _k3b_inputs(h, wr, br, wgu, bgu, wd, bd, lng, lnb):
    return dict(h=np.ascontiguousarray(h), wr=np.ascontiguousarray(wr.reshape(8, 128, N_EXP).transpose(1, 0, 2)).astype(np.float32),
                br=np.ascontiguousarray(br.reshape(1, -1)).astype(np.float32), wgu=wgu,
                bgu=np.ascontiguousarray(bgu.reshape(N_EXP, 16, 128).transpose(2, 0, 1)).astype(np.float32),
                wd=wd, bd=np.ascontiguousarray(bd).astype(np.float32),
                lng=np.ascontiguousarray(lng.reshape(1, -1)).astype(np.float32), lnb=np.ascontiguousarray(lnb.reshape(1, -1)).astype(np.float32),
                ident=np.eye(128, dtype=np.float32))


def build_l2():
    nc = bass.Bass("TRN2", target_bir_lowering=False)
    P = Prog(nc)
    phase_k2a(nc, P, 'a_')
    P.barrier()
    phase_k2b(nc, P, 'b_')
    P.emit(final_keys=['out'])
    return nc, P


def build_l3(next_layer=None):
    nc = bass.Bass("TRN2", target_bir_lowering=False)
    P = Prog(nc)
    o3a = phase_k3a(nc, P, 'a_')
    P.barrier()
    o3b = phase_k3b(nc, P, 'b_', 0, N_EXP, True, True, ext={'h': o3a['h']})
    if next_layer is not None:
        P.barrier()
        phase_k1(nc, P, 'k_', next_layer, ext={'x': o3b['out']})
    P.emit(final_keys=['out'])
    return nc, P


def _launch(nc, in_maps):
    res = run_bass_kernel_spmd(nc, in_maps, core_ids=list(range(NCORES)))
    return res.results


def _pref(px, d):
    return {px + k: v for k, v in d.items()}


def kernel(x, w_in, b_fox_f, hgrn_lb_logits, hgrn_norm_g, w_branch_a, w_branch_b, w_branch_c, w_out,
           ln1_g, ln1_b, w_router, b_router, w_gu, b_gu, w_down, b_down, ln2_g, ln2_b):
    f32 = lambda a: np.ascontiguousarray(np.asarray(a, dtype=np.float32))
    x_cur = f32(x).reshape(-1, D_MODEL)
    lbl = f32(hgrn_lb_logits)
    w_perm = np.ascontiguousarray(f32(w_in[0])[:, K1_COLS])
    r1 = _launch(build_k1(0)[0], [k1_inputs(c, x_cur[c * TOK:(c + 1) * TOK], w_perm, f32(b_fox_f[0]), lbl) for c in range(NCORES)])
    del w_perm
    for l in range(DEPTH):
        catT = lambda nm, b: np.concatenate([r1[2 * b][nm], r1[2 * b + 1][nm]], axis=1)
        cat0 = lambda nm, b: np.concatenate([r1[2 * b][nm], r1[2 * b + 1][nm]], axis=0)
        in2 = []
        for b in range(BATCH):
            f = dict(aqT=catT('aqT', b), akT=catT('akT', b), av=cat0('av', b), fqT=catT('fqT', b), fkT=catT('fkT', b),
                     fv=cat0('fv', b), iqT=catT('iqT', b), ikT=catT('ikT', b), small=cat0('small', b))
            h = dict(qpT=catT('qpT', b), kpT=catT('kpT', b), kp=cat0('kp', b), cv=cat0('cv', b), em=catT('em', b), g=catT('g', b))
            for j in range(2):
                d = _pref('a_', k2a_inputs(j, f['aqT'], f['akT'], f['av'], f['fqT'], f['fkT'], f['fv'], f['iqT'], f['ikT'], f['small']))
                d.update(_pref('b_', k2b_inputs(j, h['qpT'], h['kpT'], h['kp'], h['cv'], h['em'], h['g'])))
                in2.append(d)
            del f, h
        r2 = _launch(build_l2()[0], in2)
        del in2
        nxt_l = l + 1 if l + 1 < DEPTH else None
        wgu_l, wd_l = f32(w_gu[l]), f32(w_down[l])
        if nxt_l is not None:
            w_perm = np.ascontiguousarray(f32(w_in[nxt_l])[:, K1_COLS])
        in3 = []
        for b in range(BATCH):
            oa = np.empty((64, 8, SEQ), dtype=NPBF)
            ob = np.empty((64, 8, SEQ), dtype=NPBF)
            for j in range(2):
                cols = _slot_cols(j)
                oa[:, :, cols] = r2[2 * b + j]['a_oaT']
                ob[:, :, cols] = r2[2 * b + j]['a_obT']
            oc = np.concatenate([r2[2 * b]['b_ocT'], r2[2 * b + 1]['b_ocT']], axis=1)
            for cc in range(2):
                c = 2 * b + cc
                sl = slice(cc * TOK, (cc + 1) * TOK)
                d = _pref('a_', k3a_inputs(np.ascontiguousarray(oa[:, :, sl]), np.ascontiguousarray(ob[:, :, sl]), np.ascontiguousarray(oc[:, :, sl]),
                                           r1[c]['cgT'], r1[c]['gT'], x_cur[c * TOK:(c + 1) * TOK],
                                           f32(w_branch_a[l]), f32(w_branch_b[l]), f32(w_branch_c[l]), f32(w_out[l]),
                                           f32(hgrn_norm_g[l]), f32(ln1_g[l]), f32(ln1_b[l])))
                d3b = _k3b_inputs(np.zeros((1, 1), np.float32), f32(w_router[l]), f32(b_router[l]), wgu_l, f32(b_gu[l]), wd_l, f32(b_down[l]),
                                  f32(ln2_g[l]), f32(ln2_b[l]))
                del d3b['h']
                d.update(_pref('b_', d3b))
                if nxt_l is not None:
                    d1 = k1_inputs(c, np.zeros((1, 1), np.float32), w_perm, f32(b_fox_f[nxt_l]), lbl)
                    del d1['x']
                    d.update(_pref('k_', d1))
                in3.append(d)
        del r1, r2
        r3 = _launch(build_l3(nxt_l)[0], in3)
        del in3
        x_cur = np.concatenate([np.asarray(r3[c]['b_out'], dtype=np.float32) for c in range(NCORES)], axis=0)
        if nxt_l is not None:
            r1 = [{k[2:]: v for k, v in r3[c].items() if k.startswith('k_')} for c in range(NCORES)]
        del r3
    return x_cur.reshape(BATCH, SEQ, D_MODEL)
```

```python
import numpy as np
import ml_dtypes
import concourse.bass as bass
import concourse.mybir as mybir
from concourse.bass_utils import run_bass_kernel_spmd

F32 = mybir.dt.float32
BF16 = mybir.dt.bfloat16
I32 = mybir.dt.int32
ALU = mybir.AluOpType
AF = mybir.ActivationFunctionType
AX = mybir.AxisListType
NPBF = ml_dtypes.bfloat16

D_MODEL = 1024
SEQ = 8192
BATCH = 4
DEPTH = 2
NCORES = 8
TOK = 4096
ALPHA = (2 * DEPTH) ** 0.25
N_EXP = 32

class Prog:
    def __init__(self, nc):
        self.nc = nc
        self.ops = []
        self.last_w = {}
        self.readers = {}

    def op(self, eng, fn, reads=(), writes=(), dma=None):
        idx = len(self.ops)
        deps = set()
        for b in reads:
            w = self.last_w.get(b)
            if w is not None:
                deps.add(w)
        for b in writes:
            w = self.last_w.get(b)
            if w is not None:
                deps.add(w)
            for r in self.readers.get(b, ()):
                deps.add(r)
        for b in writes:
            self.last_w[b] = idx
            self.readers[b] = []
        for b in reads:
            if b not in writes:
                self.readers.setdefault(b, []).append(idx)
        deps.discard(idx)
        self.ops.append(dict(eng=eng, fn=fn, deps=deps, dma=dma))
        return idx

    def dma(self, q, out, in_, reads=(), writes=(), key=None, **kw):
        assert key is not None
        return self.op(q, lambda e: e.dma_start(out=out, in_=in_, **kw), reads, writes, dma=key)

    def mm(self, out, lhsT, rhs, start, stop, reads=(), writes=()):
        return self.op('pe', lambda e: e.matmul(out, lhsT, rhs, start=start, stop=stop), reads, writes)

    def tr(self, out, in_, ident, reads=(), writes=()):
        return self.op('pe', lambda e: e.transpose(out, in_, ident), reads, writes)

    def barrier(self):
        last = {}
        for i, o in enumerate(self.ops):
            if o['fn'] is None:
                continue
            last[('dma', o['dma']) if o['dma'] is not None else ('eng', o['eng'])] = i
        deps = set(last.values())
        for eng in ('pe', 'act', 'dve', 'pool', 'sp'):
            self.ops.append(dict(eng=eng, fn=None, deps=set(deps), dma=None))
        self.last_w = {}
        self.readers = {}

    def emit(self, final_keys=()):
        nc = self.nc
        ops = self.ops
        n = len(ops)
        need = [False] * n
        for o in ops:
            best = {}
            ed = set()
            for d in o['deps']:
                po = ops[d]
                if po['eng'] == 'pe' and o['eng'] == 'pe' and po['dma'] is None and o['dma'] is None:
                    continue
                if po['dma'] is not None:
                    ed.add(d)
                else:
                    best[po['eng']] = max(best.get(po['eng'], -1), d)
            ed.update(best.values())
            o['edeps'] = ed
            for d in ed:
                need[d] = True
        for i, o in enumerate(ops):
            if o['dma'] is not None:
                need[i] = True
        eng_cnt = {}
        key_cnt = {}
        sig = [None] * n
        key_before = [None] * n
        key_events = {}
        for i, o in enumerate(ops):
            if o['dma'] is not None:
                k = o['dma']
                key_cnt[k] = key_cnt.get(k, 0) + 1
                key_events.setdefault(k, []).append(i)
                sig[i] = ('dma:' + str(k), 16 * key_cnt[k])
            elif need[i]:
                e = o['eng']
                eng_cnt[e] = eng_cnt.get(e, 0) + 1
                sig[i] = ('eng:' + e, eng_cnt[e])
        semnames = sorted(set(s[0] for s in sig if s is not None))
        import bisect
        from contextlib import ExitStack
        with ExitStack() as st:
            sems = {nm: st.enter_context(nc.semaphore('s%d' % j)) for j, nm in enumerate(semnames)}
            block = st.enter_context(nc.Block())
            by_eng = {}
            for i, o in enumerate(ops):
                by_eng.setdefault(o['eng'], []).append(i)

            def run(engname, e):
                waited = {}
                for i in by_eng.get(engname, []):
                    o = ops[i]
                    wl = {}
                    for d in o['edeps']:
                        po = ops[d]
                        nm, val = sig[d]
                        if po['dma'] is not None:
                            ev = key_events[po['dma']]
                            cnt = bisect.bisect_left(ev, i)
                            val = 16 * cnt
                        if val > wl.get(nm, 0):
                            wl[nm] = val
                    for nm, val in wl.items():
                        if val > waited.get(nm, 0):
                            e.wait_ge(sems[nm], val)
                            waited[nm] = val
                    if o['fn'] is None:
                        continue
                    ins = o['fn'](e)
                    if sig[i] is not None:
                        nm, val = sig[i]
                        ins.then_inc(sems[nm], 16 if o['dma'] is not None else 1)
                for k in final_keys:
                    ev = key_events.get(k, [])
                    if ev and ops[ev[-1]]['eng'] == engname:
                        e.wait_ge(sems['dma:' + str(k)], 16 * len(ev))

            @block.tensor
            def _(e):
                run('pe', e)

            @block.scalar
            def _(e):
                run('act', e)

            @block.vector
            def _(e):
                run('dve', e)

            @block.gpsimd
            def _(e):
                run('pool', e)

            @block.sync
            def _(e):
                run('sp', e)
        self.stats = dict(n_ops=n, eng_cnt=eng_cnt, n_sems=len(semnames))


SPL = dict(aq=(0, 512), ak=(512, 1024), av=(1024, 1536), iq=(1536, 1792), ik=(1792, 1856), iw=(1856, 1860),
           fq=(1860, 2372), fk=(2372, 2884), fv=(2884, 3396), ff=(3396, 3404), cq=(3404, 3916), cf=(3916, 4428),
           ci=(4428, 4940), cg=(4940, 5452), gt=(5452, 8524))


def _rot_cols(lo, hi):
    c = np.arange(lo, hi)
    base = lo + ((c - lo) // 64) * 64
    return base + ((c - base + 32) % 64)


def k1_weight_columns():
    cols = []
    off = {}
    pos = 0

    def add(name, idx):
        nonlocal pos
        off[name] = (pos, len(idx))
        cols.append(np.asarray(idx))
        pos += len(idx)

    for nm in ('aq', 'ak', 'iq', 'ik'):
        lo, hi = SPL[nm]
        add(nm, np.arange(lo, hi))
        add(nm + '_rot', _rot_cols(lo, hi))
    for nm in ('fq', 'fk', 'cq', 'cf', 'cg', 'gt', 'av', 'fv', 'ci'):
        lo, hi = SPL[nm]
        add(nm, np.arange(lo, hi))
    add('small', np.concatenate([np.arange(*SPL['iw']), np.arange(*SPL['ff'])]))
    return np.concatenate(cols), off


K1_COLS, K1_OFF = k1_weight_columns()
K1_NCOLS = len(K1_COLS)


def rope_tables(positions):
    half = 32
    inv = (10000.0 ** (-np.arange(half, dtype=np.float32) / half)).astype(np.float32)
    ang = positions.astype(np.float32)[None, :] * inv[:, None]
    cos = np.cos(ang).astype(np.float32)
    sin = np.sin(ang).astype(np.float32)
    cosT = np.concatenate([cos, cos, cos, cos], axis=0)
    sinT = np.concatenate([-sin, sin, -sin, sin], axis=0)
    return np.ascontiguousarray(cosT), np.ascontiguousarray(sinT)


def build_k1(layer=0):
    nc = bass.Bass("TRN2", target_bir_lowering=False)
    P = Prog(nc)
    phase_k1(nc, P, '', layer)
    P.emit(final_keys=['out'])
    return nc, P


def phase_k1(nc, P, px, layer=0, ext=None):
    ext = ext or {}
    T = TOK
    NT = T // 128
    NTC = T // 512
    x_d = ext['x'] if 'x' in ext else nc.dram_tensor(px + "x", [T, D_MODEL], F32, kind="ExternalInput").ap()
    w_d = nc.dram_tensor(px + "w", [D_MODEL, K1_NCOLS], F32, kind="ExternalInput").ap()
    cos_d = nc.dram_tensor(px + "cosT", [128, T], F32, kind="ExternalInput").ap()
    sin_d = nc.dram_tensor(px + "sinT", [128, T], F32, kind="ExternalInput").ap()
    bfox_d = nc.dram_tensor(px + "bfox", [1, 8], F32, kind="ExternalInput").ap()
    lbl_d = nc.dram_tensor(px + "lbl", [128, 2, 4], F32, kind="ExternalInput").ap()
    ident_d = nc.dram_tensor(px + "ident", [128, 128], F32, kind="ExternalInput").ap()
    rmask_d = nc.dram_tensor(px + "rmask", [128, 512], F32, kind="ExternalInput").ap()

    def out_t(name, shape, dt):
        return nc.dram_tensor(px + name, shape, dt, kind="ExternalOutput").ap()

    o_aqT = out_t("aqT", [512, T], BF16)
    o_akT = out_t("akT", [512, T], BF16)
    o_iqT = out_t("iqT", [256, T], BF16)
    o_ikT = out_t("ikT", [64, T], BF16)
    o_fqT = out_t("fqT", [512, T], BF16)
    o_fkT = out_t("fkT", [512, T], BF16)
    o_av = out_t("av", [T, 512], BF16)
    o_fv = out_t("fv", [T, 512], BF16)
    o_cv = out_t("cv", [T, 512], BF16)
    o_small = out_t("small", [T, 12], F32)
    o_qpT = out_t("qpT", [512, T], BF16)
    o_kpT = out_t("kpT", [512, T], BF16)
    o_kp = out_t("kp", [T, 512], BF16)
    o_em = out_t("em", [512, T // 64], F32)
    o_g = out_t("g", [512, T // 64], F32)
    o_cgT = out_t("cgT", [512, T], BF16)
    o_gT = out_t("gT", [3072, T], BF16)

    from contextlib import ExitStack
    with ExitStack() as st:
        def sb(name, shape, dt):
            return st.enter_context(nc.sbuf_tensor(px + name, shape, dt))
        xT = sb("xT", [128, 8, T], BF16)
        cosT = sb("cosT_s", [128, T], F32)
        sinT = sb("sinT_s", [128, T], F32)
        ident = sb("ident_s", [128, 128], F32)
        identb = sb("identb", [128, 128], BF16)
        rmask = sb("rmask_s", [128, 512], F32)
        lb = sb("lb_s", [128, 4], F32)
        oml = sb("oml", [128, 4], F32)
        noml = sb("noml", [128, 4], F32)
        bfox = sb("bfox_s", [128, 8], F32)
        xin = [sb("xin%d" % i, [128, D_MODEL], F32) for i in range(2)]
        xbf = [sb("xbf%d" % i, [128, D_MODEL], BF16) for i in range(2)]
        wst = [sb("wst%d" % i, [128, 8, 512], F32) for i in range(2)]
        wbf = [sb("wbf%d" % i, [128, 8, 512], BF16) for i in range(2)]
        t1 = [sb("t1_%d" % i, [128, 512], F32) for i in range(2)]
        t2 = [sb("t2_%d" % i, [128, 512], F32) for i in range(2)]
        t3 = [sb("t3_%d" % i, [128, 512], F32) for i in range(2)]
        t4 = [sb("t4_%d" % i, [128, 512], F32) for i in range(2)]
        ob = [sb("ob%d" % i, [128, 512], BF16) for i in range(4)]
        ob2 = [sb("ob2_%d" % i, [128, 512], BF16) for i in range(2)]
        osm = [sb("osm%d" % i, [128, 12], F32) for i in range(2)]
        emg = sb("emg", [128, 2, 4, T // 64], F32)
        ps = st.enter_context(nc.psum_tensor(px + "ps", [128, 8, 512], F32))
        psb = ps
        cnt = dict(ps=0, ob=0, ob2=0, t=0, w=0, x=0, osm=0)

        def nxt(k, n):
            v = cnt[k] % n
            cnt[k] += 1
            return v

        P.dma('sp', cosT[:], cos_d, writes=['cosT'], key='c0')
        P.dma('sp', sinT[:], sin_d, writes=['sinT'], key='c1')
        P.dma('sp', ident[:], ident_d, writes=['ident'], key='c2')
        P.dma('sp', rmask[:], rmask_d, writes=['rmask'], key='c3')
        lbl = sb("lbl_s", [128, 2, 4], F32)
        P.dma('sp', lbl[:], lbl_d, writes=['lbl'], key='c4')
        if layer == 0:
            P.op('dve', lambda e: e.memset(lb[:], 0.0), writes=['lb'])
        else:
            P.op('dve', lambda e: e.tensor_tensor(lb[:], lbl[:, 1, :], lbl[:, 0, :], ALU.subtract), reads=['lbl'], writes=['lb'])
            P.op('act', lambda e: e.activation(lb[:], lb[:], AF.Sigmoid), reads=['lb'], writes=['lb'])
        P.dma('sp', bfox[:], bfox_d.partition_broadcast(128), writes=['bfox'], key='c5')
        P.op('dve', lambda e: e.tensor_copy(identb[:], ident[:]), reads=['ident'], writes=['identb'])
        P.op('dve', lambda e: e.tensor_scalar(oml[:], lb[:], -1.0, 1.0, ALU.mult, ALU.add), reads=['lb'], writes=['oml'])
        P.op('dve', lambda e: e.tensor_scalar(noml[:], lb[:], 1.0, -1.0, ALU.mult, ALU.add), reads=['lb'], writes=['noml'])

        for ti in range(NT):
            b = nxt('x', 2)
            P.dma('sp', xin[b][:], x_d[ti * 128:(ti + 1) * 128, :], writes=[('xin', b)], key=('xin', b))
            P.op('act', lambda e, b=b: e.copy(xbf[b][:], xin[b][:]), reads=[('xin', b)], writes=[('xbf', b)])
            for half in range(2):
                pb = nxt('ps', 8)
                pst = ps[:, pb, :].bitcast(BF16)
                for j in range(4):
                    k = half * 4 + j
                    P.tr(pst[:, j * 128:(j + 1) * 128], xbf[b][:, k * 128:(k + 1) * 128], identb[:],
                         reads=[('xbf', b), 'identb'], writes=[('ps', pb)])
                P.op('dve', lambda e, pst=pst, half=half, ti=ti: e.tensor_copy(
                    xT[:, half * 4:(half + 1) * 4, ti * 128:(ti + 1) * 128],
                    pst[:, 0:512].rearrange("p (j t) -> p j t", j=4)),
                    reads=[('ps', pb)], writes=[('xT', ti)])

        w_v = w_d.rearrange("(c p) n -> p c n", p=128)

        def load_w(col0, ncols):
            b = nxt('w', 2)
            P.dma('sp', wst[b][:, :, 0:ncols], w_v[:, :, col0:col0 + ncols], writes=[('wst', b)], key=('wst', b))
            P.op('pool', lambda e: e.tensor_copy(wbf[b][:, :, 0:ncols], wst[b][:, :, 0:ncols]),
                 reads=[('wst', b)], writes=[('wbf', b)])
            return b

        def fm_matmul(wb, c0, m, tc):
            pb = nxt('ps', 8)
            for k in range(8):
                P.mm(ps[0:m, pb, :], wbf[wb][:, k, c0:c0 + m], xT[:, k, tc * 512:(tc + 1) * 512], k == 0, k == 7,
                     reads=[('wbf', wb)] + [('xT', tc * 4 + i) for i in range(4)], writes=[('ps', pb)])
            return pb

        all_xT = [('xT', i) for i in range(NT)]

        jobs = []
        def rope_group(nm, o_d):
            s0, n = K1_OFF[nm]
            r0, _ = K1_OFF[nm + '_rot']
            nblk = max(1, n // 128)
            m = min(n, 128)
            for bi in range(nblk):
              def load(bi=bi):
                b = nxt('w', 2)
                P.dma('sp', wst[b][:, :, 0:m], w_v[:, :, s0 + bi * 128:s0 + bi * 128 + m], writes=[('wst', b)], key=('wst', b))
                P.dma('sp', wst[b][:, :, 128:128 + m], w_v[:, :, r0 + bi * 128:r0 + bi * 128 + m], writes=[('wst', b)], key=('wst', b))
                P.op('pool', lambda e, b=b: e.tensor_copy(wbf[b][:, :, 0:256], wst[b][:, :, 0:256]),
                     reads=[('wst', b)], writes=[('wbf', b)])
                return b
              def comp(b, bi=bi):
                for tc in range(NTC):
                    p0 = fm_matmul(b, 0, m, tc)
                    p1 = fm_matmul(b, 128, m, tc)
                    tb = nxt('t', 2)
                    sl = slice(tc * 512, (tc + 1) * 512)
                    P.op('dve', lambda e, p0=p0, tb=tb, sl=sl: e.tensor_tensor(t1[tb][0:m, :], ps[0:m, p0, :], cosT[0:m, sl], ALU.mult),
                         reads=[('ps', p0), 'cosT'], writes=[('t1', tb)])
                    P.op('dve', lambda e, p1=p1, tb=tb, sl=sl: e.tensor_tensor(t2[tb][0:m, :], ps[0:m, p1, :], sinT[0:m, sl], ALU.mult),
                         reads=[('ps', p1), 'sinT'], writes=[('t2', tb)])
                    o = nxt('ob', 4)
                    P.op('pool', lambda e, tb=tb, o=o: e.tensor_tensor(ob[o][0:m, :], t1[tb][0:m, :], t2[tb][0:m, :], ALU.add),
                         reads=[('t1', tb), ('t2', tb)], writes=[('ob', o)])
                    P.dma('sp', o_d[bi * 128:bi * 128 + m, sl], ob[o][0:m, :], reads=[('ob', o)], key='out')
              jobs.append((load, comp))

        rope_group('aq', o_aqT)
        rope_group('ak', o_akT)
        rope_group('iq', o_iqT)
        rope_group('ik', o_ikT)

        def plain_group(nm, o_d, func):
            s0, n = K1_OFF[nm]
            for g0 in range(0, n, 512):
                gn = min(512, n - g0)
                def load(g0=g0, gn=gn):
                    return load_w(s0 + g0, gn)
                def comp(b, g0=g0, gn=gn):
                  for bi in range(gn // 128):
                    for tc in range(NTC):
                        p0 = fm_matmul(b, bi * 128, 128, tc)
                        o = nxt('ob', 4)
                        sl = slice(tc * 512, (tc + 1) * 512)
                        P.op('act', lambda e, p0=p0, o=o: e.activation(ob[o][:], ps[:, p0, :], func),
                             reads=[('ps', p0)], writes=[('ob', o)])
                        r0 = g0 + bi * 128
                        P.dma('sp', o_d[r0:r0 + 128, sl], ob[o][:], reads=[('ob', o)], key='out')
                jobs.append((load, comp))

        plain_group('fq', o_fqT, AF.Copy)
        plain_group('fk', o_fkT, AF.Copy)
        plain_group('cg', o_cgT, AF.Sigmoid)
        plain_group('gt', o_gT, AF.Sigmoid)

        sq0, _ = K1_OFF['cq']
        sf0, _ = K1_OFF['cf']
        for pr in range(4):
          def load(pr=pr):
            b = nxt('w', 2)
            P.dma('sp', wst[b][:, :, 0:128], w_v[:, :, sq0 + pr * 128:sq0 + (pr + 1) * 128], writes=[('wst', b)], key=('wst', b))
            P.dma('sp', wst[b][:, :, 128:256], w_v[:, :, sf0 + pr * 128:sf0 + (pr + 1) * 128], writes=[('wst', b)], key=('wst', b))
            P.op('pool', lambda e, b=b: e.tensor_copy(wbf[b][:, :, 0:256], wst[b][:, :, 0:256]),
                 reads=[('wst', b)], writes=[('wbf', b)])
            return b
          def comp(b, pr=pr):
            for tc in range(NTC):
                pq = fm_matmul(b, 0, 128, tc)
                pf = fm_matmul(b, 128, 128, tc)
                tb = nxt('t', 2)
                sl = slice(tc * 512, (tc + 1) * 512)
                P.op('act', lambda e, pf=pf, tb=tb: e.activation(t1[tb][:], ps[:, pf, :], AF.Sigmoid),
                     reads=[('ps', pf)], writes=[('t1', tb)])
                P.op('act', lambda e, tb=tb, pr=pr: e.activation(t2[tb][:], t1[tb][:], AF.Ln, bias=lb[:, pr:pr + 1], scale=oml[:, pr:pr + 1]),
                     reads=[('t1', tb), 'lb', 'oml'], writes=[('t2', tb)])
                P.op('pool', lambda e, tb=tb, pr=pr: e.tensor_scalar(t3[tb][:], t1[tb][:], noml[:, pr:pr + 1], oml[:, pr:pr + 1], ALU.mult, ALU.add),
                     reads=[('t1', tb), 'oml', 'noml'], writes=[('t3', tb)])
                P.op('dve', lambda e, tb=tb: e.tensor_tensor_scan(t4[tb][:], rmask[:], t2[tb][:], 0.0, ALU.mult, ALU.add),
                     reads=[('t2', tb), 'rmask'], writes=[('t4', tb)])
                b3 = t4[tb][:].rearrange("p (c t) -> p c t", t=64)
                P.op('act', lambda e, b3=b3, pr=pr, tc=tc: e.activation(emg[:, 0, pr, tc * 8:(tc + 1) * 8], b3[:, :, 31], AF.Exp),
                     reads=[('t4', tb)], writes=[('emg', pr)])
                P.op('dve', lambda e, tb=tb, b3=b3: e.tensor_tensor(t2[tb][:].rearrange("p (c t) -> p c t", t=64), b3,
                                                                   b3[:, :, 31:32].to_broadcast([128, 8, 64]), ALU.subtract),
                     reads=[('t4', tb)], writes=[('t2', tb)])
                P.op('act', lambda e, tb=tb: e.activation(t4[tb][:], t2[tb][:], AF.Exp),
                     reads=[('t2', tb)], writes=[('t4', tb)])
                P.op('act', lambda e, tb=tb: e.activation(t1[tb][:], t2[tb][:], AF.Exp, scale=-1.0),
                     reads=[('t2', tb)], writes=[('t1', tb)])
                P.op('pool', lambda e, tb=tb, pr=pr, tc=tc: e.tensor_copy(emg[:, 1, pr, tc * 8:(tc + 1) * 8],
                                                                       t4[tb][:].rearrange("p (c t) -> p c t", t=64)[:, :, 63]),
                     reads=[('t4', tb)], writes=[('emg', pr)])
                o = nxt('ob', 4)
                P.op('dve', lambda e, pq=pq, tb=tb, o=o: e.tensor_tensor(ob[o][:], ps[:, pq, :], t4[tb][:], ALU.mult),
                     reads=[('ps', pq), ('t4', tb)], writes=[('ob', o)])
                P.dma('sp', o_qpT[pr * 128:(pr + 1) * 128, sl], ob[o][:], reads=[('ob', o)], key='out')
                o2 = nxt('ob', 4)
                P.op('pool', lambda e, tb=tb, o2=o2: e.tensor_tensor(ob[o2][:], t3[tb][:], t1[tb][:], ALU.mult),
                     reads=[('t3', tb), ('t1', tb)], writes=[('ob', o2)])
                P.dma('sp', o_kpT[pr * 128:(pr + 1) * 128, sl], ob[o2][:], reads=[('ob', o2)], key='out')
                pb = nxt('ps', 8)
                pst = ps[:, pb, :].bitcast(BF16)
                for j in range(4):
                    P.tr(pst[:, j * 128:(j + 1) * 128], ob[o2][:, j * 128:(j + 1) * 128], identb[:],
                         reads=[('ob', o2), 'identb'], writes=[('ps', pb)])
                o3 = nxt('ob2', 2)
                P.op('act', lambda e, pst=pst, o3=o3: e.copy(ob2[o3][:], pst[:, 0:512]),
                     reads=[('ps', pb)], writes=[('ob2', o3)])
                P.dma('sp', o_kp[tc * 512:(tc + 1) * 512, pr * 128:(pr + 1) * 128].rearrange("(j t) c -> t j c", t=128),
                      ob2[o3][:].rearrange("t (j c) -> t j c", j=4), reads=[('ob2', o3)], key='out')
            P.dma('sp', o_em[pr * 128:(pr + 1) * 128, :], emg[:, 0, pr, :], reads=[('emg', pr)], key='out')
            P.dma('sp', o_g[pr * 128:(pr + 1) * 128, :], emg[:, 1, pr, :], reads=[('emg', pr)], key='out')
          jobs.append((load, comp))

        for nm, o_d in (('av', o_av), ('fv', o_fv), ('ci', o_cv)):
            s0, n = K1_OFF[nm]
            def load(s0=s0):
                return load_w(s0, 512)
            def comp(b, o_d=o_d):
              for ti in range(NT):
                pb = nxt('ps', 8)
                for k in range(8):
                    P.mm(ps[:, pb, :], xT[:, k, ti * 128:(ti + 1) * 128], wbf[b][:, k, :], k == 0, k == 7,
                         reads=[('wbf', b), ('xT', ti)], writes=[('ps', pb)])
                o = nxt('ob', 4)
                P.op('act', lambda e, pb=pb, o=o: e.copy(ob[o][:], ps[:, pb, :]), reads=[('ps', pb)], writes=[('ob', o)])
                P.dma('sp', o_d[ti * 128:(ti + 1) * 128, :], ob[o][:], reads=[('ob', o)], key='out')
            jobs.append((load, comp))

        s0, n = K1_OFF['small']
        def load(s0=s0):
            return load_w(s0, 12)
        def comp(b):
          for ti in range(NT):
            pb = nxt('ps', 8)
            for k in range(8):
                P.mm(ps[:, pb, 0:12], xT[:, k, ti * 128:(ti + 1) * 128], wbf[b][:, k, 0:12], k == 0, k == 7,
                     reads=[('wbf', b), ('xT', ti)], writes=[('ps', pb)])
            o = nxt('osm', 2)
            P.op('dve', lambda e, pb=pb, o=o: e.tensor_scalar(osm[o][:, 0:4], ps[:, pb, 0:4], 1.0 / 16.0, None, ALU.mult),
                 reads=[('ps', pb)], writes=[('osm', o)])
            P.op('dve', lambda e, pb=pb, o=o: e.tensor_tensor(osm[o][:, 4:12], ps[:, pb, 4:12], bfox[:], ALU.add),
                 reads=[('ps', pb), 'bfox'], writes=[('osm', o)])
            P.op('act', lambda e, o=o: e.activation(osm[o][:, 4:12], osm[o][:, 4:12], AF.Sigmoid),
                 reads=[('osm', o)], writes=[('osm', o)])
            P.op('act', lambda e, o=o: e.activation(osm[o][:, 4:12], osm[o][:, 4:12], AF.Ln),
                 reads=[('osm', o)], writes=[('osm', o)])
            P.dma('sp', o_small[ti * 128:(ti + 1) * 128, :], osm[o][:], reads=[('osm', o)], key='out')
        jobs.append((load, comp))

        nb = jobs[0][0]()
        for ji in range(len(jobs)):
            cur = nb
            if ji + 1 < len(jobs):
                nb = jobs[ji + 1][0]()
            jobs[ji][1](cur)

    return dict(aqT=o_aqT)


def k1_inputs(core, x_slice, w_perm, b_fox, lb_logits):
    pos = (np.arange(TOK) + (core % 2) * TOK).astype(np.float32)
    cosT, sinT = rope_tables(pos)
    rmask = np.ones((128, 512), np.float32)
    rmask[:, ::64] = 0.0
    lbl = np.ascontiguousarray(lb_logits.reshape(2, 4, 128).transpose(2, 0, 1)).astype(np.float32)
    return dict(x=np.ascontiguousarray(x_slice), w=w_perm, cosT=cosT, sinT=sinT,
                bfox=np.ascontiguousarray(b_fox.reshape(1, 8)).astype(np.float32), lbl=lbl,
                ident=np.eye(128, dtype=np.float32), rmask=rmask)


NSLOT = 8
NBIS = 16
TOPK = 256.0
NEG = -1.0e30


def build_k2a():
    nc = bass.Bass("TRN2", target_bir_lowering=False)
    P = Prog(nc)
    phase_k2a(nc, P, '')
    P.emit(final_keys=['out'])
    return nc, P


def phase_k2a(nc, P, px, ext=None):
    ext = ext or {}
    S = SEQ
    QT = NSLOT * 512
    din = lambda name, shape, dt: ext[name] if name in ext else nc.dram_tensor(px + name, shape, dt, kind="ExternalInput").ap()
    aq_d = din("aq", [4, 128, QT], BF16)
    ak_d = din("ak", [4, 128, S], BF16)
    av_d = din("av", [4, 128, 64, 130], BF16)
    fq_d = din("fq", [4, 128, QT], BF16)
    fk_d = din("fk", [4, 128, S], BF16)
    fv_d = din("fv", [4, 128, 64, 130], BF16)
    iq_d = din("iq", [128, 2, QT], BF16)
    ik_d = din("ik2", [128, S], BF16)
    iw_d = din("iw", [128, NSLOT * 4, 4], F32)
    lf_d = din("lf", [128, 64, 8], F32)
    adm_d = din("adm", [4, 128, 1024], F32)
    cm_d = din("cm", [128, 8, 512], BF16)
    jf_d = din("jflag", [128, 1], F32)
    tri_d = din("tri", [128, 128], F32)
    ident_d = din("identb", [128, 128], BF16)
    pw_d = din("pw2", [128, NBIS], F32)
    oa_d = nc.dram_tensor(px + "oaT", [64, 8, QT], BF16, kind="ExternalOutput").ap()
    ob_d = nc.dram_tensor(px + "obT", [64, 8, QT], BF16, kind="ExternalOutput").ap()

    from contextlib import ExitStack
    with ExitStack() as st:
        def sb(name, shape, dt):
            return st.enter_context(nc.sbuf_tensor(px + name, shape, dt))
        Sc = sb("Sc", [128, S], F32)
        Mk = sb("Mk", [128, S], BF16)
        MT = sb("MT", [128, 64, 512], BF16)
        ik2 = sb("ik2s", [128, S], BF16)
        KT = [sb("KT%d" % i, [128, 2048], BF16) for i in range(2)]
        VA = [sb("VA%d" % i, [128, 16, 130], BF16) for i in range(2)]
        qT = [sb("qT%d" % i, [128, 512], BF16) for i in range(2)]
        iqc = sb("iqc", [128, 2, 512], BF16)
        iw = sb("iws", [128, NSLOT * 4, 4], F32)
        lf = sb("lfs", [128, 64, 8], F32)
        cw = sb("cw", [128, 64, 8], F32)
        offs = sb("offs", [128, 65, 8], F32)
        tot = sb("tot", [128, 64, 8], F32)
        adm = [sb("adm%d" % i, [128, 1024], F32) for i in range(2)]
        cm = sb("cms", [128, 8, 512], BF16)
        jf = sb("jfs", [128, 1], F32)
        tri = sb("tris", [128, 128], F32)
        ones = sb("ones", [128, 128], F32)
        identb = sb("identbs", [128, 128], BF16)
        pw = sb("pws", [128, NBIS], F32)
        rl = [sb("rl%d" % i, [128, 512], F32) for i in range(3)]
        pT = [sb("pT%d" % i, [128, 512], BF16) for i in range(6)]
        U = [sb("U%d" % i, [65, 512], F32) for i in range(4)]
        R = [sb("R%d" % i, [65, 512], F32) for i in range(4)]
        oT = [sb("oT%d" % i, [64, 512], BF16) for i in range(2)]
        bfx = [sb("bfx%d" % i, [128, 64], F32) for i in range(2)]
        cref2 = [sb("cref%d" % i, [128, 8], F32) for i in range(2)]
        sm = sb("sm", [128, 16], F32)
        dk = sb("dk", [128, NBIS], F32)
        ps = st.enter_context(nc.psum_tensor(px + "ps", [128, 8, 512], F32))
        cnt = {}

        def nxt(k, n):
            v = cnt.get(k, 0) % n
            cnt[k] = cnt.get(k, 0) + 1
            return v

        for i, (t, d, kn) in enumerate([(ik2, ik_d, 'ik2s'), (iw, iw_d, 'iws'), (lf, lf_d, 'lfs'), (cm, cm_d, 'cms'), (jf, jf_d, 'jfs'),
                                        (tri, tri_d, 'tris'), (identb, ident_d, 'identbs'), (pw, pw_d, 'pws')]):
            P.dma('sp', t[:], d, writes=[kn], key='c%d' % i)
        P.op('pool', lambda e: e.memset(ones[:], 1.0), writes=['ones'])

        lf2 = lf[:].rearrange("p b h -> p (b h)")
        P.mm(ps[:, 0, :], tri[:], lf2, True, True, reads=['tris', 'lfs'], writes=[('ps', 0)])
        P.mm(ps[:, 1, :], ones[:], lf2, True, True, reads=['ones', 'lfs'], writes=[('ps', 1)])
        P.op('act', lambda e: e.copy(tot[:].rearrange("p b h -> p (b h)"), ps[:, 1, :]), reads=[('ps', 1)], writes=['tot'])
        P.op('dve', lambda e: e.memset(offs[:, 0, :], 0.0), writes=['offs'])
        for bl in range(64):
            P.op('dve', lambda e, bl=bl: e.tensor_tensor(offs[:, bl + 1, :], offs[:, bl, :], tot[:, bl, :], ALU.add),
                 reads=['offs', 'tot'], writes=['offs'])
        P.op('dve', lambda e: e.tensor_tensor(cw[:], ps[:, 0, :].rearrange("p (b h) -> p b h", h=8), offs[:, 0:64, :], ALU.add),
             reads=[('ps', 0), 'offs'], writes=['cw'])
        P.op('dve', lambda e: e.tensor_scalar(cw[:], cw[:], -1.0, None, ALU.mult), reads=['cw'], writes=['cw'])

        def gbank():
            return nxt('g', 4)

        def attention(slot, branch):
            nblk = (2 * slot + 2) * 4
            q_d, k_d, v_d, o_d = (aq_d, ak_d, av_d, oa_d) if branch == 'a' else (fq_d, fk_d, fv_d, ob_d)
            jobs = []
            for pr in range(4):
                segs = [(s0, min(16, nblk - s0)) for s0 in range(0, nblk, 16)]
                for si, (s0, sn) in enumerate(segs):
                    def load(pr=pr, s0=s0, sn=sn, si=si):
                        b = nxt('kv', 2)
                        P.dma('sp', KT[b][:, 0:sn * 128], k_d[pr, :, s0 * 128:(s0 + sn) * 128], writes=[('KT', b)], key=('KT', b))
                        P.dma('sp', VA[b][:, 0:sn, :], v_d[pr, :, s0:s0 + sn, :], writes=[('VA', b)], key=('VA', b))
                        qb_ = None
                        if si == 0:
                            qb_ = nxt('q', 2)
                            P.dma('sp', qT[qb_][:], q_d[pr, :, slot * 512:(slot + 1) * 512], writes=[('qT', qb_)], key=('qT', qb_))
                        return (b, qb_)

                    def comp(ld, st_, pr=pr, s0=s0, sn=sn, si=si, last=(si == len(segs) - 1)):
                        b, qb_ = ld
                        if si == 0:
                            st_['q'] = qb_
                            st_['acc'] = [4, 5]
                            if branch == 'b':
                                for hh in range(2):
                                    h = pr * 2 + hh
                                    P.op('pool', lambda e, hh=hh, h=h: e.tensor_scalar(bfx[hh][:, 0:nblk], cw[:, 0:nblk, h], cref2[slot % 2][:, h:h + 1], 60.0, ALU.subtract, ALU.min),
                                         reads=['cw', ('cref', slot % 2)], writes=[('bfx', hh)])
                        qb = st_['q']
                        LA = 3
                        pend = []

                        def emit_pv(it):
                            hh, kl, pi = it
                            kb = s0 + kl
                            acc = st_['acc'][hh]
                            P.mm(ps[0:65, acc, :], VA[b][:, kl, hh * 65:(hh + 1) * 65], pT[pi][:], kb == 0, kb == nblk - 1,
                                 reads=[('VA', b), ('pT', pi)], writes=[('ps', acc)])
                            if last and kl == sn - 1:
                                h = pr * 2 + hh
                                ui = nxt('U', 4)
                                P.op('act', lambda e, ui=ui, acc=acc: e.copy(U[ui][:], ps[0:65, acc, :]), reads=[('ps', acc)], writes=[('U', ui)])

                                def norm(ui=ui, h=h):
                                    P.op('dve', lambda e, ui=ui: e.reciprocal(R[ui][64:65, :], U[ui][64:65, :]), reads=[('U', ui)], writes=[('R', ui)])
                                    P.mm(ps[0:64, 7, :], ones[64:65, 0:64], R[ui][64:65, :], True, True, reads=['ones', ('R', ui)], writes=[('ps', 7)])
                                    oi = nxt('oT', 2)
                                    P.op('dve', lambda e, ui=ui, oi=oi: e.tensor_tensor(oT[oi][:], U[ui][0:64, :], ps[0:64, 7, :], ALU.mult),
                                         reads=[('U', ui), ('ps', 7)], writes=[('oT', oi)])
                                    P.dma('sp', o_d[:, h, slot * 512:(slot + 1) * 512], oT[oi][:], reads=[('oT', oi)], key='out')
                                st_.setdefault('dcur', []).append(norm)

                        for hh in range(2):
                            for kl in range(sn):
                                kb = s0 + kl
                                g = gbank()
                                P.mm(ps[:, g, :], KT[b][hh * 64:(hh + 1) * 64, kl * 128:(kl + 1) * 128], qT[qb][hh * 64:(hh + 1) * 64, :], True, True,
                                     reads=[('KT', b), ('qT', qb)], writes=[('ps', g)])
                                pi = nxt('pT', 6)
                                meng = 'pool' if (kb % 2 == 0 or branch == 'b') else 'dve'
                                if branch == 'a':
                                    P.op('act', lambda e, g=g, pi=pi: e.activation(pT[pi][:], ps[:, g, :], AF.Exp, scale=0.125),
                                         reads=[('ps', g)], writes=[('pT', pi)])
                                    P.op(meng, lambda e, pi=pi, kb=kb: e.tensor_tensor(pT[pi][:], pT[pi][:], MT[:, kb, :], ALU.mult),
                                         reads=[('pT', pi), 'MT'], writes=[('pT', pi)])
                                else:
                                    P.op('act', lambda e, g=g, pi=pi, hh=hh, kb=kb: e.activation(pT[pi][:], ps[:, g, :], AF.Exp, bias=bfx[hh][:, kb:kb + 1], scale=0.125),
                                         reads=[('ps', g), ('bfx', hh)], writes=[('pT', pi)])
                                    if kb >= nblk - 8:
                                        P.op(meng, lambda e, pi=pi, kb=kb: e.tensor_tensor(pT[pi][:], pT[pi][:], cm[:, kb - (nblk - 8), :], ALU.mult),
                                             reads=[('pT', pi), 'cms'], writes=[('pT', pi)])
                                pend.append((hh, kl, pi))
                                if len(pend) > LA:
                                    emit_pv(pend.pop(0))
                        while pend:
                            emit_pv(pend.pop(0))
                        for fn_ in st_.get('dprev', []):
                            fn_()
                        st_['dprev'] = st_.get('dcur', [])
                        st_['dcur'] = []
                    jobs.append((load, comp))
            return jobs

        def jobs_gen(jobs):
            st_ = {}
            nb = jobs[0][0]()
            for ji in range(len(jobs)):
                cur = nb
                if ji + 1 < len(jobs):
                    nb = jobs[ji + 1][0]()
                jobs[ji][1](cur, st_)
                if ji == len(jobs) - 1:
                    for fn_ in st_.get('dprev', []) + st_.get('dcur', []):
                        fn_()
                    st_['dprev'] = st_['dcur'] = []
                yield

        def run_jobs(jobs):
            for _ in jobs_gen(jobs):
                pass

        def mask_gen(slot):
            cref = cref2[slot % 2]
            nblk = (2 * slot + 2) * 4
            n = nblk * 128
            P.dma('sp', iqc[:], iq_d[:, :, slot * 512:(slot + 1) * 512], writes=['iqc'], key='iqc')
            P.op('dve', lambda e, slot=slot: e.tensor_tensor(cref[:], offs[:, 8 * slot + 4, :], offs[:, 8 * slot, :], ALU.subtract),
                 reads=['offs'], writes=[('cref', slot % 2)])
            P.op('dve', lambda e, slot=slot: e.scalar_tensor_tensor(cref[:], cref[:], jf[:, 0:1], offs[:, 8 * slot, :], ALU.mult, ALU.add),
                 reads=[('cref', slot % 2), 'offs', 'jfs'], writes=[('cref', slot % 2)])
            P.op('dve', lambda e: e.tensor_scalar(cref[:], cref[:], -1.0, None, ALU.mult), reads=[('cref', slot % 2)], writes=[('cref', slot % 2)])
            for qb in range(4):
                qi = slot * 4 + qb
                ai = nxt('adm', 2)
                P.dma('sp', adm[ai][:], adm_d[qb], writes=[('adm', ai)], key=('adm', ai))
                for sc in range(n // 512):
                    cs = slice(sc * 512, (sc + 1) * 512)
                    for h in range(4):
                        g = gbank()
                        hb = (h % 2) * 64
                        P.mm(ps[:, g, :], iqc[hb:hb + 64, h // 2, qb * 128:(qb + 1) * 128], ik2[hb:hb + 64, cs], True, True,
                             reads=['iqc', 'ik2s'], writes=[('ps', g)])
                        ri = nxt('rl', 3)
                        P.op('act', lambda e, g=g, ri=ri: e.activation(rl[ri][:], ps[:, g, :], AF.Relu), reads=[('ps', g)], writes=[('rl', ri)])
                        if h == 0:
                            P.op('dve', lambda e, ri=ri, cs=cs, qi=qi: e.tensor_scalar(Sc[:, cs], rl[ri][:], iw[:, qi, 0:1], None, ALU.mult),
                                 reads=[('rl', ri), 'iws'], writes=[('Sc', sc)])
                        else:
                            P.op('dve', lambda e, ri=ri, cs=cs, qi=qi, h=h: e.scalar_tensor_tensor(Sc[:, cs], rl[ri][:], iw[:, qi, h:h + 1], Sc[:, cs], ALU.mult, ALU.add),
                                 reads=[('rl', ri), 'iws', ('Sc', sc)], writes=[('Sc', sc)])
                scall = [('Sc', sc) for sc in range(n // 512)]
                P.op('dve', lambda e, n=n: e.tensor_reduce(sm[:, 0:1], Sc[:, 0:n], AX.X, ALU.max, apply_absolute_value=True),
                     reads=scall, writes=['sm0'])
                P.op('dve', lambda e: e.tensor_scalar(sm[:, 1:2], sm[:, 0:1], -1.0, -1.0, ALU.mult, ALU.add), reads=['sm0'], writes=['lo'])
                P.op('dve', lambda e: e.tensor_scalar(sm[:, 2:3], sm[:, 0:1], 2.0, 2.0, ALU.mult, ALU.add), reads=['sm0'], writes=['w0'])
                P.op('dve', lambda e: e.tensor_scalar(dk[:], pw[:], sm[:, 2:3], None, ALU.mult), reads=['w0', 'pws'], writes=['dk'])
                P.op('dve', lambda e, n=n, ai=ai: e.tensor_tensor(Sc[:, n - 1024:n], Sc[:, n - 1024:n], adm[ai][:], ALU.add),
                     reads=[('adm', ai)] + scall[-2:], writes=scall[-2:])
                for k in range(NBIS):
                    P.op('dve', lambda e, k=k: e.tensor_tensor(sm[:, 3:4], sm[:, 1:2], dk[:, k:k + 1], ALU.add), reads=['lo', 'dk'], writes=['mid'])
                    P.op('dve', lambda e, n=n: e.tensor_scalar(Mk[:, 0:n], Sc[:, 0:n], sm[:, 3:4], None, ALU.is_ge, ALU.add, accum_out=sm[:, 4:5]),
                         reads=scall + ['mid'], writes=['Mk', 'cnt'])
                    P.op('dve', lambda e, k=k: e.scalar_tensor_tensor(sm[:, 5:6], sm[:, 4:5], TOPK, dk[:, k:k + 1], ALU.is_ge, ALU.mult),
                         reads=['cnt', 'dk'], writes=['stp'])
                    P.op('dve', lambda e: e.tensor_tensor(sm[:, 1:2], sm[:, 1:2], sm[:, 5:6], ALU.add), reads=['lo', 'stp'], writes=['lo'])
                P.op('dve', lambda e, n=n: e.tensor_scalar(Mk[:, 0:n], Sc[:, 0:n], sm[:, 1:2], None, ALU.is_ge),
                     reads=scall + ['lo'], writes=['Mk'])
                yield
                pst = ps[:, 6, :].bitcast(BF16)
                for k0 in range(0, nblk, 4):
                    for j in range(4):
                        P.tr(pst[:, j * 128:(j + 1) * 128], Mk[:, (k0 + j) * 128:(k0 + j + 1) * 128], identb[:],
                             reads=['Mk', 'identbs'], writes=[('ps', 6)])
                    P.op('act', lambda e, k0=k0, qb=qb: e.copy(MT[:, k0:k0 + 4, qb * 128:(qb + 1) * 128],
                                                              pst[:, 0:512].rearrange("p (j t) -> p j t", j=4)),
                         reads=[('ps', 6)], writes=['MT'])
                yield

        def drain(g):
            for _ in g:
                pass

        drain(mask_gen(0))
        for slot in range(NSLOT):
            run_jobs(attention(slot, 'a'))
            gf = jobs_gen(attention(slot, 'b'))
            gm = mask_gen(slot + 1) if slot + 1 < NSLOT else iter(())
            fa = ma = True
            while fa or ma:
                if fa:
                    fa = next(gf, 'end') != 'end'
                if ma:
                    ma = next(gm, 'end') != 'end'


    return dict(oaT=oa_d, obT=ob_d)


def _slot_cols(j):
    return np.concatenate([np.arange((2 * i + j) * 512, (2 * i + j + 1) * 512) for i in range(NSLOT)])


def k2a_consts(j):
    p = np.arange(128)
    col = np.arange(1024)
    adm = np.zeros((4, 128, 1024), np.float32)
    for qb in range(4):
        lim = j * 512 + qb * 128 + (p // 64) * 64 + 64
        adm[qb] = np.where(col[None, :] < lim[:, None], 0.0, NEG)
    kbl = np.arange(8)
    q = np.arange(512)
    cm = ((kbl[None, :, None] * 128 + p[:, None, None]) <= (j * 512 + q[None, None, :])).astype(np.float32).astype(NPBF)
    tri = (p[:, None] <= p[None, :]).astype(np.float32)
    pw = np.tile((2.0 ** -(np.arange(NBIS, dtype=np.float32) + 1))[None, :], (128, 1)).astype(np.float32)
    return dict(adm=adm, cm=np.ascontiguousarray(cm), jflag=np.full((128, 1), float(j), np.float32), tri=tri,
                identb=np.eye(128, dtype=np.float32).astype(NPBF), pw2=pw)


def _vaug(v_full):
    S = v_full.shape[0]
    va = np.ones((S, 8, 65), dtype=v_full.dtype)
    va[:, :, :64] = v_full.reshape(S, 8, 64)
    return np.ascontiguousarray(va.reshape(S // 128, 128, 4, 130).transpose(2, 1, 0, 3))


def k2a_inputs(j, aqT, akT, av, fqT, fkT, fv, iqT, ikT, small):
    cols = _slot_cols(j)
    S = SEQ
    d = dict(
        aq=np.ascontiguousarray(aqT.reshape(4, 128, S)[:, :, cols]),
        ak=np.ascontiguousarray(akT.reshape(4, 128, S)),
        av=_vaug(av),
        fq=np.ascontiguousarray(fqT.reshape(4, 128, S)[:, :, cols]),
        fk=np.ascontiguousarray(fkT.reshape(4, 128, S)),
        fv=_vaug(fv),
        iq=np.ascontiguousarray(iqT.reshape(2, 128, S)[:, :, cols].transpose(1, 0, 2)),
        ik2=np.ascontiguousarray(np.concatenate([ikT, ikT], axis=0)),
        iw=np.ascontiguousarray(small[cols, 0:4].reshape(NSLOT * 4, 128, 4).transpose(1, 0, 2)),
        lf=np.ascontiguousarray(small[:, 4:12].reshape(64, 128, 8).transpose(1, 0, 2)),
    )
    d.update(k2a_consts(j))
    return d


def build_k2b():
    nc = bass.Bass("TRN2", target_bir_lowering=False)
    P = Prog(nc)
    phase_k2b(nc, P, '')
    P.emit(final_keys=['out'])
    return nc, P


def phase_k2b(nc, P, px, ext=None):
    ext = ext or {}
    S = SEQ
    NCH = S // 64
    GRP = 16
    din = lambda name, shape, dt: ext[name] if name in ext else nc.dram_tensor(px + name, shape, dt, kind="ExternalInput").ap()
    qp_d = din("qpT", [64, 4, S], BF16)
    kpT_d = din("kpT", [64, 4, S], BF16)
    kp_d = din("kp", [64, NCH, 256], BF16)
    v_d = din("v", [64, NCH, 256], BF16)
    em_d = din("em", [64, 4, NCH], F32)
    g_d = din("g", [64, 4, NCH], F32)
    triu_d = din("triu", [64, 64], F32)
    oc_d = nc.dram_tensor(px + "ocT", [64, 4, S], BF16, kind="ExternalOutput").ap()

    from contextlib import ExitStack
    with ExitStack() as st:
        def sb(name, shape, dt):
            return st.enter_context(nc.sbuf_tensor(px + name, shape, dt))
        qp = [sb("qp%d" % i, [64, 4, GRP * 64], BF16) for i in range(2)]
        kpT = [sb("kpT%d" % i, [64, 4, GRP * 64], BF16) for i in range(2)]
        kp = [sb("kp%d" % i, [64, GRP, 256], BF16) for i in range(2)]
        vv = [sb("vv%d" % i, [64, GRP, 256], BF16) for i in range(2)]
        ocb = [sb("ocb%d" % i, [64, 4, GRP * 64], BF16) for i in range(2)]
        em = sb("ems", [64, 4, NCH], F32)
        gg = sb("ggs", [64, 4, NCH], F32)
        triu = sb("trius", [64, 64], F32)
        Sm = sb("Sm", [64, 4, 64], F32)
        Smb = sb("Smb", [64, 4, 64], BF16)
        tmp = sb("tmp", [64, 4, 64], F32)
        scm = [sb("scm%d" % i, [64, 4, 64], BF16) for i in range(2)]
        ps = st.enter_context(nc.psum_tensor(px + "ps", [128, 8, 512], F32))
        cnt = {}

        def nxt(k, n):
            v = cnt.get(k, 0) % n
            cnt[k] = cnt.get(k, 0) + 1
            return v

        P.dma('sp', em[:], em_d, writes=['em'], key='c0')
        P.dma('sp', gg[:], g_d, writes=['gg'], key='c1')
        P.dma('sp', triu[:], triu_d, writes=['triu'], key='c2')
        P.op('dve', lambda e: e.tensor_tensor(gg[:, :, 0:NCH - 1], gg[:, :, 0:NCH - 1], em[:, :, 1:NCH], ALU.mult), reads=['em', 'gg'], writes=['gg'])
        P.op('dve', lambda e: e.memset(Sm[:], 0.0), writes=['Sm'])
        P.op('dve', lambda e: e.memset(Smb[:], 0.0), writes=['Smb'])

        ngrp = NCH // GRP

        def load(gi):
            b = gi % 2
            ts_ = slice(gi * GRP * 64, (gi + 1) * GRP * 64)
            cs_ = slice(gi * GRP, (gi + 1) * GRP)
            P.dma('sp', qp[b][:], qp_d[:, :, ts_], writes=[('qp', b)], key=('qp', b))
            P.dma('sp', kpT[b][:], kpT_d[:, :, ts_], writes=[('kpT', b)], key=('kpT', b))
            P.dma('sp', kp[b][:], kp_d[:, cs_, :], writes=[('kp', b)], key=('kp', b))
            P.dma('sp', vv[b][:], v_d[:, cs_, :], writes=[('vv', b)], key=('vv', b))

        load(0)
        for gi in range(ngrp):
            if gi + 1 < ngrp:
                load(gi + 1)
            b = gi % 2
            for cl in range(GRP):
                c = gi * GRP + cl
                tsl = slice(cl * 64, (cl + 1) * 64)
                gs = nxt('sc', 2)
                for hl in range(4):
                    P.mm(ps[0:64, gs, hl * 64:(hl + 1) * 64], kpT[b][:, hl, tsl], qp[b][:, hl, tsl], True, True,
                         reads=[('kpT', b), ('qp', b)], writes=[('ps', gs)])
                si = nxt('scm', 2)
                P.op('dve', lambda e, gs=gs, si=si: e.tensor_tensor(scm[si][:], ps[0:64, gs, 0:256].rearrange("p (h t) -> p h t", h=4),
                                                                  triu[:].unsqueeze(1).to_broadcast([64, 4, 64]), ALU.mult),
                     reads=[('ps', gs), 'triu'], writes=[('scm', si)])
                go = 2 + nxt('o', 2)
                for hl in range(4):
                    P.mm(ps[0:64, go, hl * 64:(hl + 1) * 64], vv[b][:, cl, hl * 64:(hl + 1) * 64], scm[si][:, hl, :], True, False,
                         reads=[('vv', b), ('scm', si)], writes=[('ps', go)])
                    P.mm(ps[0:64, go, hl * 64:(hl + 1) * 64], Smb[:, hl, :], qp[b][:, hl, tsl], False, True,
                         reads=['Smb', ('qp', b)], writes=[('ps', go)])
                P.op('act', lambda e, go=go, b=b, tsl=tsl: e.copy(ocb[b][:, :, tsl], ps[0:64, go, 0:256].rearrange("p (h t) -> p h t", h=4)),
                     reads=[('ps', go)], writes=[('ocb', b)])
                gk = 4 + nxt('kv', 2)
                for hl in range(4):
                    P.mm(ps[0:64, gk, hl * 64:(hl + 1) * 64], kp[b][:, cl, hl * 64:(hl + 1) * 64], vv[b][:, cl, hl * 64:(hl + 1) * 64], True, True,
                         reads=[('kp', b), ('vv', b)], writes=[('ps', gk)])
                if c < NCH - 1:
                    P.op('dve', lambda e, gk=gk: e.tensor_tensor(tmp[:], ps[0:64, gk, 0:256].rearrange("p (h t) -> p h t", h=4), Sm[:], ALU.add),
                         reads=[('ps', gk), 'Sm'], writes=['tmp'])
                    P.op('dve', lambda e, c=c: e.tensor_tensor(Sm[:], tmp[:], gg[:, :, c:c + 1].to_broadcast([64, 4, 64]), ALU.mult),
                         reads=['tmp', 'gg'], writes=['Sm'])
                    P.op('dve', lambda e, c=c: e.tensor_tensor(Smb[:], tmp[:], gg[:, :, c:c + 1].to_broadcast([64, 4, 64]), ALU.mult),
                         reads=['tmp', 'gg'], writes=['Smb'])
            P.dma('sp', oc_d[:, :, gi * GRP * 64:(gi + 1) * GRP * 64], ocb[b][:], reads=[('ocb', b)], key='out')
    return dict(ocT=oc_d)


def k2b_inputs(j, qpT, kpT, kp, cv, em, g):
    S = SEQ
    hs = slice(4 * j, 4 * j + 4)
    cs = slice(256 * j, 256 * j + 256)
    return dict(
        qpT=np.ascontiguousarray(qpT.reshape(8, 64, S)[hs].transpose(1, 0, 2)),
        kpT=np.ascontiguousarray(kpT.reshape(8, 64, S)[hs].transpose(1, 0, 2)),
        kp=np.ascontiguousarray(kp[:, cs].reshape(S // 64, 64, 256).transpose(1, 0, 2)),
        v=np.ascontiguousarray(cv[:, cs].reshape(S // 64, 64, 256).transpose(1, 0, 2)),
        em=np.ascontiguousarray(em.reshape(8, 64, S // 64)[hs].transpose(1, 0, 2)).astype(np.float32),
        g=np.ascontiguousarray(g.reshape(8, 64, S // 64)[hs].transpose(1, 0, 2)).astype(np.float32),
        triu=np.triu(np.ones((64, 64), np.float32)),
    )


def emit_layernorm(P, y, out, lng, lnb, stats, mv, rstd, ykey, okey, gkeys, tag):
    for c in range(2):
        P.op('dve', lambda e, c=c: e.bn_stats(stats[:, c, :], y[:, c * 512:(c + 1) * 512]), reads=[ykey], writes=[(tag, 'st', c)])
    P.op('dve', lambda e: e.bn_aggr(mv[:], stats[:].rearrange("p c s -> p (c s)")), reads=[(tag, 'st', 0), (tag, 'st', 1)], writes=[(tag, 'mv')])
    P.op('act', lambda e: e.activation(rstd[:], mv[:, 1:2], AF.Sqrt, bias=1e-5), reads=[(tag, 'mv')], writes=[(tag, 'rs')])
    P.op('dve', lambda e: e.reciprocal(rstd[:], rstd[:]), reads=[(tag, 'rs')], writes=[(tag, 'rs')])
    P.op('dve', lambda e: e.tensor_scalar(out, y, mv[:, 0:1], rstd[:, 0:1], ALU.subtract, ALU.mult), reads=[ykey, (tag, 'mv'), (tag, 'rs')], writes=[okey])
    P.op('pool', lambda e: e.tensor_tensor(out, out, lng, ALU.mult), reads=[okey, gkeys[0]], writes=[okey])
    P.op('pool', lambda e: e.tensor_tensor(out, out, lnb, ALU.add), reads=[okey, gkeys[1]], writes=[okey])


def build_k3a():
    nc = bass.Bass("TRN2", target_bir_lowering=False)
    P = Prog(nc)
    phase_k3a(nc, P, '')
    P.emit(final_keys=['out'])
    return nc, P


def phase_k3a(nc, P, px, ext=None):
    ext = ext or {}
    T = TOK
    din = lambda name, shape, dt: ext[name] if name in ext else nc.dram_tensor(px + name, shape, dt, kind="ExternalInput").ap()
    oa_d = din("oaT", [64, 8, T], BF16)
    ob_d = din("obT", [64, 8, T], BF16)
    oc_d = din("ocT", [64, 8, T], BF16)
    cg_d = din("cgT", [64, 8, T], BF16)
    gt_d = din("gT", [128, 24, T], BF16)
    x_d = din("x", [T, D_MODEL], F32)
    wp_d = [din("wp%d" % i, [64, 8, D_MODEL], F32) for i in range(3)]
    wo_d = din("wo", [128, 8, D_MODEL], F32)
    gn_d = din("gn", [64, 8], F32)
    lng_d = din("lng", [1, D_MODEL], F32)
    lnb_d = din("lnb", [1, D_MODEL], F32)
    h_d = nc.dram_tensor(px + "h", [T, D_MODEL], F32, kind="ExternalOutput").ap()

    from contextlib import ExitStack
    with ExitStack() as st:
        def sb(name, shape, dt):
            return st.enter_context(nc.sbuf_tensor(px + name, shape, dt))
        wst = sb("wst", [128, 8, 512], F32)
        wp = [sb("wpb%d" % i, [64, 8, D_MODEL], BF16) for i in range(3)]
        wo = sb("wob", [128, 8, D_MODEL], BF16)
        gn = sb("gns", [64, 8], F32)
        lng = sb("lngs", [128, D_MODEL], F32)
        lnb = sb("lnbs", [128, D_MODEL], F32)
        on64 = sb("on64", [64, 64], BF16)
        oT = [[sb("oT%d_%d" % (br, i), [64, 8, 512], BF16) for i in range(1)] for br in range(3)]
        cg = [sb("cg%d" % i, [64, 8, 512], BF16) for i in range(1)]
        gt = [sb("gt%d" % i, [128, 3, 512], BF16) for i in range(2)]
        mT = sb("mT", [128, 8, 512], BF16)
        sq = [sb("sq%d" % i, [64, 512], BF16) for i in range(2)]
        rs = [sb("rs%d" % i, [64, 512], F32) for i in range(2)]
        tA = [sb("tA%d" % i, [128, 512], F32) for i in range(2)]
        tB = [sb("tB%d" % i, [128, 512], F32) for i in range(2)]
        tC = [sb("tC%d" % i, [128, 512], F32) for i in range(2)]
        xin = [sb("xin%d" % i, [128, D_MODEL], F32) for i in range(2)]
        yb = [sb("yb%d" % i, [128, D_MODEL], F32) for i in range(2)]
        hb = [sb("hb%d" % i, [128, D_MODEL], F32) for i in range(2)]
        stats = sb("stats", [128, 2, 6], F32)
        mv = sb("mv", [128, 2], F32)
        rstd = sb("rstd", [128, 1], F32)
        ps = st.enter_context(nc.psum_tensor(px + "ps", [128, 8, 512], F32))
        cnt = {}

        def nxt(k, n):
            v = cnt.get(k, 0) % n
            cnt[k] = cnt.get(k, 0) + 1
            return v

        for i in range(3):
            for hf in range(2):
                P.dma('sp', wst[0:64], wp_d[i][:, :, hf * 512:(hf + 1) * 512], writes=['wst'], key='wst')
                P.op('act', lambda e, i=i, hf=hf: e.copy(wp[i][:, :, hf * 512:(hf + 1) * 512], wst[0:64]), reads=['wst'], writes=[('wp', i)])
        for hf in range(2):
            P.dma('sp', wst[:], wo_d[:, :, hf * 512:(hf + 1) * 512], writes=['wst'], key='wst')
            P.op('act', lambda e, hf=hf: e.copy(wo[:, :, hf * 512:(hf + 1) * 512], wst[:]), reads=['wst'], writes=['wo'])
        P.dma('sp', gn[:], gn_d, writes=['gn'], key='c0')
        P.dma('sp', lng[:], lng_d.partition_broadcast(128), writes=['lng'], key='c1')
        P.dma('sp', lnb[:], lnb_d.partition_broadcast(128), writes=['lnb'], key='c2')
        P.op('pool', lambda e: e.memset(on64[:], 1.0 / 64.0), writes=['on64'])

        NTC = T // 512

        def load(tc):
            b = 0
            sl = slice(tc * 512, (tc + 1) * 512)
            for br, d in enumerate((oa_d, ob_d, oc_d)):
                P.dma('sp', oT[br][b][:], d[:, :, sl], writes=[('oT', br, b)], key=('oT', br, b))
            P.dma('sp', cg[b][:], cg_d[:, :, sl], writes=[('cg', b)], key=('cg', b))

        def load_gt(tc, mc):
            gi = nxt('gt', 2)
            sl = slice(tc * 512, (tc + 1) * 512)
            for br in range(3):
                P.dma('sp', gt[gi][:, br, :], gt_d[:, br * 8 + mc, sl], writes=[('gt', gi)], key=('gt', gi))
            return gi

        for tc in range(NTC):
            load(tc)
            b = 0
            oc = oT[2][b]
            for h in range(8):
                si = nxt('sq', 2)
                P.op('dve', lambda e, h=h, si=si: e.tensor_tensor(sq[si][:], oc[:, h, :], oc[:, h, :], ALU.mult), reads=[('oT', 2, b)], writes=[('sq', si)])
                g = nxt('g', 4)
                P.mm(ps[0:64, g, :], on64[:], sq[si][:], True, True, reads=['on64', ('sq', si)], writes=[('ps', g)])
                ri = nxt('rs', 2)
                P.op('act', lambda e, g=g, ri=ri: e.activation(rs[ri][:], ps[0:64, g, :], AF.Sqrt, bias=1e-6), reads=[('ps', g)], writes=[('rs', ri)])
                P.op('dve', lambda e, ri=ri: e.reciprocal(rs[ri][:], rs[ri][:]), reads=[('rs', ri)], writes=[('rs', ri)])
                P.op('dve', lambda e, h=h, ri=ri: e.scalar_tensor_tensor(rs[ri][:], oc[:, h, :], gn[:, h:h + 1], rs[ri][:], ALU.mult, ALU.mult),
                     reads=[('oT', 2, b), 'gn', ('rs', ri)], writes=[('rs', ri)])
                P.op('pool', lambda e, h=h, ri=ri: e.tensor_tensor(oc[:, h, :], rs[ri][:], cg[b][:, h, :], ALU.mult),
                     reads=[('rs', ri), ('cg', b)], writes=[('oT', 2, b)])
            gnext = load_gt(tc, 0)
            for mc in range(8):
                gcur = gnext
                if mc + 1 < 8:
                    gnext = load_gt(tc, mc + 1)
                gb = []
                for br in range(3):
                    g = nxt('g', 4)
                    for h in range(8):
                        P.mm(ps[:, g, :], wp[br][:, h, mc * 128:(mc + 1) * 128], oT[br][b][:, h, :], h == 0, h == 7,
                             reads=[('wp', br), ('oT', br, b)], writes=[('ps', g)])
                    gb.append(g)
                ti = nxt('t', 2)
                for br, tt in enumerate((tA, tB, tC)):
                    P.op('dve', lambda e, br=br, tt=tt, ti=ti, g=gb[br], gcur=gcur: e.tensor_tensor(tt[ti][:], ps[:, g, :], gt[gcur][:, br, :], ALU.mult),
                         reads=[('ps', gb[br]), ('gt', gcur)], writes=[('t', br, ti)])
                P.op('pool', lambda e, ti=ti: e.tensor_tensor(tA[ti][:], tA[ti][:], tB[ti][:], ALU.add), reads=[('t', 0, ti), ('t', 1, ti)], writes=[('t', 0, ti)])
                P.op('pool', lambda e, ti=ti, mc=mc: e.tensor_tensor(mT[:, mc, :], tA[ti][:], tC[ti][:], ALU.add), reads=[('t', 0, ti), ('t', 2, ti)], writes=[('mT', mc)])
            for tt in range(4):
                tix = tc * 4 + tt
                xi = nxt('x', 2)
                P.dma('sp', xin[xi][:], x_d[tix * 128:(tix + 1) * 128, :], writes=[('xin', xi)], key=('xin', xi))
                yi = nxt('y', 2)
                for half in range(2):
                    g = 4 + nxt('go', 4)
                    for k in range(8):
                        P.mm(ps[:, g, :], mT[:, k, tt * 128:(tt + 1) * 128], wo[:, k, half * 512:(half + 1) * 512], k == 0, k == 7,
                             reads=[('mT', k), 'wo'], writes=[('ps', g)])
                    P.op('dve', lambda e, g=g, xi=xi, yi=yi, half=half: e.scalar_tensor_tensor(
                        yb[yi][:, half * 512:(half + 1) * 512], xin[xi][:, half * 512:(half + 1) * 512], ALPHA, ps[:, g, :], ALU.mult, ALU.add),
                        reads=[('xin', xi), ('ps', g)], writes=[('yb', yi)])
                hi = nxt('h', 2)
                emit_layernorm(P, yb[yi][:], hb[hi][:], lng[:], lnb[:], stats, mv, rstd, ('yb', yi), ('hb', hi), ('lng', 'lnb'), 'ln')
                P.dma('sp', h_d[tix * 128:(tix + 1) * 128, :], hb[hi][:], reads=[('hb', hi)], key='out')
    return dict(h=h_d)


def _fm64(a_T):
    return np.ascontiguousarray(a_T.reshape(8, 64, -1).transpose(1, 0, 2))


def k3a_inputs(oaT, obT, ocT, cgT, gT, x, wpa, wpb, wpc, wo, gn, lng, lnb):
    T = TOK
    f = lambda w: np.ascontiguousarray(w.reshape(8, 64, D_MODEL).transpose(1, 0, 2)).astype(np.float32)
    return dict(oaT=oaT, obT=obT, ocT=ocT, cgT=_fm64(cgT),
                gT=np.ascontiguousarray(gT.reshape(24, 128, T).transpose(1, 0, 2)),
                x=np.ascontiguousarray(x), wp0=f(wpa), wp1=f(wpb), wp2=f(wpc),
                wo=np.ascontiguousarray(wo.reshape(8, 128, D_MODEL).transpose(1, 0, 2)).astype(np.float32),
                gn=np.ascontiguousarray(gn.reshape(8, 64).T).astype(np.float32),
                lng=np.ascontiguousarray(lng.reshape(1, -1)).astype(np.float32),
                lnb=np.ascontiguousarray(lnb.reshape(1, -1)).astype(np.float32))


def build_k3b(e0=0, ne=N_EXP, first=True, last=True):
    nc = bass.Bass("TRN2", target_bir_lowering=False)
    P = Prog(nc)
    phase_k3b(nc, P, '', e0, ne, first, last)
    P.emit(final_keys=['out'])
    return nc, P


def phase_k3b(nc, P, px, e0=0, ne=N_EXP, first=True, last=True, ext=None):
    ext = ext or {}
    T = TOK
    QC = 1024
    NQ = T // QC
    din = lambda name, shape, dt: ext[name] if name in ext else nc.dram_tensor(px + name, shape, dt, kind="ExternalInput").ap()
    h_d = din("h", [T, D_MODEL], F32)
    wr_d = din("wr", [128, 8, N_EXP], F32)
    br_d = din("br", [1, N_EXP], F32)
    wgu_d = din("wgu", [ne, D_MODEL, 2 * D_MODEL], F32)
    bgu_d = din("bgu", [128, N_EXP, 16], F32)
    wd_d = din("wd", [ne, D_MODEL, D_MODEL], F32)
    accin_d = None if first else din("acc_in", [T, D_MODEL], F32)
    bd_d = din("bd", [N_EXP, D_MODEL], F32)
    lng_d = din("lng", [1, D_MODEL], F32)
    lnb_d = din("lnb", [1, D_MODEL], F32)
    ident_d = din("ident", [128, 128], F32)
    out_d = nc.dram_tensor(px + ("out" if last else "acc_out"), [T, D_MODEL], F32, kind="ExternalOutput").ap()

    from contextlib import ExitStack
    with ExitStack() as st:
        def sb(name, shape, dt):
            return st.enter_context(nc.sbuf_tensor(px + name, shape, dt))
        hTc = sb("hTc", [128, 8, QC], BF16)
        acc = sb("acc", [128, QC // 128, D_MODEL], F32)
        actT = sb("actT", [128, 8, QC], BF16)
        gst = [sb("gst%d" % i, [128, 8, 512], F32) for i in range(2)]
        gbf = [sb("gbf%d" % i, [128, 8, 512], BF16) for i in range(2)]
        dst = gst
        dbf = [sb("dbf%d" % i, [128, 8, D_MODEL], BF16) for i in range(2)]
        big = [sb("big%d" % i, [128, D_MODEL], F32) for i in range(4)]
        hT32 = sb("hT32", [128, 8, 128], F32)
        wr = sb("wrs", [128, 8, N_EXP], F32)
        brb = sb("brb", [128, N_EXP], F32)
        bgu = sb("bgus", [128, N_EXP, 16], F32)
        bd = sb("bds", [N_EXP, D_MODEL], F32)
        lng = sb("lngs", [128, D_MODEL], F32)
        lnb = sb("lnbs", [128, D_MODEL], F32)
        ident = sb("idents", [128, 128], F32)
        Gc = sb("Gc", [128, QC // 128, N_EXP], F32)
        GTc = sb("GTc", [N_EXP, QC], F32)
        lg = sb("lg", [128, N_EXP], F32)
        ex = sb("ex", [128, N_EXP], F32)
        m8 = sb("m8", [128, 8], F32)
        sm = sb("sm", [128, 4], F32)
        tG = [sb("tG%d" % i, [128, 512], F32) for i in range(2)]
        tS = [sb("tS%d" % i, [128, 512], F32) for i in range(2)]
        tU = [sb("tU%d" % i, [128, 512], F32) for i in range(2)]
        stats = sb("stats", [128, 2, 6], F32)
        mv = sb("mv", [128, 2], F32)
        rstd = sb("rstd", [128, 1], F32)
        ps = st.enter_context(nc.psum_tensor(px + "ps", [128, 8, 512], F32))
        cnt = {}

        def nxt(k, n):
            v = cnt.get(k, 0) % n
            cnt[k] = cnt.get(k, 0) + 1
            return v

        for i, (t, d, kn) in enumerate([(wr, wr_d, 'wrs'), (bgu, bgu_d, 'bgus'), (bd, bd_d, 'bds'), (ident, ident_d, 'idents')]):
            P.dma('sp', t[:], d, writes=[kn], key='c%d' % i)
        P.dma('sp', brb[:], br_d.partition_broadcast(128), writes=['brb'], key='c4')
        P.dma('sp', lng[:], lng_d.partition_broadcast(128), writes=['lng'], key='c5')
        P.dma('sp', lnb[:], lnb_d.partition_broadcast(128), writes=['lnb'], key='c6')

        wgu_v = wgu_d.rearrange("e (c p) n -> e p c n", p=128)
        wd_v = wd_d.rearrange("e (c p) n -> e p c n", p=128)

        def gu_unit(q, e, f2):
            def load():
                b = nxt('gw', 2)
                s_ = nxt('ds', 2)
                P.dma('sp', gst[s_][:, :, 0:256], wgu_v[e - e0, :, :, f2 * 256:(f2 + 1) * 256], writes=[('gst', s_)], key=('gst', s_))
                P.dma('sp', gst[s_][:, :, 256:512], wgu_v[e - e0, :, :, 1024 + f2 * 256:1024 + (f2 + 1) * 256], writes=[('gst', s_)], key=('gst', s_))
                P.op('act', lambda e_: e_.copy(gbf[b][:], gst[s_][:]), reads=[('gst', s_)], writes=[('gbf', b)])
                return b

            def comp(b):
                for fl in range(2):
                    fc = f2 * 2 + fl
                    for tcc in range(QC // 512):
                        tsl = slice(tcc * 512, (tcc + 1) * 512)
                        g1 = nxt('g', 4)
                        for k in range(8):
                            P.mm(ps[:, g1, :], gbf[b][:, k, fl * 128:(fl + 1) * 128], hTc[:, k, tsl], k == 0, k == 7,
                                 reads=[('gbf', b), 'hTc'], writes=[('ps', g1)])
                        g2 = nxt('g', 4)
                        for k in range(8):
                            P.mm(ps[:, g2, :], gbf[b][:, k, 256 + fl * 128:256 + (fl + 1) * 128], hTc[:, k, tsl], k == 0, k == 7,
                                 reads=[('gbf', b), 'hTc'], writes=[('ps', g2)])
                        ti = nxt('t', 2)
                        P.op('dve', lambda e_, g1=g1, ti=ti, fc=fc: e_.tensor_scalar(tG[ti][:], ps[:, g1, :], bgu[:, e, fc:fc + 1], 7.0, ALU.add, ALU.min),
                             reads=[('ps', g1), 'bgus'], writes=[('tG', ti)])
                        P.op('act', lambda e_, ti=ti: e_.activation(tS[ti][:], tG[ti][:], AF.Sigmoid, scale=1.702), reads=[('tG', ti)], writes=[('tS', ti)])
                        P.op('dve', lambda e_, g2=g2, ti=ti, fc=fc: e_.tensor_scalar(tU[ti][:], ps[:, g2, :], bgu[:, e, 8 + fc:9 + fc], 7.0, ALU.add, ALU.min),
                             reads=[('ps', g2), 'bgus'], writes=[('tU', ti)])
                        P.op('dve', lambda e_, ti=ti: e_.tensor_scalar(tU[ti][:], tU[ti][:], -7.0, 1.0, ALU.max, ALU.add), reads=[('tU', ti)], writes=[('tU', ti)])
                        P.op('pool', lambda e_, ti=ti: e_.tensor_tensor(tG[ti][:], tG[ti][:], tS[ti][:], ALU.mult), reads=[('tG', ti), ('tS', ti)], writes=[('tG', ti)])
                        P.op('pool', lambda e_, ti=ti, fc=fc, tsl=tsl: e_.tensor_tensor(actT[:, fc, tsl], tG[ti][:], tU[ti][:], ALU.mult),
                             reads=[('tG', ti), ('tU', ti)], writes=[('actT', fc)])
            return load, comp

        def down_unit(q, e):
            def load():
                b = nxt('dw', 2)
                for half in range(2):
                    s_ = nxt('ds', 2)
                    P.dma('sp', dst[s_][:], wd_v[e - e0, :, :, half * 512:(half + 1) * 512], writes=[('gst', s_)], key=('gst', s_))
                    P.op('act', lambda e_, s_=s_, half=half: e_.copy(dbf[b][:, :, half * 512:(half + 1) * 512], dst[s_][:]), reads=[('gst', s_)], writes=[('dbf', b)])
                return b

            def comp(b):
                for tt in range(QC // 128):
                    for half in range(2):
                        g = 4 + nxt('gd', 4)
                        for fc in range(8):
                            P.mm(ps[:, g, :], actT[:, fc, tt * 128:(tt + 1) * 128], dbf[b][:, fc, half * 512:(half + 1) * 512], fc == 0, fc == 7,
                                 reads=[('actT', fc), ('dbf', b)], writes=[('ps', g)])
                        P.op('dve', lambda e_, g=g, tt=tt, half=half: e_.scalar_tensor_tensor(
                            acc[:, tt, half * 512:(half + 1) * 512], ps[:, g, :], Gc[:, tt, e:e + 1], acc[:, tt, half * 512:(half + 1) * 512], ALU.mult, ALU.add),
                            reads=[('ps', g), 'Gc', ('acc', tt)], writes=[('acc', tt)])
            return load, comp

        units = []
        for q in range(NQ):
            for e in range(e0, e0 + ne):
                for f2 in range(4):
                    units.append(('gu', q, e, f2) + gu_unit(q, e, f2))
                units.append(('dn', q, e, None) + down_unit(q, e))

        def prologue(q):
            for tt in range(QC // 128):
                tix = q * (QC // 128) + tt
                bi = nxt('big', 4)
                P.dma('sp', big[bi][:], h_d[tix * 128:(tix + 1) * 128, :], writes=[('big', bi)], key=('big', bi))
                for half in range(2):
                    g = nxt('g', 4)
                    for j in range(4):
                        k = half * 4 + j
                        P.tr(ps[:, g, j * 128:(j + 1) * 128], big[bi][:, k * 128:(k + 1) * 128], ident[:], reads=[('big', bi), 'idents'], writes=[('ps', g)])
                    P.op('act', lambda e_, g=g, half=half: e_.copy(hT32[:, half * 4:(half + 1) * 4, :], ps[:, g, :].rearrange("p (j t) -> p j t", j=4)),
                         reads=[('ps', g)], writes=[('hT32', half)])
                    P.op('dve', lambda e_, half=half, tt=tt: e_.tensor_copy(hTc[:, half * 4:(half + 1) * 4, tt * 128:(tt + 1) * 128],
                                                                          hT32[:, half * 4:(half + 1) * 4, :]),
                         reads=[('hT32', half)], writes=['hTc'])
                g = nxt('g', 4)
                for k in range(8):
                    P.mm(ps[:, g, 0:N_EXP], hT32[:, k, :], wr[:, k, :], k == 0, k == 7, reads=[('hT32', k // 4), 'wrs'], writes=[('ps', g)])
                P.op('dve', lambda e_, g=g: e_.tensor_tensor(lg[:], ps[:, g, 0:N_EXP], brb[:], ALU.add), reads=[('ps', g), 'brb'], writes=['lg'])
                P.op('dve', lambda e_: e_.max(m8[:], lg[:]), reads=['lg'], writes=['m8'])
                P.op('dve', lambda e_: e_.tensor_scalar(sm[:, 0:1], m8[:, 0:1], -1.0, None, ALU.mult), reads=['m8'], writes=['sm0'])
                P.op('act', lambda e_: e_.activation(ex[:], lg[:], AF.Exp, bias=sm[:, 0:1]), reads=['lg', 'sm0'], writes=['ex'])
                P.op('dve', lambda e_: e_.scalar_tensor_tensor(ex[:], lg[:], m8[:, 3:4], ex[:], ALU.is_ge, ALU.mult), reads=['lg', 'm8', 'ex'], writes=['ex'])
                P.op('dve', lambda e_: e_.reduce_sum(sm[:, 1:2], ex[:], AX.X), reads=['ex'], writes=['sm1'])
                P.op('dve', lambda e_: e_.reciprocal(sm[:, 1:2], sm[:, 1:2]), reads=['sm1'], writes=['sm1'])
                P.op('dve', lambda e_, tt=tt: e_.tensor_scalar(Gc[:, tt, :], ex[:], sm[:, 1:2], None, ALU.mult), reads=['ex', 'sm1'], writes=['Gc'])
                g = nxt('g', 4)
                P.tr(ps[0:N_EXP, g, 0:128], Gc[:, tt, :], ident[:], reads=['Gc', 'idents'], writes=[('ps', g)])
                P.op('act', lambda e_, g=g, tt=tt: e_.copy(GTc[:, tt * 128:(tt + 1) * 128], ps[0:N_EXP, g, 0:128]), reads=[('ps', g)], writes=['GTc'])
                if first:
                    for half in range(2):
                        g = 4 + nxt('gd', 4)
                        P.mm(ps[:, g, :], GTc[:, tt * 128:(tt + 1) * 128], bd[:, half * 512:(half + 1) * 512], True, True, reads=['GTc', 'bds'], writes=[('ps', g)])
                        P.op('act', lambda e_, g=g, tt=tt, half=half: e_.copy(acc[:, tt, half * 512:(half + 1) * 512], ps[:, g, :]), reads=[('ps', g)], writes=[('acc', tt)])
                else:
                    P.dma('sp', acc[:, tt, :], accin_d[tix * 128:(tix + 1) * 128, :], writes=[('acc', tt)], key=('accin', tt))

        def epilogue(q):
            for tt in range(QC // 128):
                tix = q * (QC // 128) + tt
                if not last:
                    P.dma('sp', out_d[tix * 128:(tix + 1) * 128, :], acc[:, tt, :], reads=[('acc', tt)], key='out')
                    continue
                bi = nxt('big', 4)
                P.dma('sp', big[bi][:], h_d[tix * 128:(tix + 1) * 128, :], writes=[('big', bi)], key=('big', bi))
                yi = nxt('big', 4)
                P.op('dve', lambda e_, bi=bi, yi=yi, tt=tt: e_.scalar_tensor_tensor(big[yi][:], big[bi][:], ALPHA, acc[:, tt, :], ALU.mult, ALU.add),
                     reads=[('big', bi), ('acc', tt)], writes=[('big', yi)])
                oi = nxt('big', 4)
                emit_layernorm(P, big[yi][:], big[oi][:], lng[:], lnb[:], stats, mv, rstd, ('big', yi), ('big', oi), ('lng', 'lnb'), 'ln')
                P.dma('sp', out_d[tix * 128:(tix + 1) * 128, :], big[oi][:], reads=[('big', oi)], key='out')

        nb = units[0][4]()
        for ui in range(len(units)):
            kind, q, e, f2, load, comp = units[ui]
            cur = nb
            if kind == 'gu' and e == e0 and f2 == 0:
                prologue(q)
            if ui + 1 < len(units):
                nb = units[ui + 1][4]()
            comp(cur)
            if kind == 'dn' and e == e0 + ne - 1:
                epilogue(q)
    return dict(out=out_d)


def k3b_inputs(h, wr, br, wgu, bgu, wd, bd, lng, lnb, acc_in=None):
    d = _k3b_inputs(h, wr, br, wgu, bgu, wd, bd, lng, lnb)
    if acc_in is not None:
        d['acc_in'] = np.ascontiguousarray(acc_in)
    return d


def _k3b_inputs(h, wr, br, wgu, bgu, wd, bd, lng, lnb):
    return dict(h=np.ascontiguousarray(h), wr=np.ascontiguousarray(wr.reshape(8, 128, N_EXP).transpose(1, 0, 2)).astype(np.float32),
                br=np.ascontiguousarray(br.reshape(1, -1)).astype(np.float32), wgu=wgu,
                bgu=np.ascontiguousarray(bgu.reshape(N_EXP, 16, 128).transpose(2, 0, 1)).astype(np.float32),
                wd=wd, bd=np.ascontiguousarray(bd).astype(np.float32),
                lng=np.ascontiguousarray(lng.reshape(1, -1)).astype(np.float32), lnb=np.ascontiguousarray(lnb.reshape(1, -1)).astype(np.float32),
                ident=np.eye(128, dtype=np.float32))


def build_l2():
    nc = bass.Bass("TRN2", target_bir_lowering=False)
    P = Prog(nc)
    phase_k2a(nc, P, 'a_')
    P.barrier()
    phase_k2b(nc, P, 'b_')
    P.emit(final_keys=['out'])
    return nc, P


def build_l3(next_layer=None):
    nc = bass.Bass("TRN2", target_bir_lowering=False)
    P = Prog(nc)
    o3a = phase_k3a(nc, P, 'a_')
    P.barrier()
    o3b = phase_k3b(nc, P, 'b_', 0, N_EXP, True, True, ext={'h': o3a['h']})
    if next_layer is not None:
        P.barrier()
        phase_k1(nc, P, 'k_', next_layer, ext={'x': o3b['out']})
    P.emit(final_keys=['out'])
    return nc, P


def _launch(nc, in_maps):
    res = run_bass_kernel_spmd(nc, in_maps, core_ids=list(range(NCORES)))
    return res.results


def _pref(px, d):
    return {px + k: v for k, v in d.items()}


def kernel(x, w_in, b_fox_f, hgrn_lb_logits, hgrn_norm_g, w_branch_a, w_branch_b, w_branch_c, w_out,
           ln1_g, ln1_b, w_router, b_router, w_gu, b_gu, w_down, b_down, ln2_g, ln2_b):
    f32 = lambda a: np.ascontiguousarray(np.asarray(a, dtype=np.float32))
    x_cur = f32(x).reshape(-1, D_MODEL)
    lbl = f32(hgrn_lb_logits)
    w_perm = np.ascontiguousarray(f32(w_in[0])[:, K1_COLS])
    r1 = _launch(build_k1(0)[0], [k1_inputs(c, x_cur[c * TOK:(c + 1) * TOK], w_perm, f32(b_fox_f[0]), lbl) for c in range(NCORES)])
    del w_perm
    for l in range(DEPTH):
        catT = lambda nm, b: np.concatenate([r1[2 * b][nm], r1[2 * b + 1][nm]], axis=1)
        cat0 = lambda nm, b: np.concatenate([r1[2 * b][nm], r1[2 * b + 1][nm]], axis=0)
        in2 = []
        for b in range(BATCH):
            f = dict(aqT=catT('aqT', b), akT=catT('akT', b), av=cat0('av', b), fqT=catT('fqT', b), fkT=catT('fkT', b),
                     fv=cat0('fv', b), iqT=catT('iqT', b), ikT=catT('ikT', b), small=cat0('small', b))
            h = dict(qpT=catT('qpT', b), kpT=catT('kpT', b), kp=cat0('kp', b), cv=cat0('cv', b), em=catT('em', b), g=catT('g', b))
            for j in range(2):
                d = _pref('a_', k2a_inputs(j, f['aqT'], f['akT'], f['av'], f['fqT'], f['fkT'], f['fv'], f['iqT'], f['ikT'], f['small']))
                d.update(_pref('b_', k2b_inputs(j, h['qpT'], h['kpT'], h['kp'], h['cv'], h['em'], h['g'])))
                in2.append(d)
            del f, h
        r2 = _launch(build_l2()[0], in2)
        del in2
        nxt_l = l + 1 if l + 1 < DEPTH else None
        wgu_l, wd_l = f32(w_gu[l]), f32(w_down[l])
        if nxt_l is not None:
            w_perm = np.ascontiguousarray(f32(w_in[nxt_l])[:, K1_COLS])
        in3 = []
        for b in range(BATCH):
            oa = np.empty((64, 8, SEQ), dtype=NPBF)
            ob = np.empty((64, 8, SEQ), dtype=NPBF)
            for j in range(2):
                cols = _slot_cols(j)
                oa[:, :, cols] = r2[2 * b + j]['a_oaT']
                ob[:, :, cols] = r2[2 * b + j]['a_obT']
            oc = np.concatenate([r2[2 * b]['b_ocT'], r2[2 * b + 1]['b_ocT']], axis=1)
            for cc in range(2):
                c = 2 * b + cc
                sl = slice(cc * TOK, (cc + 1) * TOK)
                d = _pref('a_', k3a_inputs(np.ascontiguousarray(oa[:, :, sl]), np.ascontiguousarray(ob[:, :, sl]), np.ascontiguousarray(oc[:, :, sl]),
                                           r1[c]['cgT'], r1[c]['gT'], x_cur[c * TOK:(c + 1) * TOK],
                                           f32(w_branch_a[l]), f32(w_branch_b[l]), f32(w_branch_c[l]), f32(w_out[l]),
                                           f32(hgrn_norm_g[l]), f32(ln1_g[l]), f32(ln1_b[l])))
                d3b = _k3b_inputs(np.zeros((1, 1), np.float32), f32(w_router[l]), f32(b_router[l]), wgu_l, f32(b_gu[l]), wd_l, f32(b_down[l]),
                                  f32(ln2_g[l]), f32(ln2_b[l]))
                del d3b['h']
                d.update(_pref('b_', d3b))
                if nxt_l is not None:
                    d1 = k1_inputs(c, np.zeros((1, 1), np.float32), w_perm, f32(b_fox_f[nxt_l]), lbl)
                    del d1['x']
                    d.update(_pref('k_', d1))
                in3.append(d)
        del r1, r2
        r3 = _launch(build_l3(nxt_l)[0], in3)
        del in3
        x_cur = np.concatenate([np.asarray(r3[c]['b_out'], dtype=np.float32) for c in range(NCORES)], axis=0)
        if nxt_l is not None:
            r1 = [{k[2:]: v for k, v in r3[c].items() if k.startswith('k_')} for c in range(NCORES)]
        del r3
    return x_cur.reshape(BATCH, SEQ, D_MODEL)
```
